# Optimizing a Trainium2 kernel written in Bass

```python
import math
import jax, jax.numpy as jnp
from jax import lax
import numpy as np

D_MODEL = 1024
BATCH = 8
SEQ = 8192
DEPTH = 2

MIX_WIDTH = D_MODEL // 2
RET_HEAD_DIM = 128
RET_HEADS = MIX_WIDTH // RET_HEAD_DIM
RET_CHUNK = 128
ROPE_BASE = 10000.0
RWKV_HEAD_DIM = 64
RWKV_HEADS = MIX_WIDTH // RWKV_HEAD_DIM
DECAY_LORA = 64
AAA_LORA = 64
GATE_LORA = 128
CONV_WIDTH = 3
N_BRANCHES = 3
RET_COLS = 4 * MIX_WIDTH
RWKV_COLS = 3 * MIX_WIDTH + DECAY_LORA + AAA_LORA + GATE_LORA
CONV_COLS = 3 * MIX_WIDTH
IN_COLS = RET_COLS + RWKV_COLS + CONV_COLS
N_GROUPS = 4
EXPERTS_PER_GROUP = 8
N_EXPERTS = N_GROUPS * EXPERTS_PER_GROUP
EXPERT_FF = D_MODEL // 2
TOP_K = 2
MOE_BLOCK = 128
NORM_EPS = 1e-6
HEAD_NORM_EPS = 1e-5
RWKV_NORM_EPS = 64e-5

kernel_name = 'hybrid_retention_rwkv7_shortconv_hmoe_block'

F32 = jnp.float32


def rms_norm(x, g):
    xf = x.astype(F32)
    y = xf * lax.rsqrt(jnp.mean(xf * xf, axis=-1, keepdims=True) + NORM_EPS)
    return (y * g.astype(F32)).astype(x.dtype)


def modulate(h, shift, scale):
    return (h.astype(F32) * (1.0 + scale[:, None, :]) + shift[:, None, :]).astype(h.dtype)


def head_norm(y, g, eps):
    yf = y.astype(F32)
    mu = jnp.mean(yf, axis=-1, keepdims=True)
    var = jnp.mean(jnp.square(yf - mu), axis=-1, keepdims=True)
    return (yf - mu) * lax.rsqrt(var + eps) * g.astype(F32).reshape(y.shape[-2:])


def rotary(t, positions):
    half = t.shape[-1] // 2
    inv_freq = ROPE_BASE ** (-jnp.arange(half, dtype=F32) / half)
    ang = positions.astype(F32)[..., None] * inv_freq
    cos = jnp.cos(ang)[:, :, None, :]
    sin = jnp.sin(ang)[:, :, None, :]
    t1 = t[..., :half].astype(F32)
    t2 = t[..., half:].astype(F32)
    return jnp.concatenate([t1 * cos - t2 * sin, t1 * sin + t2 * cos], axis=-1)


def retention(q, k, v, g, positions, gn_g):
    bsz, seq, _ = q.shape
    split = lambda t: t.reshape(bsz, seq, RET_HEADS, RET_HEAD_DIM)
    qh = rotary(split(q), positions)
    kh = rotary(split(k), positions) * (RET_HEAD_DIM ** -0.5)
    vh = split(v).astype(F32)
    n_chunks = seq // RET_CHUNK

    def to_chunks(t):
        return t.reshape(bsz, n_chunks, RET_CHUNK, RET_HEADS, RET_HEAD_DIM).transpose(1, 0, 3, 2, 4)

    log_gamma = jnp.log1p(-jnp.exp2(-5.0 - jnp.arange(RET_HEADS, dtype=F32)))
    pos = jnp.arange(RET_CHUNK, dtype=F32)
    rel = pos[:, None] - pos[None, :]
    decay_in = jnp.where(rel >= 0, jnp.exp(log_gamma[:, None, None] * jnp.maximum(rel, 0.0)), 0.0)
    decay_k = jnp.exp(log_gamma[:, None] * (RET_CHUNK - 1 - pos))
    decay_q = jnp.exp(log_gamma[:, None] * (pos + 1.0))
    decay_chunk = jnp.exp(log_gamma * RET_CHUNK)

    def chunk_step(state, qkv):
        qc, kc, vc = qkv
        scores = jnp.einsum('bhid,bhjd->bhij', qc, kc) * decay_in
        inner = jnp.einsum('bhij,bhjv->bhiv', scores, vc)
        cross = jnp.einsum('bhid,bhdv->bhiv', qc, state) * decay_q[None, :, :, None]
        state = state * decay_chunk[None, :, None, None] + jnp.einsum(
            'bhjd,bhjv->bhdv', kc * decay_k[None, :, :, None], vc)
        return state, inner + cross

    state0 = jnp.zeros((bsz, RET_HEADS, RET_HEAD_DIM, RET_HEAD_DIM), F32)
    _, out = lax.scan(chunk_step, state0, (to_chunks(qh), to_chunks(kh), to_chunks(vh)))
    out = out.transpose(1, 0, 3, 2, 4).reshape(bsz, seq, RET_HEADS, RET_HEAD_DIM)
    out = head_norm(out, gn_g, HEAD_NORM_EPS).reshape(bsz, seq, MIX_WIDTH)
    return (jax.nn.silu(g.astype(F32)) * out).astype(q.dtype)


def rwkv7_time_mix(p, mu, w0, w_lora, a0, a_lora, g_lora, k_k, k_a, r_k, gn_g):
    bsz, seq, _ = p.shape
    mw = MIX_WIDTH
    pf = p.astype(F32)
    prev = jnp.pad(pf, ((0, 0), (1, 0), (0, 0)))[:, :-1]
    pf = pf + mu * (prev - pf)
    o_w = 3 * mw
    o_a = o_w + DECAY_LORA
    o_g = o_a + AAA_LORA
    r = pf[..., 0:mw]
    k = pf[..., mw:2 * mw]
    v = pf[..., 2 * mw:3 * mw]
    wd = pf[..., o_w:o_a]
    ad = pf[..., o_a:o_g]
    gd = pf[..., o_g:o_g + GATE_LORA]
    w = jnp.exp(-math.exp(-0.5) * jax.nn.sigmoid(w0 + jnp.tanh(wd) @ w_lora))
    a = jax.nn.sigmoid(a0 + ad @ a_lora)
    g = jax.nn.sigmoid(gd) @ g_lora
    heads = lambda t: t.reshape(bsz, seq, RWKV_HEADS, RWKV_HEAD_DIM)
    r, w, k, v, a = heads(r), heads(w), heads(k), heads(v), heads(a)
    kk = k * k_k.reshape(RWKV_HEADS, RWKV_HEAD_DIM)
    kh = kk * lax.rsqrt(jnp.maximum(jnp.sum(kk * kk, axis=-1, keepdims=True), 1e-12))
    kt = k * (1.0 + (a - 1.0) * k_a.reshape(RWKV_HEADS, RWKV_HEAD_DIM))

    def time_step(state, inp):
        rt, wt, ktt, vt, kht, at = inp
        sk = jnp.einsum('bhvk,bhk->bhv', state, kht)
        state = (state * wt[:, :, None, :]
                 - sk[..., None] * (at * kht)[:, :, None, :]
                 + vt[..., None] * ktt[:, :, None, :])
        return state, jnp.einsum('bhvk,bhk->bhv', state, rt)

    tm = lambda t: jnp.moveaxis(t, 1, 0)
    state0 = jnp.zeros((bsz, RWKV_HEADS, RWKV_HEAD_DIM, RWKV_HEAD_DIM), F32)
    _, y = lax.scan(time_step, state0, (tm(r), tm(w), tm(kt), tm(v), tm(kh), tm(a)))
    y = jnp.moveaxis(y, 0, 1)
    y = head_norm(y, gn_g, RWKV_NORM_EPS)
    y = y + jnp.sum(r * kt * r_k.astype(F32), axis=-1, keepdims=True) * v
    return (y.reshape(bsz, seq, mw) * g).astype(p.dtype)


def short_conv(p, conv_w):
    h, bgate, cgate = jnp.split(p, 3, axis=-1)
    u = cgate * h
    seq = u.shape[1]
    up = jnp.pad(u, ((0, 0), (CONV_WIDTH - 1, 0), (0, 0)))
    conv = conv_w[0] * up[:, 0:seq]
    for i in range(1, CONV_WIDTH):
        conv = conv + conv_w[i] * up[:, i:i + seq]
    return bgate * conv


def hier_moe(h, w_router_group, b_router_group, w_router_expert, b_router_expert,
             w_exp_gate, w_exp_up, w_exp_down):
    bsz, seq, d = h.shape
    n_tok = bsz * seq
    xf = h.reshape(n_tok, d)
    g_logits = (xf @ w_router_group + b_router_group).astype(F32)
    g_prob = jax.nn.softmax(g_logits, axis=-1)
    g_sel = jnp.argmax(g_logits, axis=-1).astype(jnp.int32)
    p_group = jnp.take_along_axis(g_prob, g_sel[:, None], axis=-1)
    e_logits = (xf @ w_router_expert + b_router_expert).astype(F32).reshape(n_tok, N_GROUPS, EXPERTS_PER_GROUP)
    e_logits = jnp.take_along_axis(e_logits, g_sel[:, None, None], axis=1)[:, 0]
    top_v, top_i = lax.top_k(e_logits, TOP_K)
    weights = (jax.nn.softmax(top_v, axis=-1) * p_group).reshape(-1)
    eid = (g_sel[:, None] * EXPERTS_PER_GROUP + top_i).reshape(-1).astype(jnp.int32)
    tok = jnp.repeat(jnp.arange(n_tok, dtype=jnp.int32), TOP_K)
    n_assign = n_tok * TOP_K
    order = jnp.argsort(eid)
    eid_s, tok_s, w_s = eid[order], tok[order], weights[order]
    counts = jnp.bincount(eid, length=N_EXPERTS).astype(jnp.int32)
    starts = jnp.cumsum(counts) - counts
    padded = (counts + MOE_BLOCK - 1) // MOE_BLOCK * MOE_BLOCK
    pends = jnp.cumsum(padded)
    pstarts = pends - padded
    dest = pstarts[eid_s] + jnp.arange(n_assign, dtype=jnp.int32) - starts[eid_s]
    n_rows = n_assign + N_EXPERTS * MOE_BLOCK
    n_blocks = n_rows // MOE_BLOCK
    row_tok = jnp.full((n_rows,), n_tok, jnp.int32).at[dest].set(tok_s)
    row_w = jnp.zeros((n_rows,), F32).at[dest].set(w_s)
    block_e = jnp.minimum(
        jnp.searchsorted(pends, jnp.arange(n_blocks, dtype=jnp.int32) * MOE_BLOCK, side='right'),
        N_EXPERTS - 1)
    x_rows = jnp.concatenate([xf, jnp.zeros((1, d), xf.dtype)], axis=0)[row_tok]
    x_rows = x_rows.reshape(n_blocks, MOE_BLOCK, d)

    def expert_block(args):
        xb, e = args
        return (jax.nn.silu(xb @ w_exp_gate[e]) * (xb @ w_exp_up[e])) @ w_exp_down[e]

    y_rows = lax.map(expert_block, (x_rows, block_e)).reshape(n_rows, d)
    y_rows = y_rows.astype(F32) * row_w[:, None]
    out = jax.ops.segment_sum(y_rows, row_tok, num_segments=n_tok + 1)[:n_tok]
    return out.reshape(bsz, seq, d).astype(h.dtype)


def decoder_layer(x, c, positions, norm1_g, norm2_g, w_ada, b_ada, w_in, w_gate, b_gate,
                  ret_gn_g, rwkv_mu, rwkv_w0, rwkv_w_lora, rwkv_a0, rwkv_a_lora, rwkv_g_lora,
                  rwkv_k_k, rwkv_k_a, rwkv_r_k, rwkv_gn_g, conv_w, w_branch, w_o,
                  w_router_group, b_router_group, w_router_expert, b_router_expert,
                  w_exp_gate, w_exp_up, w_exp_down):
    d = D_MODEL
    mod = jax.nn.silu(c.astype(F32)) @ w_ada + b_ada
    shift1, scale1, gate1, shift2, scale2, gate2 = jnp.split(mod, 6, axis=-1)

    h = modulate(rms_norm(x, norm1_g), shift1, scale1)
    p = h @ w_in
    p_ret = p[..., :RET_COLS]
    p_rwkv = p[..., RET_COLS:RET_COLS + RWKV_COLS]
    p_conv = p[..., RET_COLS + RWKV_COLS:]
    q, k, v, g = jnp.split(p_ret, 4, axis=-1)
    y_ret = retention(q, k, v, g, positions, ret_gn_g)
    y_rwkv = rwkv7_time_mix(p_rwkv, rwkv_mu, rwkv_w0, rwkv_w_lora, rwkv_a0, rwkv_a_lora,
                            rwkv_g_lora, rwkv_k_k, rwkv_k_a, rwkv_r_k, rwkv_gn_g)
    y_conv = short_conv(p_conv, conv_w)
    gates = jax.nn.sigmoid((h @ w_gate + b_gate).astype(F32))
    merged = jnp.zeros(x.shape, F32)
    for i, y in enumerate((y_ret, y_rwkv, y_conv)):
        merged = merged + gates[..., i * d:(i + 1) * d] * (y @ w_branch[i])
    x = x + (gate1[:, None, :] * (merged.astype(x.dtype) @ w_o)).astype(x.dtype)

    h2 = modulate(rms_norm(x, norm2_g), shift2, scale2)
    y_moe = hier_moe(h2, w_router_group, b_router_group, w_router_expert, b_router_expert,
                     w_exp_gate, w_exp_up, w_exp_down)
    x = x + (gate2[:, None, :] * y_moe).astype(x.dtype)
    return x


def setup_inputs(seed: int = 0) -> dict:
    key = jax.random.key(seed)
    k = jax.random.split(key, 32)
    L, D, MW = DEPTH, D_MODEL, MIX_WIDTH

    def nrm(kk, shape, scale):
        return scale * jax.random.normal(kk, shape, jnp.float32)

    return {
        'x': nrm(k[0], (BATCH, SEQ, D), 1.0),
        'c': nrm(k[1], (BATCH, D), 1.0),
        'positions': jnp.tile(jnp.arange(SEQ, dtype=jnp.int32)[None, :], (BATCH, 1)),
        'norm1_g': 1.0 + nrm(k[2], (L, D), 0.02),
        'norm2_g': 1.0 + nrm(k[3], (L, D), 0.02),
        'final_norm_g': 1.0 + nrm(k[4], (D,), 0.02),
        'w_ada': nrm(k[5], (L, D, 6 * D), 0.5 * D ** -0.5),
        'b_ada': nrm(k[6], (L, 6 * D), 0.01),
        'w_in': nrm(k[7], (L, D, IN_COLS), D ** -0.5),
        'w_gate': nrm(k[8], (L, D, N_BRANCHES * D), D ** -0.5),
        'b_gate': nrm(k[9], (L, N_BRANCHES * D), 0.01),
        'ret_gn_g': 1.0 + nrm(k[10], (L, MW), 0.02),
        'rwkv_mu': jax.random.uniform(k[11], (L, RWKV_COLS), jnp.float32),
        'rwkv_w0': nrm(k[12], (L, MW), 0.5),
        'rwkv_w_lora': nrm(k[13], (L, DECAY_LORA, MW), DECAY_LORA ** -0.5),
        'rwkv_a0': nrm(k[14], (L, MW), 0.5),
        'rwkv_a_lora': nrm(k[15], (L, AAA_LORA, MW), AAA_LORA ** -0.5),
        'rwkv_g_lora': nrm(k[16], (L, GATE_LORA, MW), GATE_LORA ** -0.5),
        'rwkv_k_k': 0.85 + nrm(k[17], (L, MW), 0.05),
        'rwkv_k_a': 1.0 + nrm(k[18], (L, MW), 0.05),
        'rwkv_r_k': nrm(k[19], (L, RWKV_HEADS, RWKV_HEAD_DIM), 0.1),
        'rwkv_gn_g': 1.0 + nrm(k[20], (L, MW), 0.02),
        'conv_w': nrm(k[21], (L, CONV_WIDTH, MW), CONV_WIDTH ** -0.5),
        'w_branch': nrm(k[22], (L, N_BRANCHES, MW, D), MW ** -0.5),
        'w_o': nrm(k[23], (L, D, D), D ** -0.5),
        'w_router_group': nrm(k[24], (L, D, N_GROUPS), D ** -0.5),
        'b_router_group': nrm(k[25], (L, N_GROUPS), 0.01),
        'w_router_expert': nrm(k[26], (L, D, N_EXPERTS), D ** -0.5),
        'b_router_expert': nrm(k[27], (L, N_EXPERTS), 0.01),
        'w_exp_gate': nrm(k[28], (L, N_EXPERTS, D, EXPERT_FF), D ** -0.5),
        'w_exp_up': nrm(k[29], (L, N_EXPERTS, D, EXPERT_FF), D ** -0.5),
        'w_exp_down': nrm(k[30], (L, N_EXPERTS, EXPERT_FF, D), EXPERT_FF ** -0.5),
    }


def reference(x, c, positions, norm1_g, norm2_g, final_norm_g, w_ada, b_ada, w_in, w_gate, b_gate,
              ret_gn_g, rwkv_mu, rwkv_w0, rwkv_w_lora, rwkv_a0, rwkv_a_lora, rwkv_g_lora,
              rwkv_k_k, rwkv_k_a, rwkv_r_k, rwkv_gn_g, conv_w, w_branch, w_o,
              w_router_group, b_router_group, w_router_expert, b_router_expert,
              w_exp_gate, w_exp_up, w_exp_down):
    for l in range(DEPTH):
        x = decoder_layer(
            x, c, positions, norm1_g[l], norm2_g[l], w_ada[l], b_ada[l], w_in[l], w_gate[l], b_gate[l],
            ret_gn_g[l], rwkv_mu[l], rwkv_w0[l], rwkv_w_lora[l], rwkv_a0[l], rwkv_a_lora[l],
            rwkv_g_lora[l], rwkv_k_k[l], rwkv_k_a[l], rwkv_r_k[l], rwkv_gn_g[l], conv_w[l],
            w_branch[l], w_o[l], w_router_group[l], b_router_group[l], w_router_expert[l],
            b_router_expert[l], w_exp_gate[l], w_exp_up[l], w_exp_down[l])
    return rms_norm(x, final_norm_g)
```

```python
import contextlib
import math
import os
import numpy as np
import concourse.bass as bass
import concourse.mybir as mybir
from concourse.bass_utils import run_bass_kernel_spmd

ALU = mybir.AluOpType
AF = mybir.ActivationFunctionType
AX = mybir.AxisListType
F32 = mybir.dt.float32
BF16 = mybir.dt.bfloat16
I32 = mybir.dt.int32
EPOCH = 30000

D = 1024
MW = 512
TB = 512
L_DEPTH = 2
NE = 32
FF = 512
CDEC = math.exp(-0.5)
O_N1, O_N2, O_BADA, O_BGATE, O_RGN, O_MU, O_W0, O_A0, O_KK, O_KA, O_RK, O_WGN, O_CONVW, O_FN, NS = \
    0, 8, 16, 64, 88, 92, 106, 110, 114, 118, 122, 126, 130, 142, 150
C_ID, C_MEAND, C_MEAN128, C_B64MEAN, C_B64ONES, C_RMT, C_DT, C_XI, C_ZETA, C_INVF, C_MST, C_MIT, C_MSX, C_I64, C_SEG, C_IOTA32, C_MB, C_PID, C_LTRI, C_ONES, NCONST = \
    0, 128, 256, 384, 512, 640, 768, 1280, 1792, 1796, 1797, 2309, 2821, 3333, 3845, 4357, 4389, 4645, 4646, 4774, 4902
RT = 2
BLK = 128 * RT


class Buf:
    __slots__ = ("name", "w", "r", "excl", "rowgrp")

    def __init__(self, name="", excl=False):
        self.name = name
        self.w = None
        self.r = {}
        self.excl = excl
        self.rowgrp = None


class Tl:
    __slots__ = ("h", "b")

    def __init__(self, h, name="", excl=False):
        self.h = h
        self.b = Buf(name, excl)

    def __getitem__(self, idx):
        return self.h[idx]


class Sched:
    def __init__(self, nc, stack, n_dma=24, same_sync=True):
        self.nc = nc
        self.stack = stack
        self.E = {"pe": nc.tensor, "act": nc.scalar, "dve": nc.vector, "pool": nc.gpsimd, "sp": nc.sync}
        self.sem = {}
        self.cnt = {k: 0 for k in ("pe", "act", "dve", "pool")}
        self.ndma = n_dma
        for cls in ("dh", "ds"):
            for j in range(n_dma):
                self.sem[(cls, j)] = stack.enter_context(nc.semaphore(f"s_{cls}{j}"))
        self.dval = {(cls, j): 0 for cls in ("dh", "ds") for j in range(n_dma)}
        self.di = {"dh": 0, "ds": 0}
        self.seen = {e: {} for e in self.E}
        self.same_sync = same_sync
        self.nins = 0
        self.rec = []
        self.lazy_pe = os.environ.get("LAZY_PE", "0") == "1"
        self.pe_emitted = 0
        self.pe_last_ins = None
        self.pe_mark_idx = []
        self.pe_mark_kv = []
        self.pe_seen_idx = {}
        self.reorder = os.environ.get("NO_REORDER", "0") != "1"
        self.window = int(os.environ.get("SCHED_WINDOW", "6"))

    def _semfor(self, key):
        if key not in self.sem:
            self.sem[key] = self.stack.enter_context(self.nc.semaphore(f"s_{key[0]}{key[1]}"))
        return self.sem[key]

    def _resolve_pe(self, idx):
        import bisect
        j = bisect.bisect_left(self.pe_mark_idx, idx)
        if j < len(self.pe_mark_idx):
            return self.pe_mark_kv[j]
        c = len(self.pe_mark_idx)
        key, v = ("pe", c // EPOCH), c % EPOCH + 1
        self.pe_last_ins.then_inc(self._semfor(key), 1)
        self.pe_mark_idx.append(self.pe_emitted)
        self.pe_mark_kv.append((key, v))
        return key, v

    def _wait(self, e, key, val):
        if key == ("pe", "lazy"):
            if val <= 0 or self.pe_seen_idx.get(e, 0) >= val:
                return
            self.pe_seen_idx[e] = val
            key, val = self._resolve_pe(val)
        if val <= 0 or self.seen[e].get(key, 0) >= val:
            return
        self.E[e].wait_ge(self._semfor(key), val)
        self.seen[e][key] = val
        self.nins += 1

    def _deps(self, e, reads, writes):
        need = {}

        def add(k, v):
            if k[0] == e and (e == "pe" or not self.same_sync):
                return
            if need.get(k, 0) < v:
                need[k] = v

        for b in reads:
            if b.w is not None:
                add(*b.w)
            if b.excl:
                for k, v in b.r.items():
                    if k[0] != e:
                        add(k, v)
        for b in writes:
            if b.w is not None:
                add(*b.w)
            for k, v in b.r.items():
                add(k, v)
        for k, v in need.items():
            self._wait(e, k, v)

    @staticmethod
    def _stamp(st, reads, writes):
        k, v = st
        for b in reads:
            if b.r.get(k, 0) < v:
                b.r[k] = v
        for b in writes:
            b.w = st
            b.r = {}

    def op(self, e, fn, reads=(), writes=(), cost=200.0, rowgrp=-1):
        reads = [getattr(x, "b", x) for x in reads]
        writes = [getattr(x, "b", x) for x in writes]
        self.rec.append(("op", e, fn, reads, writes, float(cost), float(cost), rowgrp))

    def dma(self, q, out, in_, reads=(), writes=(), starter=None, nbytes=65536, **kw):
        reads = [getattr(x, "b", x) for x in reads]
        writes = [getattr(x, "b", x) for x in writes]
        if starter is None:
            starter = lambda E: E.dma_start(out=out, in_=in_, **kw)
        issue = 1200.0 if q == "pool" else 150.0
        lat = 2500.0 + nbytes / 150.0
        self.rec.append(("dma", q, starter, reads, writes, issue, lat, -1))

    def _emit_op(self, e, fn, reads, writes, rowgrp):
        if rowgrp != -1 and writes:
            b = writes[0]
            if rowgrp is not None and b.rowgrp is not None and b.rowgrp != rowgrp and b.w is not None and b.w[0][0] == "pe":
                self._wait("pe", b.w[0], b.w[1])
            b.rowgrp = rowgrp
        self._deps(e, reads, writes)
        ins = fn(self.E[e])
        self.nins += 1
        if e == "pe" and self.lazy_pe:
            self.pe_emitted += 1
            self.pe_last_ins = ins
            self.cnt[e] += 1
            self._stamp((("pe", "lazy"), self.pe_emitted), reads, writes)
            return
        c = self.cnt[e]
        ep, v = c // EPOCH, c % EPOCH + 1
        self.cnt[e] = c + 1
        key = (e, ep)
        ins.then_inc(self._semfor(key), 1)
        self._stamp((key, v), reads, writes)

    def _emit_dma(self, q, starter, reads, writes):
        cls = "ds" if q == "pool" else "dh"
        j = self.di[cls] % self.ndma
        self.di[cls] += 1
        key = (cls, j)
        self._wait(q, key, self.dval[key])
        self._deps(q, reads, writes)
        ins = starter(self.E[q])
        ins.then_inc(self.sem[key], 16)
        self.dval[key] += 16
        self.nins += 1
        self._stamp((key, self.dval[key]), reads, writes)

    def flush(self):
        rec, self.rec = self.rec, []
        n = len(rec)
        if n == 0:
            return
        order = range(n)
        if self.reorder and n > 2:
            order = self._schedule(rec)
        for i in order:
            kind, e, fn, reads, writes, _, _, rowgrp = rec[i]
            if kind == "op":
                self._emit_op(e, fn, reads, writes, rowgrp)
            else:
                self._emit_dma(e, fn, reads, writes)

    def _schedule(self, rec):
        import heapq
        n = len(rec)
        lastw, readers = {}, {}
        npred = [0] * n
        succ = [[] for _ in range(n)]
        for i, (kind, e, fn, reads, writes, ic, lat, rg) in enumerate(rec):
            deps = set()
            for b in reads:
                w = lastw.get(id(b))
                if w is not None:
                    deps.add(w)
            for b in writes:
                w = lastw.get(id(b))
                if w is not None:
                    deps.add(w)
                for r in readers.get(id(b), ()):
                    deps.add(r)
            deps.discard(i)
            npred[i] = len(deps)
            for d in deps:
                succ[d].append(i)
            for b in reads:
                readers.setdefault(id(b), []).append(i)
            for b in writes:
                lastw[id(b)] = i
                readers[id(b)] = []
        engs = ("pe", "act", "dve", "pool", "sp")
        prio_mode = os.environ.get("SCHED_PRIO", "cp")
        if prio_mode == "cp":
            bl = [0.0] * n
            for i in range(n - 1, -1, -1):
                m = 0.0
                for j in succ[i]:
                    if bl[j] > m:
                        m = bl[j]
                bl[i] = m + rec[i][6]
            if os.environ.get("SCHED_DEBUG") == "1":
                print(f"[sched] critical path us={max(bl) / 1e3:.1f}", flush=True)
            rank = sorted(range(n), key=lambda i: (-bl[i], i))
            pos = [0] * n
            for r_, i in enumerate(rank):
                pos[i] = r_
            enc = lambda i: pos[i]
            dec = lambda p: rank[p]
        else:
            enc = dec = lambda i: i
        ready = {e: [] for e in engs}
        avail = [0.0] * n
        tfree = {e: 0.0 for e in engs}
        for i in range(n):
            if npred[i] == 0:
                heapq.heappush(ready[rec[i][1]], enc(i))
        out = []
        W = self.window
        DELTA = 300.0
        SYNC = 350.0
        done = 0
        while done < n:
            best = None
            for e in engs:
                h = ready[e]
                if not h:
                    continue
                te = tfree[e]
                cand = heapq.nsmallest(W, h) if len(h) > 1 else h
                for p in cand:
                    i = dec(p)
                    st = avail[i] if avail[i] > te else te
                    if best is None or st < best[0] - DELTA or (abs(st - best[0]) <= DELTA and p < best[1]):
                        best = (st, p, e)
                    if st <= te:
                        break
            st, p, e = best
            i = dec(p)
            h = ready[e]
            if h[0] == p:
                heapq.heappop(h)
            else:
                h.remove(p)
                heapq.heapify(h)
            kind, _, _, _, _, ic, lat, _ = rec[i]
            tfree[e] = st + ic
            fin = st + lat
            out.append(i)
            done += 1
            for j in succ[i]:
                a = fin + (SYNC if rec[j][1] != e else 60.0)
                if a > avail[j]:
                    avail[j] = a
                npred[j] -= 1
                if npred[j] == 0:
                    heapq.heappush(ready[rec[j][1]], enc(j))
        if os.environ.get("SCHED_DEBUG") == "1":
            busy = {e: 0.0 for e in engs}
            for r in rec:
                busy[r[1]] += r[5]
            print(f"[sched] n={n} makespan_us={max(tfree.values()) / 1e3:.1f} busy_us=" + " ".join(f"{e}:{busy[e] / 1e3:.1f}" for e in engs), flush=True)
        return out

    def cur(self, k):
        if k == "pe" and self.lazy_pe:
            return (("pe", "lazy"), self.pe_emitted) if self.pe_emitted > 0 else None
        c = self.cnt[k]
        return ((k, (c - 1) // EPOCH), (c - 1) % EPOCH + 1) if c > 0 else None

    def barrier(self, engines=("pe", "act", "dve", "pool", "sp")):
        self.flush()
        for e in engines:
            for key, v in self.dval.items():
                self._wait(e, key, v)
            for k in ("pe", "act", "dve", "pool"):
                if k == e:
                    continue
                st = self.cur(k)
                if st is not None:
                    self._wait(e, st[0], st[1])


class _VH:
    dtype = F32

    def __init__(self, h, k):
        nd = len(h.shape)
        flat = h[:].rearrange("p a c -> p (a c)") if nd == 3 else h[:]
        self.ap = flat[:, k * 1024:(k + 1) * 1024].bitcast(F32)

    def __getitem__(self, idx):
        return self.ap[idx]


class View:
    __slots__ = ("h", "b")

    def __init__(self, base, k, name):
        self.h = _VH(base.h, k)
        self.b = Buf(name)

    def __getitem__(self, idx):
        return self.h[idx]


class Pool_:
    def __init__(self, tiles):
        self.free = list(tiles)

    def get(self):
        return self.free.pop(0)

    def put(self, *ts):
        for t in ts:
            self.free.append(t)


class Builder:
    def __init__(self, T, dbg=False, stages=("conv", "ret", "rwkv", "moe"), nlayers=L_DEPTH, same_sync=True):
        assert T % 1024 == 0
        self.T = T
        self.NB = T // TB
        self.stages = stages
        self.nlayers = nlayers
        self.same_sync = same_sync
        self.sparse = os.environ.get("MOE_DENSE", "0") != "1"
        self.nc = bass.Bass("TRN2", target_bir_lowering=False)
        self.dbg = dbg

    def sb(self, st, name, shape, dt):
        self._uid = getattr(self, "_uid", 0) + 1
        return Tl(st.enter_context(self.nc.sbuf_tensor(f"sb{self._uid}_{name}", shape, dt)), name)

    @staticmethod
    def _fsz(ap):
        n = 1
        for d in ap.shape[1:]:
            n *= int(d)
        return n

    def _ecost(self, eng, ap):
        n = self._fsz(ap)
        return (130.0 + 1.05 * n) if eng == "dve" else (200.0 + 2.0 * n)

    def mm(self, ps, out_ap, lt, lhsT_ap, rt, rhs_ap, start=True, stop=True, rowgrp=None):
        n = self._fsz(rhs_ap)
        cost = 64.0 + 0.45 * n
        if lt.h.dtype == F32:
            cost *= 4.0
        self.S.op("pe", lambda e: e.matmul(out_ap, lhsT_ap, rhs_ap, start=start, stop=stop), [lt, rt], [ps], cost=cost, rowgrp=rowgrp)

    def act(self, out_t, out_ap, in_t, in_ap, func, bias=None, scale=None, extra=()):
        kw = {}
        if bias is not None:
            kw["bias"] = bias
        if scale is not None:
            kw["scale"] = scale
        self.S.op("act", lambda e: e.activation(out=out_ap, in_=in_ap, func=func, **kw), [in_t, *extra], [out_t], cost=230.0 + 0.85 * self._fsz(out_ap))

    def tt(self, eng, out_t, out_ap, a_t, a_ap, b_t, b_ap, op):
        self.S.op(eng, lambda e: e.tensor_tensor(out=out_ap, in0=a_ap, in1=b_ap, op=op), [a_t, b_t], [out_t], cost=self._ecost(eng, out_ap))

    def ts(self, eng, out_t, out_ap, a_t, a_ap, s1, s2=None, op0=ALU.mult, op1=None, extra=()):
        if op1 is None:
            self.S.op(eng, lambda e: e.tensor_scalar(out=out_ap, in0=a_ap, scalar1=s1, scalar2=None, op0=op0), [a_t, *extra], [out_t], cost=self._ecost(eng, out_ap))
        else:
            self.S.op(eng, lambda e: e.tensor_scalar(out=out_ap, in0=a_ap, scalar1=s1, scalar2=s2, op0=op0, op1=op1), [a_t, *extra], [out_t], cost=self._ecost(eng, out_ap))

    def stt(self, eng, out_t, out_ap, a_t, a_ap, scalar, b_t, b_ap, op0, op1, extra=()):
        self.S.op(eng, lambda e: e.scalar_tensor_tensor(out=out_ap, in0=a_ap, scalar=scalar, in1=b_ap, op0=op0, op1=op1),
                  [a_t, b_t, *extra], [out_t], cost=self._ecost(eng, out_ap))

    def cp(self, eng, out_t, out_ap, in_t, in_ap):
        if eng == "act":
            self.S.op("act", lambda e: e.copy(out=out_ap, in_=in_ap), [in_t], [out_t], cost=230.0 + 0.85 * self._fsz(out_ap))
        else:
            self.S.op(eng, lambda e: e.tensor_copy(out=out_ap, in_=in_ap), [in_t], [out_t], cost=self._ecost(eng, out_ap))

    def memset(self, t, ap, val, eng="pool"):
        self.S.op(eng, lambda e: e.memset(ap, val), [], [t], cost=150.0 + 0.5 * self._fsz(ap))

    def reduce(self, out_t, out_ap, in_t, in_ap, op, extra=()):
        self.S.op("dve", lambda e: e.tensor_reduce(out=out_ap, in_=in_ap, axis=AX.X, op=op), [in_t, *extra], [out_t], cost=130.0 + 1.05 * self._fsz(in_ap))

    def scan(self, out_t, out_ap, d0_t, d0_ap, d1_t, d1_ap):
        self.S.op("dve", lambda e: e.tensor_tensor_scan(out=out_ap, data0=d0_ap, data1=d1_ap, initial=0.0, op0=ALU.mult, op1=ALU.add),
                  [d0_t, d1_t], [out_t], cost=130.0 + 2.1 * self._fsz(out_ap))

    def recip(self, out_t, out_ap, in_t, in_ap):
        self.S.op("dve", lambda e: e.reciprocal(out=out_ap, in_=in_ap), [in_t], [out_t], cost=120.0 + 4.0 * self._fsz(out_ap))

    def rsqrt_inplace(self, t, ap, src_t, src_ap, eps):
        self.act(t, ap, src_t, src_ap, AF.Sqrt, bias=self.eps_ap(eps), scale=1.0, extra=[self.epsT])
        self.recip(t, ap, t, ap)

    def eps_ap(self, eps):
        return self.epsT[:, self.eps_idx[eps]:self.eps_idx[eps] + 1]

    def build(self):
        nc = self.nc
        T = self.T
        Lr = L_DEPTH
        dt_in = lambda n, s, d=F32: nc.dram_tensor(n, s, d, kind="ExternalInput").ap()
        self.xT_in = dt_in("xT", [D, T])
        self.pos_in = dt_in("pos", [1, T], I32)
        self.cT_in = dt_in("cT", [128, 8])
        self.small_in = dt_in("small", [Lr, 128, NS])
        self.consts_in = dt_in("consts", [128, NCONST])
        self.w_ada = dt_in("w_ada", [Lr, D, 6 * D])
        self.w_in = dt_in("w_in", [Lr, D, 5376])
        self.w_gate = dt_in("w_gate", [Lr, D, 3 * D])
        self.w_lora = dt_in("rwkv_w_lora", [Lr, 64, MW])
        self.a_lora = dt_in("rwkv_a_lora", [Lr, 64, MW])
        self.g_lora = dt_in("rwkv_g_lora", [Lr, 128, MW])
        self.w_branch = dt_in("w_branch", [Lr, 3, MW, D])
        self.w_o = dt_in("w_o", [Lr, D, D])
        self.w_r = dt_in("w_r", [Lr, D, 36])
        self.b_r = dt_in("b_r", [Lr, 128, 36])
        self.w_eg = dt_in("w_exp_gate", [Lr, NE, D, FF])
        self.w_eu = dt_in("w_exp_up", [Lr, NE, D, FF])
        self.w_ed = dt_in("w_exp_down", [Lr, NE, FF, D])
        self.out = nc.dram_tensor("outT", [D, T], F32, kind="ExternalOutput").ap()
        self.xmid = nc.dram_tensor("xmid", [D, T], F32).ap()
        self.xl0 = nc.dram_tensor("xl0", [D, T], F32).ap()
        self.NG = 25
        self.wbf = nc.dram_tensor("wbf", [Lr, self.NG, 128, 4096], BF16).ap()
        self.wbf_b = [[Buf(f"wbf{l}_{g}") for g in range(self.NG)] for l in range(Lr)]
        self.NBLK = (2 * T) // BLK + NE
        self.NRW = self.NBLK * BLK
        self.h2rows = nc.dram_tensor("h2rows", [T + 1, D], BF16).ap()
        self.yrows = nc.dram_tensor("yrows", [self.NRW, D], F32).ap()
        self.rowtok = nc.dram_tensor("rowtok", [self.NRW, 1], I32).ap()
        self.wexp = [[nc.dram_tensor(f"wexp{l}_{w}", [NE * 128, 4096], BF16).ap() for w in range(3)] for l in range(Lr)]
        self.wexp_b = [[Buf(f"wexp{l}_{i}") for i in range(3 * NE)] for l in range(Lr)]
        self.h2rows_b = [Buf(f"h2r{i}") for i in range(T // 128 + 1)]
        self.yrows_b = [Buf(f"yr{i}") for i in range(self.NBLK * RT)]
        self.rowtok_fill_b = Buf("rtfill")
        self.scat_b = [Buf(f"scat{i}") for i in range(2 * (T // 128))]
        self.dbg_out = {}
        if self.dbg:
            for nm, rows in (("d_h", D), ("d_ycv", MW), ("d_yrt", MW), ("d_yrw", MW), ("d_xmid", D), ("d_xl", D)):
                self.dbg_out[nm] = nc.dram_tensor(nm, [Lr, rows, T], F32, kind="ExternalOutput").ap()
        self.db = {n: [Buf(f"{n}{b}") for b in range(self.NB)] for n in ("xmid", "xl0", "out")}

        with contextlib.ExitStack() as st:
            self.S = S = Sched(nc, st, n_dma=12, same_sync=self.same_sync)
            sb = lambda n, s, d: self.sb(st, n, s, d)
            self.consts = sb("consts", [128, NCONST], F32)
            self.cbf = sb("cbf", [128, 1024], BF16)
            self.small = [sb(f"small{l}", [128, NS], F32) for l in range(Lr)]
            self.der = [sb(f"der{l}", [128, 96], F32) for l in range(Lr)]
            self.epsT = sb("epsT", [128, 4], F32)
            self.eps_idx = {1e-6: 0, 1e-5: 1, 64e-5: 2, 0.0: 3}
            self.cT = sb("cT", [128, 8], F32)
            self.ret_st32 = [sb(f"rst32_{l}", [128, 4, 128], F32) for l in range(Lr)]
            self.ret_stb = [sb(f"rstb_{l}", [128, 4, 128], BF16) for l in range(Lr)]
            self.rw_S32 = [sb(f"wS32_{l}", [128, 4, 128], F32) for l in range(Lr)]
            self.rw_Sb = [sb(f"wSb_{l}", [128, 4, 128], BF16) for l in range(Lr)]
            self.uext = [[sb(f"uext{l}_{c}", [128, 514], F32) for c in range(4)] for l in range(Lr)]
            self.carry = [sb(f"carry{l}", [128, 14], F32) for l in range(Lr)]
            self.lora_bf = [sb(f"lora{l}", [128, 2, MW], BF16) for l in range(Lr)]
            self.psum = Pool_([Tl(st.enter_context(nc.psum_tensor(f"ps{i}", [128, 512], F32)), f"ps{i}", excl=True) for i in range(8)])

            self.setup()
            for l in range(self.nlayers):
                self.wconvert(l)
            for l in range(self.nlayers):
                self.layer_setup(l)
            S.barrier()
            for l in range(self.nlayers):
                src = self.xT_in if l == 0 else self.xl0
                srcb = None if l == 0 else self.db["xl0"]
                with contextlib.ExitStack() as st2:
                    self.mixer_alloc(st2)
                    self.ws_begin(l)
                    for b in range(self.NB):
                        self.mixer_block(l, b, src, srcb)
                    S.barrier()
                last = (l == self.nlayers - 1)
                dst, dstb = (self.out, self.db["out"]) if last else (self.xl0, self.db["xl0"])
                with contextlib.ExitStack() as st2:
                    if self.sparse:
                        self.moe_alloc_sparse(st2)
                        self.moe_sparse(l, dst, dstb, last)
                    else:
                        self.moe_alloc(st2)
                        for sbk in range(T // 1024):
                            self.moe_super(l, sbk, dst, dstb, last)
                    S.barrier()
            S.barrier()
        return nc

    def wgroups(self, l):
        win = self.w_in[l].rearrange("(k p) c -> p k c", p=128)
        wg_v = self.w_gate[l].rearrange("(k p) c -> p k c", p=128)
        wo_v = self.w_o[l].rearrange("(k p) c -> p k c", p=128)
        gs = []
        for c0 in (3840, 4352, 4864, 0, 512, 1024, 1536, 2048, 2560, 3072):
            gs.append((win[:, :, c0:c0 + 512], 8, 512))
        gs.append((win[:, :, 3584:3840], 8, 256))
        for i in range(3):
            wb_v = self.w_branch[l, i].rearrange("(k p) c -> p k c", p=128)
            for half in range(2):
                gs.append((wg_v[:, :, i * 1024 + half * 512:i * 1024 + (half + 1) * 512], 8, 512))
                gs.append((wb_v[:, :, half * 512:(half + 1) * 512], 4, 512))
        for half in range(2):
            gs.append((wo_v[:, :, half * 512:(half + 1) * 512], 8, 512))
        assert len(gs) == self.NG
        return gs

    def wconvert(self, l):
        for g, (src, k, c) in enumerate(self.wgroups(l)):
            dst = self.wbf[l, g][:, 0:k * c].rearrange("p (k c) -> p k c", k=k)
            self.S.dma("pool", dst, src, writes=[self.wbf_b[l][g]])

    def econvert(self, l, i):
        which, e = i // NE, i % NE
        srcs = (self.w_eg, self.w_eu, self.w_ed)
        k = 8 if which < 2 else 4
        src = srcs[which][l, e].rearrange("(k p) c -> p k c", p=128)
        dst = self.wexp[l][which][e * 128:(e + 1) * 128, :].rearrange("p (k c) -> p k c", k=k)
        self.S.dma("pool", dst, src, writes=[self.wexp_b[l][i]])

    def idma(self, out, out_off, in_, in_off, reads, writes, **kw):
        nb = 128 * self._fsz(out) * 2
        self.S.dma("pool", None, None, reads=reads, writes=writes, nbytes=nb,
                   starter=lambda E: E.indirect_dma_start(out=out, out_offset=out_off, in_=in_, in_offset=in_off, **kw))

    def ws_begin(self, l):
        self.ws_l = l
        self.ws_shapes = [(k, c) for (_, k, c) in self.wgroups(l)]
        self.ws_idx = 0
        self.ws_total = self.NB * self.NG
        self.ws_q = []
        self._ws_issue()

    def _ws_issue(self):
        l = self.ws_l
        while self.wring.free and self.ws_idx < self.ws_total:
            t = self.wring.get()
            g = self.ws_idx % self.NG
            self.ws_idx += 1
            k, c = self.ws_shapes[g]
            src = self.wbf[l, g][:, 0:k * c].rearrange("p (k c) -> p k c", k=k)
            self.S.dma("sp", t[:, 0:k, 0:c], src, reads=[self.wbf_b[l][g]], writes=[t])
            self.ws_q.append(t)

    def wnext(self):
        if not self.ws_q:
            self._ws_issue()
        return self.ws_q.pop(0)

    def wput(self, *ts):
        self.wring.put(*ts)
        self._ws_issue()

    def setup(self):
        S = self.S
        S.dma("sp", self.consts[:], self.consts_in, writes=[self.consts])
        S.dma("pool", self.cbf[:, 0:128], self.consts_in[:, C_ID:C_ID + 128], writes=[self.cbf])
        S.dma("pool", self.cbf[:, 128:256], self.consts_in[:, C_RMT:C_RMT + 128], writes=[self.cbf])
        S.dma("pool", self.cbf[:, 256:512], self.consts_in[:, C_LTRI:C_LTRI + 256], writes=[self.cbf])
        S.dma("pool", self.cbf[:, 512:1024], self.consts_in[:, C_MEAND:C_MEAND + 512], writes=[self.cbf])
        S.dma("sp", self.cT[:], self.cT_in, writes=[self.cT])
        for eps, i in self.eps_idx.items():
            self.memset(self.epsT, self.epsT[:, i:i + 1], float(eps))
        for l in range(L_DEPTH):
            S.dma("sp", self.small[l][:], self.small_in[l], writes=[self.small[l]])
            S.dma("pool", self.lora_bf[l][0:64, 0, :], self.w_lora[l], writes=[self.lora_bf[l]])
            S.dma("pool", self.lora_bf[l][64:128, 0, :], self.a_lora[l], writes=[self.lora_bf[l]])
            S.dma("pool", self.lora_bf[l][:, 1, :], self.g_lora[l], writes=[self.lora_bf[l]])
            for t in (self.ret_st32[l], self.ret_stb[l], self.rw_S32[l], self.rw_Sb[l], self.carry[l]):
                self.memset(t, t[:], 0.0)
            for c in range(4):
                self.memset(self.uext[l][c], self.uext[l][c][:], 0.0)
        self.act(self.cT, self.cT[:], self.cT, self.cT[:], AF.Silu)

    def layer_setup(self, l):
        S = self.S
        nc = self.nc
        der = self.der[l]
        sm = self.small[l]
        wa_v = self.w_ada[l].rearrange("(k p) c -> p k c", p=128)
        pm = self.psum.get()
        with contextlib.ExitStack() as st:
            wt = [self.sb(st, f"wada{l}_{i}", [128, 8, 1024], F32) for i in range(2)]
            for g in range(6):
                w = wt[g % 2]
                S.dma("sp", w[:], wa_v[:, :, g * 1024:(g + 1) * 1024], writes=[w])
                for j in range(8):
                    col = g * 8 + j
                    for k in range(8):
                        self.mm(pm, pm[:, col:col + 1], w, w[:, k, j * 128:(j + 1) * 128], self.cT, self.cT[:, k:k + 1], k == 0, k == 7)
            self.tt("dve", der, der[:, 0:48], pm, pm[:, 0:48], sm, sm[:, O_BADA:O_BADA + 48], ALU.add)
            S.barrier(("sp", "pe", "dve"))
        self.psum.put(pm)
        self.stt("dve", der, der[:, 48:56], der, der[:, 8:16], 1.0, sm, sm[:, O_N1:O_N1 + 8], ALU.add, ALU.mult)
        self.stt("dve", der, der[:, 56:64], der, der[:, 32:40], 1.0, sm, sm[:, O_N2:O_N2 + 8], ALU.add, ALU.mult)
        self.ts("dve", der, der[:, 64:78], sm, sm[:, O_MU:O_MU + 14], -1.0, 1.0, ALU.mult, ALU.add)
        self.ts("dve", der, der[:, 78:82], sm, sm[:, O_KA:O_KA + 4], -1.0, 1.0, ALU.mult, ALU.add)

    def shift1(self, l, k): return self.der[l][:, 0 + k:1 + k]
    def gate1(self, l, k): return self.der[l][:, 16 + k:17 + k]
    def shift2(self, l, k): return self.der[l][:, 24 + k:25 + k]
    def gate2(self, l, k): return self.der[l][:, 40 + k:41 + k]
    def G1(self, l, k): return self.der[l][:, 48 + k:49 + k]
    def G2(self, l, k): return self.der[l][:, 56 + k:57 + k]
    def ommu(self, l, ch): return self.der[l][:, 64 + ch:65 + ch]
    def omka(self, l, hp): return self.der[l][:, 78 + hp:79 + hp]

    def cstb(self, off):
        o = 512 + (off - C_MEAND)
        return self.cbf[:, o:o + 128]

    def cst(self, off, n=128, rows=slice(0, 128)):
        return self.consts[rows, off:off + n]

    def norm_block(self, xs, Gf, shiftf, out_fn, der_t):
        pm = self.psum.get()
        for k in range(8):
            sq = self.btmp.get()
            self.act(sq, sq[:], xs[k], xs[k][:], AF.Square)
            self.mm(pm, pm[:], self.cbf, self.cstb(C_MEAND), sq, sq[:], k == 0, k == 7)
            self.btmp.put(sq)
        rs = self.tmp.get()
        self.rsqrt_inplace(rs, rs[:], pm, pm[:], 1e-6)
        self.psum.put(pm)
        for k in range(8):
            t = self.tmp.get()
            self.stt("dve", t, t[:], xs[k], xs[k][:], Gf(k), rs, rs[:], ALU.mult, ALU.mult, extra=[der_t])
            ot, oap = out_fn(k)
            if shiftf is None:
                self.cp("act", ot, oap, t, t[:])
            else:
                self.act(ot, oap, t, t[:], AF.Identity, bias=shiftf(k), scale=1.0, extra=[der_t])
            self.tmp.put(t)
        self.tmp.put(rs)

    def wload(self, w, src_ap):
        self.S.dma("pool", w[:], src_ap, writes=[w])

    def mixer_alloc(self, st):
        sb = lambda n, s, d: self.sb(st, n, s, d)
        self.tmp = Pool_([sb(f"tmp{i}", [128, 512], F32) for i in range(15)])
        self.btmp = Pool_([sb(f"btmp{i}", [128, 512], BF16) for i in range(3)])
        self.wring = Pool_([sb(f"wr{i}", [128, 8, 512], BF16) for i in range(3)])
        self.hT = sb("hT", [128, 8, 512], BF16)
        self.cosT = sb("cosT", [128, 512], F32)
        self.sinT = sb("sinT", [128, 512], F32)
        self.ycv = sb("ycv", [128, 4, 512], BF16)
        self.yrt = sb("yrt", [128, 4, 512], BF16)
        self.yrw = sb("yrw", [128, 4, 512], BF16)
        self.B4 = [sb(f"b4_{i}", [128, 4, 512], BF16) for i in range(5)]
        self.rr = sb("rr", [128, 4, 512], F32)
        self.kk = sb("kk", [128, 4, 512], F32)
        self.lw_in = sb("lw_in", [128, 512], BF16)
        self.gsig = sb("gsig", [128, 512], BF16)
        self.pext = [sb("pext0", [128, 513], F32)] * 2
        self.YY = sb("YY", [128, 4096], BF16)
        self.PC = sb("PC", [128, 4, 8], F32)
        c64 = lambda n, w, d: sb(n, [128, w], d)
        self.KTtm = [c64("KTtm0", 512, BF16)] * 2
        self.BDtm = [c64("BDtm0", 512, BF16)] * 2
        self.Vtm = [c64("Vtm0", 512, BF16)] * 2
        self.Vpad = [c64("Vpad0", 1024, BF16)] * 2
        self.nU = c64("nU", 512, BF16)
        self.nUpad = c64("nUpad", 1024, BF16)
        self.AktT = c64("AktT", 512, BF16)
        self.BktT = c64("BktT", 512, BF16)
        self.BbT = c64("BbT", 512, BF16)
        self.Zp = [c64(f"Zp{i}", 512, BF16) for i in range(2)]
        self.Xp = [c64(f"Xp{i}", 512, BF16) for i in range(2)]
        self.RT32 = c64("RT32", 512, F32)
        self.R32 = c64("R32", 512, F32)
        self.RTb = [c64("RTb0", 512, BF16)] * 2
        self.Rb = [c64("Rb0", 512, BF16)] * 2
        self.RHSb = c64("RHSb", 512, BF16)
        self.stmp = [sb("stmp0", [128, 128], F32)] * 2
        S = self.S
        for t in (self.Vpad[0], self.nUpad):
            self.memset(t, t[:], 0.0)

    def rope_tables(self, b):
        S = self.S
        t0 = b * TB
        posi = self.tmp.get()
        S.dma("sp", posi[:].bitcast(I32), self.pos_in[:, t0:t0 + TB].partition_broadcast(128), writes=[posi])
        ang = self.tmp.get()
        kf = self.tmp.get()
        ki = self.tmp.get()
        self.cp("dve", ang, ang[:], posi, posi[:].bitcast(I32))
        self.tmp.put(posi)
        self.ts("dve", ang, ang[:], ang, ang[:], self.cst(C_INVF, 1), extra=[self.consts])
        for which, dst in ((0, self.sinT), (1, self.cosT)):
            r = self.tmp.get()
            if which == 1:
                self.ts("dve", r, r[:], ang, ang[:], float(np.pi / 2), None, ALU.add)
                src = r
            else:
                src = ang
            self.ts("dve", kf, kf[:], src, src[:], float(1.0 / (2 * np.pi)))
            kiv = ki[:].bitcast(I32)
            self.cp("dve", ki, kiv, kf, kf[:])
            self.cp("dve", kf, kf[:], ki, kiv)
            self.stt("dve", r, r[:], kf, kf[:], float(-2 * np.pi), src, src[:], ALU.mult, ALU.add)
            self.ts("dve", kf, kf[:], r, r[:], float(np.pi), float(-2 * np.pi), ALU.is_gt, ALU.mult)
            self.tt("dve", r, r[:], r, r[:], kf, kf[:], ALU.add)
            self.ts("dve", r, r[:], r, r[:], float(np.pi), float(-np.pi), ALU.min, ALU.max)
            self.act(dst, dst[:], r, r[:], AF.Sin)
            self.tmp.put(r)
        self.tmp.put(ang, kf, ki)

    def proj(self, ps, w, cols, rhs_t=None, rhs_ap_fn=None):
        for k in range(8):
            self.mm(ps, ps[:], w, w[:, k, cols], self.hT, self.hT[:, k, :], k == 0, k == 7)

    def mixer_block(self, l, b, src, srcb):
        S = self.S
        t0 = b * TB
        tsl = slice(t0, t0 + TB)
        sm = self.small[l]
        der = self.der[l]
        src_v = src.rearrange("(k p) t -> p k t", p=128)
        win_v = self.w_in[l].rearrange("(k p) c -> p k c", p=128)
        if self.sparse:
            per = -(-3 * NE // self.NB)
            for i in range(b * per, min((b + 1) * per, 3 * NE)):
                self.econvert(l, i)
        xs = [self.tmp.get() for _ in range(8)]
        for k in range(8):
            S.dma("sp", xs[k][:], src_v[:, k, tsl], reads=([srcb[b]] if srcb else []), writes=[xs[k]])
        self.norm_block(xs, lambda k: self.G1(l, k), lambda k: self.shift1(l, k), lambda k: (self.hT, self.hT[:, k, :]), der)
        self.tmp.put(*xs)
        def zero_y(y, ngroups=0):
            self.memset(y, y[:], 0.0)
            for _ in range(ngroups):
                self.wput(self.wnext())
        if "conv" in self.stages:
            self.conv_stage(l, b, win_v)
        else:
            zero_y(self.ycv, 3)
        if "ret" in self.stages:
            self.rope_tables(b)
            self.ret_stage(l, b, win_v)
        else:
            zero_y(self.yrt, 4)
        if "rwkv" in self.stages:
            self.rwkv_stage(l, b, win_v)
        else:
            zero_y(self.yrw, 4)
        if self.dbg:
            for nm, y in (("d_h", self.hT), ("d_ycv", self.ycv), ("d_yrt", self.yrt), ("d_yrw", self.yrw)):
                S.dma("pool", self.dbg_out[nm][l].rearrange("(k p) t -> p k t", p=128)[:, :, tsl], y[:], reads=[y])
        self.merge_stage(l, b, src_v, srcb)

    def conv_stage(self, l, b, win_v):
        sm = self.small[l]
        ws = [self.wnext() for _ in range(3)]
        wh, wB, wC = ws
        for cc in range(4):
            cols = slice(cc * 128, (cc + 1) * 128)
            ph, pB, pC = self.psum.get(), self.psum.get(), self.psum.get()
            self.proj(ph, wh, cols)
            self.proj(pC, wC, cols)
            self.proj(pB, wB, cols)
            hc = self.tmp.get()
            self.cp("act", hc, hc[:], ph, ph[:])
            self.psum.put(ph)
            ue = self.uext[l][cc]
            self.tt("dve", ue, ue[:, 2:514], pC, pC[:], hc, hc[:], ALU.mult)
            self.psum.put(pC)
            c1 = hc
            cw = lambda tap: sm[:, O_CONVW + tap * 4 + cc:O_CONVW + tap * 4 + cc + 1]
            self.ts("dve", c1, c1[:], ue, ue[:, 0:512], cw(0), extra=[sm])
            self.stt("dve", c1, c1[:], ue, ue[:, 1:513], cw(1), c1, c1[:], ALU.mult, ALU.add, extra=[sm])
            self.stt("dve", c1, c1[:], ue, ue[:, 2:514], cw(2), c1, c1[:], ALU.mult, ALU.add, extra=[sm])
            self.tt("dve", self.ycv, self.ycv[:, cc, :], pB, pB[:], c1, c1[:], ALU.mult)
            self.psum.put(pB)
            self.cp("pool", ue, ue[:, 0:2], ue, ue[:, 512:514])
            self.tmp.put(c1)
        self.wput(*ws)

    def ret_stage(self, l, b, win_v):
        S = self.S
        sm = self.small[l]
        qr, kr, v_tm, gs, kz_tm = self.B4
        idb = self.cbf[:, 0:128]
        rmt = self.cbf[:, 128:256]
        g128 = [math.exp(128.0 * math.log1p(-2.0 ** (-5 - h))) for h in range(4)]
        wq, wk, wv = self.wnext(), self.wnext(), self.wnext()
        for w_, dst in ((wq, qr), (wk, kr)):
            for h in range(4):
                p_ = self.psum.get()
                self.proj(p_, w_, slice(h * 128, (h + 1) * 128))
                SUB = int(os.environ.get("RET_SUB", 9))
                qb = self.btmp.get()
                if SUB >= 1:
                    self.cp("act", qb, qb[:], p_, p_[:])
                pr = self.psum.get()
                if SUB >= 2:
                    self.mm(pr, pr[:], self.cbf, rmt, qb, qb[:])
                t1, t2 = self.tmp.get(), self.tmp.get()
                if SUB >= 3:
                    self.tt("dve", t1, t1[:], p_, p_[:], self.cosT, self.cosT[:], ALU.mult)
                self.psum.put(p_)
                if SUB >= 4:
                    self.tt("dve", t2, t2[:], pr, pr[:], self.sinT, self.sinT[:], ALU.mult)
                self.psum.put(pr)
                if SUB >= 0:
                    self.tt(os.environ.get("ROT_ENG", "dve"), dst, dst[:, h, :], t1, t1[:], t2, t2[:], ALU.add)
                self.tmp.put(t1, t2)
                self.btmp.put(qb)
        self.wput(wq, wk)
        wg = self.wnext()
        for c in range(4):
            p_ = self.psum.get()
            for k in range(8):
                self.mm(p_, p_[:], self.hT, self.hT[:, k, c * 128:(c + 1) * 128], wv, wv[:, k, :], k == 0, k == 7)
            self.cp("act", v_tm, v_tm[:, c, :], p_, p_[:])
            self.psum.put(p_)
        self.wput(wv)
        for h in range(4):
            p_ = self.psum.get()
            self.proj(p_, wg, slice(h * 128, (h + 1) * 128))
            self.act(gs, gs[:, h, :], p_, p_[:], AF.Silu)
            self.psum.put(p_)
        self.wput(wg)
        for c in range(4):
            p_ = self.psum.get()
            for h in range(4):
                self.mm(p_, p_[:, h * 128:(h + 1) * 128], kr, kr[:, h, c * 128:(c + 1) * 128], self.cbf, idb)
            for h in range(4):
                self.act(kz_tm, kz_tm[:, c, h * 128:(h + 1) * 128], p_, p_[:, h * 128:(h + 1) * 128], AF.Copy,
                         scale=self.cst(C_ZETA + h, 1), extra=[self.consts])
            self.psum.put(p_)
        st32, stb = self.ret_st32[l], self.ret_stb[l]
        if int(os.environ.get("RET_STOP", 99)) <= 2:
            self.memset(self.yrt, self.yrt[:], 0.0)
            return
        for h in range(4):
            hs = slice(h * 128, (h + 1) * 128)
            psT = self.psum.get()
            for c in range(4):
                cs = slice(c * 128, (c + 1) * 128)
                self.mm(psT, psT[:, cs], kr, kr[:, h, cs], qr, qr[:, h, cs])
            sT = self.btmp.get()
            for c in range(4):
                cs = slice(c * 128, (c + 1) * 128)
                self.tt("dve", sT, sT[:, cs], psT, psT[:, cs], self.consts, self.cst(C_DT + h * 128), ALU.mult)
            self.psum.put(psT)
            pI, pC, pKV = self.psum.get(), self.psum.get(), self.psum.get()
            for c in range(4):
                cs = slice(c * 128, (c + 1) * 128)
                self.mm(pI, pI[:, cs], v_tm, v_tm[:, c, hs], sT, sT[:, cs])
            for c in range(4):
                cs = slice(c * 128, (c + 1) * 128)
                self.mm(pKV, pKV[:, cs], kz_tm, kz_tm[:, c, hs], v_tm, v_tm[:, c, hs])
            for c in range(4):
                cs = slice(c * 128, (c + 1) * 128)
                self.mm(pC, pC[:, cs], stb, stb[:, h, :], qr, qr[:, h, cs])
                self.stt("dve", st32, st32[:, h, :], st32, st32[:, h, :], float(g128[h]), pKV, pKV[:, cs], ALU.mult, ALU.add)
                self.cp("act", stb, stb[:, h, :], st32, st32[:, h, :])
            self.btmp.put(sT)
            self.psum.put(pKV)
            t1, o = self.tmp.get(), self.tmp.get()
            for c in range(4):
                cs = slice(c * 128, (c + 1) * 128)
                self.tt("dve", t1, t1[:, cs], pC, pC[:, cs], self.consts, self.cst(C_XI + h * 128), ALU.mult)
            self.psum.put(pC)
            self.tt("dve", o, o[:], pI, pI[:], t1, t1[:], ALU.add)
            self.psum.put(pI)
            self.head_norm(o, t1, C_MEAN128, 1e-5)
            self.stt("dve", self.yrt, self.yrt[:, h, :], o, o[:], sm[:, O_RGN + h:O_RGN + h + 1], gs, gs[:, h, :], ALU.mult, ALU.mult, extra=[sm])
            self.tmp.put(t1, o)

    def head_norm(self, o, scratch, mean_off, eps):
        pm = self.psum.get()
        self.mm(pm, pm[:], self.consts, self.cst(mean_off), o, o[:])
        self.tt("dve", o, o[:], o, o[:], pm, pm[:], ALU.subtract)
        self.psum.put(pm)
        sqb = self.btmp.get()
        self.act(sqb, sqb[:], o, o[:], AF.Square)
        pv = self.psum.get()
        self.mm(pv, pv[:], self.cbf, self.cstb(mean_off), sqb, sqb[:])
        self.btmp.put(sqb)
        sq = scratch
        self.rsqrt_inplace(sq, sq[:], pv, pv[:], eps)
        self.psum.put(pv)
        self.tt("dve", o, o[:], o, o[:], sq, sq[:], ALU.mult)

    def rwkv_stage(self, l, b, win_v):
        S = self.S
        sm = self.small[l]
        der = self.der[l]
        KT, BD, vv, g_rw, bon = self.B4
        rr, kk = self.rr, self.kk
        YY5 = self.YY[:].rearrange("p (hp c two t) -> p hp c two t", hp=4, c=8, two=2)
        idb = self.cbf[:, 0:128]
        lora = self.lora_bf[l]
        carry = self.carry[l]
        S32, Sb = self.rw_S32[l], self.rw_Sb[l]

        def shifted(p_, ch, out_t, out_ap, rows=slice(0, 128)):
            pe_ = self.pext[ch % 2]
            self.cp("act", pe_, pe_[:, 1:513], p_, p_[:])
            self.cp("pool", pe_, pe_[:, 0:1], carry, carry[:, ch:ch + 1])
            t = self.tmp.get()
            self.act(t, t[:], p_, p_[:], AF.Copy, scale=self.ommu(l, ch), extra=[der])
            self.stt("dve", out_t, out_ap, pe_, pe_[:, 0:512], sm[:, O_MU + ch:O_MU + ch + 1], t, t[:], ALU.mult, ALU.add, extra=[sm])
            self.cp("pool", carry, carry[:, ch:ch + 1], pe_, pe_[:, 512:513])
            self.tmp.put(t)

        for gi, dst in ((0, rr), (1, kk), (2, vv)):
            w = self.wnext()
            for cc in range(4):
                p_ = self.psum.get()
                self.proj(p_, w, slice(cc * 128, (cc + 1) * 128))
                shifted(p_, gi * 4 + cc, dst, dst[:, cc, :])
                self.psum.put(p_)
            self.wput(w)
        w = self.wnext()
        lo = self.tmp.get()
        p_ = self.psum.get()
        self.proj(p_, w, slice(0, 128))
        shifted(p_, 12, lo, lo[:])
        self.psum.put(p_)
        self.act(self.lw_in, self.lw_in[0:64, :], lo, lo[0:64, :], AF.Tanh)
        self.cp("act", self.lw_in, self.lw_in[64:128, :], lo, lo[64:128, :])
        p_ = self.psum.get()
        self.proj(p_, w, slice(128, 256))
        shifted(p_, 13, lo, lo[:])
        self.psum.put(p_)
        self.act(self.gsig, self.gsig[:], lo, lo[:], AF.Sigmoid)
        self.tmp.put(lo)
        self.wput(w)

        for hp in range(4):
            hc = slice(hp * 128, (hp + 1) * 128)
            sc = lambda off: sm[:, off + hp:off + hp + 1]
            pw = self.psum.get()
            self.mm(pw, pw[:], lora, lora[0:64, 0, hc], self.lw_in, self.lw_in[0:64, :], rowgrp=0)
            sgw = self.tmp.get()
            self.act(sgw, sgw[:], pw, pw[:], AF.Sigmoid, bias=sc(O_W0), scale=1.0, extra=[sm])
            self.psum.put(pw)
            pa = self.psum.get()
            self.mm(pa, pa[:], lora, lora[64:128, 0, hc], self.lw_in, self.lw_in[64:128, :], rowgrp=1)
            a_ = self.tmp.get()
            self.act(a_, a_[:], pa, pa[:], AF.Sigmoid, bias=sc(O_A0), scale=1.0, extra=[sm])
            self.psum.put(pa)
            pg = self.psum.get()
            self.mm(pg, pg[:], lora, lora[:, 1, hc], self.gsig, self.gsig[:])
            self.cp("act", g_rw, g_rw[:, hp, :], pg, pg[:])
            self.psum.put(pg)
            sqb = self.btmp.get()
            self.act(sqb, sqb[:], kk, kk[:, hp, :], AF.Square, scale=sc(O_KK), extra=[sm])
            pss = self.psum.get()
            self.mm(pss, pss[:], self.cbf, self.cstb(C_B64ONES), sqb, sqb[:])
            self.btmp.put(sqb)
            rn = self.tmp.get()
            self.ts("dve", rn, rn[:], pss, pss[:], 1e-12, None, ALU.max)
            self.psum.put(pss)
            self.act(rn, rn[:], rn, rn[:], AF.Sqrt, bias=self.eps_ap(0.0), scale=1.0, extra=[self.epsT])
            self.recip(rn, rn[:], rn, rn[:])
            kh = self.tmp.get()
            self.stt("dve", kh, kh[:], kk, kk[:, hp, :], sc(O_KK), rn, rn[:], ALU.mult, ALU.mult, extra=[sm])
            self.tmp.put(rn)
            kt = self.tmp.get()
            self.ts("dve", kt, kt[:], a_, a_[:], sc(O_KA), self.omka(l, hp), ALU.mult, ALU.add, extra=[sm, der])
            self.tt("dve", kt, kt[:], kt, kt[:], kk, kk[:, hp, :], ALU.mult)
            bb = a_
            self.tt("pool", bb, bb[:], a_, a_[:], kh, kh[:], ALU.mult)
            cum = self.tmp.get()
            self.scan(cum, cum[:], self.consts, self.cst(C_SEG, 512), sgw, sgw[:])
            einc, eneg = self.tmp.get(), self.tmp.get()
            self.act(einc, einc[:], cum, cum[:], AF.Exp, scale=-CDEC)
            self.act(eneg, eneg[:], cum, cum[:], AF.Exp, scale=CDEC)
            eexc = cum
            self.tt("dve", eexc, eexc[:], cum, cum[:], sgw, sgw[:], ALU.subtract)
            self.act(eexc, eexc[:], eexc, eexc[:], AF.Exp, scale=-CDEC)
            self.tmp.put(sgw)
            v3 = lambda ap: ap.rearrange("p (c t) -> p c t", t=64)
            self.tt("dve", self.YY, YY5[:, hp, :, 0, :], kh, v3(kh[:]), eexc, v3(eexc[:]), ALU.mult)
            self.tt("pool", self.YY, YY5[:, hp, :, 1, :], rr, v3(rr[:, hp, :]), einc, v3(einc[:]), ALU.mult)
            self.tt("dve", KT, KT[:, hp, :], kt, kt[:], eneg, eneg[:], ALU.mult)
            self.tt("pool", BD, BD[:, hp, :], bb, bb[:], eneg, eneg[:], ALU.mult)
            self.cp("act", self.PC, self.PC[:, hp, :], einc, v3(einc[:])[:, :, 63])
            rk = self.btmp.get()
            self.stt("dve", rk, rk[:], rr, rr[:, hp, :], sc(O_RK), kt, kt[:], ALU.mult, ALU.mult, extra=[sm])
            pb = self.psum.get()
            self.mm(pb, pb[:], self.cbf, self.cstb(C_B64ONES), rk, rk[:])
            self.btmp.put(rk)
            self.tt("dve", bon, bon[:, hp, :], pb, pb[:], vv, vv[:, hp, :], ALU.mult)
            self.psum.put(pb)
            self.tmp.put(kh, kt, bb, eexc, einc, eneg)

        yraw = [self.tmp.get() for _ in range(4)]
        hsl = lambda h: slice(h * 64, (h + 1) * 64)
        KTtm, BDtm, Vtm, Vpad = self.KTtm[0], self.BDtm[0], self.Vtm[0], self.Vpad[0]
        Vpad4 = Vpad[:].rearrange("p (hp hf v) -> p hp hf v", hp=4, hf=2)
        nUpad4 = self.nUpad[:].rearrange("p (hp hf v) -> p hp hf v", hp=4, hf=2)
        mst, mit, msx, i64 = (self.consts[:, o:o + 512] for o in (C_MST, C_MIT, C_MSX, C_I64))
        for cpair in range(4):
            chunks = (2 * cpair, 2 * cpair + 1)
            pqs = (slice(0, 64), slice(64, 128))
            csl = lambda c: slice(c * 64, (c + 1) * 64)
            for srct, dstt in ((KT, KTtm), (BD, BDtm), (vv, Vtm)):
                pT = self.psum.get()
                for q, c in enumerate(chunks):
                    for hp in range(4):
                        self.mm(pT, pT[pqs[q], hp * 128:(hp + 1) * 128], srct, srct[:, hp, csl(c)], self.cbf, idb)
                self.cp("act", dstt, dstt[:], pT, pT[:])
                if srct is vv:
                    pT4 = pT[:].rearrange("p (hp hf v) -> p hp hf v", hp=4, hf=2)
                    for hf in range(2):
                        self.cp("dve", Vpad, Vpad4[:, :, hf, hf * 64:(hf + 1) * 64], pT, pT4[:, :, hf, :])
                self.psum.put(pT)

            def abmat(lt, l_fn, rt, r_fn):
                p_ = self.psum.get()
                for hf in range(2):
                    prr = slice(hf * 64, hf * 64 + 64)
                    for q, c in enumerate(chunks):
                        for hp in range(4):
                            h = 2 * hp + hf
                            self.mm(p_, p_[pqs[q], hsl(h)], lt, l_fn(prr, hp, c), rt, r_fn(prr, hp, c), rowgrp=hf)
                return p_
            kt_fn = lambda prr, hp, c: KT[prr, hp, csl(c)]
            bd_fn = lambda prr, hp, c: BD[prr, hp, csl(c)]
            khd_fn = lambda prr, hp, c: YY5[prr, hp, c, 0, :]
            rd_fn = lambda prr, hp, c: YY5[prr, hp, c, 1, :]
            def abmat2(lt, l_fn):
                banks = (self.psum.get(), self.psum.get())
                for hf in range(2):
                    prr = slice(hf * 64, hf * 64 + 64)
                    for q, c in enumerate(chunks):
                        for hp in range(4):
                            h = 2 * hp + hf
                            p_ = banks[h // 4]
                            j = h % 4
                            self.mm(p_, p_[pqs[q], j * 128:(j + 1) * 128], lt, l_fn(prr, hp, c),
                                    self.YY, YY5[prr, hp, c, :, :].rearrange("p a t -> p (a t)"), rowgrp=hf)
                return banks
            v4 = lambda ap: ap.rearrange("p (h t) -> p h t", h=4)
            half = lambda p_, two: p_[:].rearrange("p (h two t) -> p h two t", h=4, two=2)[:, :, two, :]
            banks = abmat2(KT, kt_fn)
            for bi, p_ in enumerate(banks):
                cs4 = slice(bi * 256, (bi + 1) * 256)
                self.tt("dve", self.AktT, v4(self.AktT[:, cs4]), p_, half(p_, 0), self.consts, v4(mst[:, 0:256]), ALU.mult)
                self.tt("dve", self.BktT, v4(self.BktT[:, cs4]), p_, half(p_, 1), self.consts, v4(mit[:, 0:256]), ALU.mult)
                self.psum.put(p_)
            Zc, Xc = self.Zp[0], self.Xp[0]
            banks = abmat2(BD, bd_fn)
            for bi, p_ in enumerate(banks):
                cs4 = slice(bi * 256, (bi + 1) * 256)
                self.tt("dve", self.BbT, v4(self.BbT[:, cs4]), p_, half(p_, 1), self.consts, v4(mit[:, 0:256]), ALU.mult)
                self.stt("dve", Zc, v4(Zc[:, cs4]), p_, half(p_, 0), -1.0, self.consts, v4(mst[:, 0:256]), ALU.mult, ALU.mult)
                self.psum.put(p_)
            p_ = abmat(self.YY, khd_fn, BD, bd_fn)
            self.stt("dve", Xc, Xc[:], p_, p_[:], -1.0, self.consts, msx, ALU.mult, ALU.mult)
            self.psum.put(p_)
            RT32 = self.RT32
            RTb = self.RTb[0]
            self.tt("dve", RT32, RT32[:], Zc, Zc[:], self.consts, i64, ALU.add)
            self.cp("act", RTb, RTb[:], RT32, RT32[:])

            def sq(p_, lt, rt):
                for q in range(2):
                    for h in range(8):
                        self.mm(p_, p_[pqs[q], hsl(h)], lt, lt[pqs[q], hsl(h)], rt, rt[pqs[q], hsl(h)], rowgrp=q)
            for i in range(1, 6):
                lastit = (i == 5)
                Zn, Xn = self.Zp[i % 2], self.Xp[i % 2]
                px = self.psum.get()
                sq(px, Zc, Xc)
                if not lastit:
                    pz = self.psum.get()
                    sq(pz, Xc, Zc)
                self.cp("act", Xn, Xn[:], px, px[:])
                self.psum.put(px)
                if not lastit:
                    self.cp("dve", Zn, Zn[:], pz, pz[:])
                    self.psum.put(pz)
                prt = self.psum.get()
                sq(prt, Xn, RTb)
                self.tt("dve", RT32, RT32[:], RT32, RT32[:], prt, prt[:], ALU.add)
                self.psum.put(prt)
                self.cp("act", RTb, RTb[:], RT32, RT32[:])
                Zc, Xc = Zn, Xn
            pY = self.psum.get()
            for q, c in enumerate(chunks):
                pq = pqs[q]
                pR = self.psum.get()
                for hp in range(4):
                    self.mm(pR, pR[pq, hp * 128:(hp + 1) * 128], self.YY, YY5[:, hp, c, 0, :], Sb, Sb[:, hp, :], True, False)
                    for hf in range(2):
                        h = 2 * hp + hf
                        self.mm(pR, pR[pq, hsl(h)], self.AktT, self.AktT[pq, hsl(h)], Vtm, Vtm[pq, hsl(h)], False, hf == 1, rowgrp=q)
                self.cp("act", self.RHSb, self.RHSb[pq, :], pR, pR[pq, :])
                self.psum.put(pR)
                pU = self.psum.get()
                for h in range(8):
                    self.mm(pU, pU[pq, hsl(h)], RTb, RTb[pq, hsl(h)], self.RHSb, self.RHSb[pq, hsl(h)], rowgrp=q)
                self.act(self.nU, self.nU[pq, :], pU, pU[pq, :], AF.Copy, scale=-1.0)
                pU4 = pU[pq, :].rearrange("p (hp hf v) -> p hp hf v", hp=4, hf=2)
                for hf in range(2):
                    self.ts("dve", self.nUpad, nUpad4[pq, :, hf, hf * 64:(hf + 1) * 64], pU, pU4[:, :, hf, :], -1.0)
                self.psum.put(pU)
                for hp in range(4):
                    o = pY[:, q * 256 + hp * 64:q * 256 + (hp + 1) * 64]
                    self.mm(pY, o, Sb, Sb[:, hp, :], self.YY, YY5[:, hp, c, 1, :], True, False)
                    for hf in range(2):
                        h = 2 * hp + hf
                        self.mm(pY, o, Vpad, Vpad4[pq, hp, hf, :], self.BktT, self.BktT[pq, hsl(h)], False, False, rowgrp=q)
                        self.mm(pY, o, self.nUpad, nUpad4[pq, hp, hf, :], self.BbT, self.BbT[pq, hsl(h)], False, hf == 1, rowgrp=q)
                pS = self.psum.get()
                for hp in range(4):
                    hc = slice(hp * 128, (hp + 1) * 128)
                    self.mm(pS, pS[:, hc], KTtm, KTtm[pq, hc], Vtm, Vtm[pq, hc], True, False, rowgrp=q)
                    self.mm(pS, pS[:, hc], BDtm, BDtm[pq, hc], self.nU, self.nU[pq, hc], False, True, rowgrp=q)
                for hp in range(4):
                    hc = slice(hp * 128, (hp + 1) * 128)
                    t2 = self.stmp[hp % 2]
                    pc = self.PC[:, hp, c:c + 1]
                    self.stt("dve", t2, t2[:], pS, pS[:, hc], pc, self.consts, self.cst(C_B64ONES), ALU.mult, ALU.mult, extra=[self.PC])
                    self.stt("dve", S32, S32[:, hp, :], S32, S32[:, hp, :], pc, t2, t2[:], ALU.mult, ALU.add, extra=[self.PC])
                    self.cp("act", Sb, Sb[:, hp, :], S32, S32[:, hp, :])
                self.psum.put(pS)
            pY4 = pY[:].rearrange("p (q hp t) -> p q hp t", q=2, hp=4)
            for hp in range(4):
                self.cp("act", yraw[hp], yraw[hp][:, cpair * 128:(cpair + 1) * 128].rearrange("p (q t) -> p q t", q=2), pY, pY4[:, :, hp, :])
            self.psum.put(pY)
        for hp in range(4):
            o = yraw[hp]
            t1 = self.tmp.get()
            self.head_norm(o, t1, C_B64MEAN, 64e-5)
            self.stt("dve", o, o[:], o, o[:], sm[:, O_WGN + hp:O_WGN + hp + 1], bon, bon[:, hp, :], ALU.mult, ALU.add, extra=[sm])
            self.tt("dve", self.yrw, self.yrw[:, hp, :], o, o[:], g_rw, g_rw[:, hp, :], ALU.mult)
            self.tmp.put(t1)
        self.tmp.put(*yraw)

    def merge_stage(self, l, b, src_v, srcb):
        S = self.S
        sm = self.small[l]
        der = self.der[l]
        t0 = b * TB
        tsl = slice(t0, t0 + TB)
        macc = [self.rr, self.kk]
        merged = self.YY[:].rearrange("p (k t) -> p k t", k=8)
        wg_v = self.w_gate[l].rearrange("(k p) c -> p k c", p=128)
        ys = (self.yrt, self.yrw, self.ycv)
        for i in range(3):
            wb_v = self.w_branch[l, i].rearrange("(k p) c -> p k c", p=128)
            for half in range(2):
                wg = self.wnext()
                wb = self.wnext()
                for dcl in range(4):
                    cols = slice(dcl * 128, (dcl + 1) * 128)
                    pA, pB = self.psum.get(), self.psum.get()
                    for k in range(4):
                        self.mm(pA, pA[:], wb, wb[:, k, cols], ys[i], ys[i][:, k, :], k == 0, k == 3)
                    self.proj(pB, wg, cols)
                    sg = self.tmp.get()
                    dc = half * 4 + dcl
                    self.act(sg, sg[:], pB, pB[:], AF.Sigmoid, bias=sm[:, O_BGATE + i * 8 + dc:O_BGATE + i * 8 + dc + 1], scale=1.0, extra=[sm])
                    self.psum.put(pB)
                    m = macc[half]
                    if i == 0:
                        self.tt("dve", m, m[:, dcl, :], pA, pA[:], sg, sg[:], ALU.mult)
                    else:
                        self.tt("dve", sg, sg[:], pA, pA[:], sg, sg[:], ALU.mult)
                        if i < 2:
                            self.tt("pool", m, m[:, dcl, :], m, m[:, dcl, :], sg, sg[:], ALU.add)
                        else:
                            self.tt("pool", self.YY, merged[:, dc, :], m, m[:, dcl, :], sg, sg[:], ALU.add)
                    self.psum.put(pA)
                    self.tmp.put(sg)
                self.wput(wg, wb)
        wo_v = self.w_o[l].rearrange("(k p) c -> p k c", p=128)
        dst_v = self.xmid.rearrange("(k p) t -> p k t", p=128)
        for half in range(2):
            wo = self.wnext()
            for dcl in range(4):
                dc = half * 4 + dcl
                p_ = self.psum.get()
                for k in range(8):
                    self.mm(p_, p_[:], wo, wo[:, k, dcl * 128:(dcl + 1) * 128], self.YY, merged[:, k, :], k == 0, k == 7)
                xt = self.tmp.get()
                S.dma("sp", xt[:], src_v[:, dc, tsl], reads=([srcb[b]] if srcb else []), writes=[xt])
                self.stt("dve", xt, xt[:], p_, p_[:], self.gate1(l, dc), xt, xt[:], ALU.mult, ALU.add, extra=[der])
                self.psum.put(p_)
                S.dma("sp", dst_v[:, dc, tsl], xt[:], reads=[xt], writes=[self.db["xmid"][b]])
                if self.dbg:
                    S.dma("sp", self.dbg_out["d_xmid"][l].rearrange("(k p) t -> p k t", p=128)[:, dc, tsl], xt[:], reads=[xt])
                self.tmp.put(xt)
            self.wput(wo)

    def moe_alloc(self, st):
        sb = lambda n, s, d: self.sb(st, n, s, d)
        self.tmp = Pool_([sb(f"mtmp{i}", [128, 512], F32) for i in range(14)])
        self.h2T = sb("h2T", [128, 8, 1024], BF16)
        self.btmp = Pool_([sb(f"dbtmp{i}", [128, 512], BF16) for i in range(2)])
        self.acc = sb("acc", [128, 8, 1024], F32)
        self.Wt = sb("Wt", [128, 8, 32], F32)
        self.wr32 = sb("wr32", [128, 8, 36], F32)
        self.br = sb("br", [128, 36], F32)
        self.lg = sb("lg", [128, 36], F32)
        self.rt = sb("rt", [128, 64], F32)
        self.wge = [sb(f"wge{i}", [128, 8, 512], BF16) for i in range(2)]
        self.wue = [sb(f"wue{i}", [128, 8, 512], BF16) for i in range(2)]
        self.wde = [sb(f"wde{i}", [128, 4, 1024], BF16) for i in range(2)]
        self.actT = [sb(f"actT{i}", [128, 4, 512], BF16) for i in range(2)]
        self.moe_l = None

    def route_tile(self, l, ti):
        lg, rt, Wt = self.lg, self.rt, self.Wt
        S = self.S
        c = lambda i, n=1: rt[:, i:i + n]
        gmax, ngmax, gsum, pg, m1, m2, d_, e_, w1, w2 = (c(i) for i in range(10))
        goh, ge, sel, oh1, oh2, sel2, we = c(16, 4), c(20, 4), c(24, 8), c(32, 8), c(40, 8), c(48, 8), c(56, 8)
        R = lambda out_ap, in_ap: S.op("dve", lambda e: e.tensor_reduce(out=out_ap, in_=in_ap, axis=AX.X, op=ALU.max), [lg, rt], [rt])
        R(gmax, lg[:, 0:4])
        self.ts("dve", rt, goh, lg, lg[:, 0:4], gmax, None, ALU.is_equal, extra=[rt])
        self.ts("dve", rt, ngmax, rt, gmax, -1.0)
        self.act(rt, ge, lg, lg[:, 0:4], AF.Exp, bias=ngmax, scale=1.0, extra=[rt])
        S.op("dve", lambda e: e.tensor_reduce(out=gsum, in_=ge, axis=AX.X, op=ALU.add), [rt], [rt])
        self.recip(rt, pg, rt, gsum)
        el = lambda g: lg[:, 4 + g * 8:12 + g * 8]
        self.ts("dve", rt, sel, lg, el(0), goh[:, 0:1], extra=[rt])
        for g in range(1, 4):
            self.stt("dve", rt, sel, lg, el(g), goh[:, g:g + 1], rt, sel, ALU.mult, ALU.add)
        R(m1, sel)
        self.ts("dve", rt, oh1, rt, sel, m1, None, ALU.is_equal)
        self.stt("dve", rt, sel2, rt, oh1, -1e30, rt, sel, ALU.mult, ALU.add)
        R(m2, sel2)
        self.ts("dve", rt, oh2, rt, sel2, m2, None, ALU.is_equal)
        self.tt("dve", rt, d_, rt, m2, rt, m1, ALU.subtract)
        self.act(rt, e_, rt, d_, AF.Exp)
        self.ts("dve", rt, w1, rt, e_, 1.0, None, ALU.add)
        self.recip(rt, w1, rt, w1)
        self.tt("dve", rt, w2, rt, e_, rt, w1, ALU.mult)
        self.tt("dve", rt, w1, rt, w1, rt, pg, ALU.mult)
        self.tt("dve", rt, w2, rt, w2, rt, pg, ALU.mult)
        self.ts("dve", rt, we, rt, oh1, w1)
        self.stt("dve", rt, we, rt, oh2, w2, rt, we, ALU.mult, ALU.add)
        for g in range(4):
            self.ts("dve", Wt, Wt[:, ti, g * 8:(g + 1) * 8], rt, we, goh[:, g:g + 1])

    def moe_super(self, l, sbk, dst, dstb, last):
        S = self.S
        sm = self.small[l]
        der = self.der[l]
        xm_v = self.xmid.rearrange("(k p) t -> p k t", p=128)
        dst_v = dst.rearrange("(k p) t -> p k t", p=128)
        if self.moe_l != l:
            self.moe_l = l
            S.dma("sp", self.wr32[:], self.w_r[l].rearrange("(k p) c -> p k c", p=128), writes=[self.wr32])
            S.dma("sp", self.br[:], self.b_r[l], writes=[self.br])
        self.memset(self.acc, self.acc[:], 0.0)
        if "moe" in self.stages:
            for pc in range(2):
                b = sbk * 2 + pc
                tsl = slice(b * TB, (b + 1) * TB)
                xs = [self.tmp.get() for _ in range(8)]
                for k in range(8):
                    S.dma("sp", xs[k][:], xm_v[:, k, tsl], reads=[self.db["xmid"][b]], writes=[xs[k]])
                self.norm_block(xs, lambda k: self.G2(l, k), lambda k: self.shift2(l, k), lambda k: (xs[k], xs[k][:]), der)
                for k in range(8):
                    self.cp("pool", self.h2T, self.h2T[:, k, pc * 512:(pc + 1) * 512], xs[k], xs[k][:])
                for tl in range(4):
                    ti = pc * 4 + tl
                    pl = self.psum.get()
                    for k in range(8):
                        self.mm(pl, pl[:, 0:36], xs[k], xs[k][:, tl * 128:(tl + 1) * 128], self.wr32, self.wr32[:, k, :], k == 0, k == 7)
                    self.tt("dve", self.lg, self.lg[:], pl, pl[:, 0:36], self.br, self.br[:], ALU.add)
                    self.psum.put(pl)
                    self.route_tile(l, ti)
                self.tmp.put(*xs)
            for e_i in range(NE):
                wg, wu, wd, = self.wge[e_i % 2], self.wue[e_i % 2], self.wde[e_i % 2]
                self.wload(wg, self.w_eg[l, e_i].rearrange("(k p) c -> p k c", p=128))
                self.wload(wu, self.w_eu[l, e_i].rearrange("(k p) c -> p k c", p=128))
                self.wload(wd, self.w_ed[l, e_i].rearrange("(k p) c -> p k c", p=128))
                for pc in range(2):
                    aT = self.actT[pc]
                    for fc in range(4):
                        cols = slice(fc * 128, (fc + 1) * 128)
                        pG, pU = self.psum.get(), self.psum.get()
                        for k in range(8):
                            self.mm(pG, pG[:], wg, wg[:, k, cols], self.h2T, self.h2T[:, k, pc * 512:(pc + 1) * 512], k == 0, k == 7)
                        for k in range(8):
                            self.mm(pU, pU[:], wu, wu[:, k, cols], self.h2T, self.h2T[:, k, pc * 512:(pc + 1) * 512], k == 0, k == 7)
                        sg = self.tmp.get()
                        self.act(sg, sg[:], pG, pG[:], AF.Silu)
                        self.psum.put(pG)
                        self.tt("dve", aT, aT[:, fc, :], pU, pU[:], sg, sg[:], ALU.mult)
                        self.psum.put(pU)
                        self.tmp.put(sg)
                    for tl in range(4):
                        ti = pc * 4 + tl
                        for dh in range(2):
                            pD = self.psum.get()
                            for fc in range(4):
                                self.mm(pD, pD[:], aT, aT[:, fc, tl * 128:(tl + 1) * 128], wd, wd[:, fc, dh * 512:(dh + 1) * 512], fc == 0, fc == 3)
                            a_ap = self.acc[:, ti, dh * 512:(dh + 1) * 512]
                            self.stt("dve", self.acc, a_ap, pD, pD[:], self.Wt[:, ti, e_i:e_i + 1], self.acc, a_ap, ALU.mult, ALU.add, extra=[self.Wt])
                            self.psum.put(pD)
        for pc in range(2):
            b = sbk * 2 + pc
            tsl = slice(b * TB, (b + 1) * TB)
            xs = []
            for dc in range(8):
                pT = self.psum.get()
                for tl in range(4):
                    ti = pc * 4 + tl
                    self.mm(pT, pT[:, tl * 128:(tl + 1) * 128], self.acc, self.acc[:, ti, dc * 128:(dc + 1) * 128], self.consts, self.cst(C_ID))
                xt = self.tmp.get()
                S.dma("sp", xt[:], xm_v[:, dc, tsl], reads=[self.db["xmid"][b]], writes=[xt])
                self.stt("dve", xt, xt[:], pT, pT[:], self.gate2(l, dc), xt, xt[:], ALU.mult, ALU.add, extra=[der])
                self.psum.put(pT)
                if self.dbg:
                    S.dma("sp", self.dbg_out["d_xl"][l].rearrange("(k p) t -> p k t", p=128)[:, dc, tsl], xt[:], reads=[xt])
                if last:
                    xs.append(xt)
                else:
                    S.dma("sp", dst_v[:, dc, tsl], xt[:], reads=[xt], writes=[dstb[b]])
                    self.tmp.put(xt)
            if last:
                fn = lambda k: sm[:, O_FN + k:O_FN + k + 1]
                self.norm_block(xs, fn, None, lambda k: (xs[k], xs[k][:]), sm)
                for dc in range(8):
                    S.dma("sp", dst_v[:, dc, tsl], xs[dc][:], reads=[xs[dc]], writes=[dstb[b]])
                self.tmp.put(*xs)


    def moe_alloc_sparse(self, st):
        sb = lambda n, s, d: self.sb(st, n, s, d)
        NT = self.T // 128
        self.tmp = Pool_([sb(f"stmp{i}", [128, 512], F32) for i in range(10)])
        self.h2T = sb("h2Ts", [128, 8, 512], BF16)
        self.btmp = Pool_([sb(f"sbtmp{i}", [128, 512], BF16) for i in range(2)])
        self.h2tm = [sb("h2tm0", [128, 1024], BF16)] * 2
        self.wr32 = sb("wr32s", [128, 8, 36], F32)
        self.br = sb("brs", [128, 36], F32)
        self.lg = sb("lgs", [128, 4, 36], F32)
        self.rt = sb("rts", [128, 4, 64], F32)
        self.ohg = sb("ohg", [128, 4, 64], F32)
        self.ohs = sb("ohs", [128, 4, 32], BF16)
        self.rk = sb("rk", [128, 4, 96], F32)
        self.mcarry = sb("mcarry", [128, 32], F32)
        self.E = [sb(f"E{j}", [128, NT], F32) for j in range(2)]
        self.R = [sb(f"R{j}", [128, NT], F32) for j in range(2)]
        self.W = [sb(f"W{j}", [128, NT], F32) for j in range(2)]
        self.Df = [sb(f"Df{j}", [128, NT], F32) for j in range(2)]
        self.Di = [sb(f"Di{j}", [128, NT], I32) for j in range(2)]
        self.tidf = sb("tidf", [128, NT], F32)
        self.tidi = sb("tidi", [128, NT], I32)
        self.cmp = sb("cmp", [128, 32 * 128], BF16)
        self.sm32 = sb("sm32", [128, 5, 32], F32)
        self.bef = sb("bef", [128, 128], F32)
        self.idxwf = sb("idxwf", [128, 128], F32)
        self.idxw = sb("idxw", [128, 128], I32)
        self.sent = sb("sent", [128, self.NRW // 128], I32)
        self.zrow = sb("zrow", [1, 1024], BF16)
        self.idxr = [sb(f"idxr{i}", [128, 1], I32) for i in range(4)]
        self.xg = [sb(f"xg{i}", [128, 1024], BF16) for i in range(3)]
        self.xgT = [sb(f"xgT{i}", [128, 8, BLK], BF16) for i in range(2)]
        self.wge = [sb(f"swge{i}", [128, 8, 512], BF16) for i in range(2)]
        self.wue = [sb(f"swue{i}", [128, 8, 512], BF16) for i in range(2)]
        self.wde = [sb(f"swde{i}", [128, 4, 1024], BF16) for i in range(2)]
        self.actT = [sb(f"sactT{i}", [128, 4, BLK], BF16) for i in range(2)]
        self.yb = [sb(f"yb{i}", [128, 1024], F32) for i in range(2)]
        self.mt = [sb(f"mt{i}", [128, 1024], F32) for i in range(4)]
        self.yg = [sb(f"yg{i}", [128, 1024], F32) for i in range(2)]
        assert self.NBLK <= 128

    def route_tile2(self, l, ti):
        lg, rt = self.lg, self.rt
        S = self.S
        c = lambda i, n=1: rt[:, i:i + n]
        gmax, ngmax, gsum, pg, m1, m2, d_, e_, w1, w2 = (c(i) for i in range(10))
        goh, ge, sel, oh1, oh2, sel2, scr = c(16, 4), c(20, 4), c(24, 8), c(32, 8), c(40, 8), c(48, 8), c(56, 8)
        R = lambda out_ap, in_ap: S.op("dve", lambda e: e.tensor_reduce(out=out_ap, in_=in_ap, axis=AX.X, op=ALU.max), [lg, rt], [rt])
        R(gmax, lg[:, 0:4])
        self.ts("dve", rt, goh, lg, lg[:, 0:4], gmax, None, ALU.is_equal, extra=[rt])
        self.ts("dve", rt, ngmax, rt, gmax, -1.0)
        self.act(rt, ge, lg, lg[:, 0:4], AF.Exp, bias=ngmax, scale=1.0, extra=[rt])
        S.op("dve", lambda e: e.tensor_reduce(out=gsum, in_=ge, axis=AX.X, op=ALU.add), [rt], [rt])
        self.recip(rt, pg, rt, gsum)
        el = lambda g: lg[:, 4 + g * 8:12 + g * 8]
        self.ts("dve", rt, sel, lg, el(0), goh[:, 0:1], extra=[rt])
        for g in range(1, 4):
            self.stt("dve", rt, sel, lg, el(g), goh[:, g:g + 1], rt, sel, ALU.mult, ALU.add)
        R(m1, sel)
        self.ts("dve", rt, oh1, rt, sel, m1, None, ALU.is_equal)
        self.stt("dve", rt, sel2, rt, oh1, -1e30, rt, sel, ALU.mult, ALU.add)
        R(m2, sel2)
        self.ts("dve", rt, oh2, rt, sel2, m2, None, ALU.is_equal)
        self.tt("dve", rt, d_, rt, m2, rt, m1, ALU.subtract)
        self.act(rt, e_, rt, d_, AF.Exp)
        self.ts("dve", rt, w1, rt, e_, 1.0, None, ALU.add)
        self.recip(rt, w1, rt, w1)
        self.tt("dve", rt, w2, rt, e_, rt, w1, ALU.mult)
        self.tt("dve", self.W[0], self.W[0][:, ti:ti + 1], rt, w1, rt, pg, ALU.mult)
        self.tt("dve", self.W[1], self.W[1][:, ti:ti + 1], rt, w2, rt, pg, ALU.mult)
        ohg, rk = self.ohg, self.rk
        for j, oh in enumerate((oh1, oh2)):
            for g in range(4):
                self.ts("dve", ohg, ohg[:, j * 32 + g * 8:j * 32 + (g + 1) * 8], rt, oh, goh[:, g:g + 1])
        self.tt("dve", self.ohs, self.ohs[:], ohg, ohg[:, 0:32], ohg, ohg[:, 32:64], ALU.add)
        pr = self.psum.get()
        self.mm(pr, pr[:, 0:32], self.cbf, self.cbf[:, 256:384], self.ohs, self.ohs[:])
        self.mm(pr, pr[:, 32:64], self.cbf, self.cbf[:, 384:512], self.ohs, self.ohs[:])
        self.tt("dve", rk, rk[:, 0:32], pr, pr[:, 0:32], self.mcarry, self.mcarry[:], ALU.add)
        self.tt("dve", self.mcarry, self.mcarry[:], self.mcarry, self.mcarry[:], pr, pr[:, 32:64], ALU.add)
        self.psum.put(pr)
        iota = self.cst(C_IOTA32, 32)
        for j in range(2):
            oj = ohg[:, j * 32:(j + 1) * 32]
            self.tt("dve", rk, rk[:, 32:64], ohg, oj, self.consts, iota, ALU.mult)
            S.op("dve", lambda e: e.tensor_reduce(out=self.E[j][:, ti:ti + 1], in_=rk[:, 32:64], axis=AX.X, op=ALU.add), [rk], [self.E[j]])
            self.tt("dve", rk, rk[:, 64:96], ohg, oj, rk, rk[:, 0:32], ALU.mult)
            S.op("dve", lambda e: e.tensor_reduce(out=self.R[j][:, ti:ti + 1], in_=rk[:, 64:96], axis=AX.X, op=ALU.add), [rk], [self.R[j]])

    def route_piece(self, l, pc):
        lg, rt, ohg, rk = self.lg, self.rt, self.ohg, self.rk
        S = self.S
        t0 = pc * 4
        c = lambda i, n=1: rt[:, :, i:i + n]
        bc = lambda ap, n: ap.broadcast_to([128, 4, n])
        gmax, gsum, pg, m1, m2, d_, e_, w1, w2 = (c(i) for i in range(9))
        goh, ge, sel, oh1, oh2, sel2, scr = c(16, 4), c(20, 4), c(24, 8), c(32, 8), c(40, 8), c(48, 8), c(56, 8)
        red = lambda out_ap, in_ap, op, rd, wr: self.reduce(wr[0], out_ap, rd[0], in_ap, op)
        gl = lg[:, :, 0:4]
        red(gmax, gl, ALU.max, [lg], [rt])
        self.tt("dve", rt, goh, lg, gl, rt, bc(gmax, 4), ALU.is_equal)
        self.tt("dve", rt, ge, lg, gl, rt, bc(gmax, 4), ALU.subtract)
        self.act(rt, ge, rt, ge, AF.Exp)
        red(gsum, ge, ALU.add, [rt], [rt])
        self.recip(rt, pg, rt, gsum)
        el = lambda g: lg[:, :, 4 + g * 8:12 + g * 8]
        self.tt("dve", rt, sel, lg, el(0), rt, bc(goh[:, :, 0:1], 8), ALU.mult)
        for g in range(1, 4):
            self.tt("dve", rt, scr, lg, el(g), rt, bc(goh[:, :, g:g + 1], 8), ALU.mult)
            self.tt("dve", rt, sel, rt, sel, rt, scr, ALU.add)
        red(m1, sel, ALU.max, [rt], [rt])
        self.tt("dve", rt, oh1, rt, sel, rt, bc(m1, 8), ALU.is_equal)
        self.stt("dve", rt, sel2, rt, oh1, -1e30, rt, sel, ALU.mult, ALU.add)
        red(m2, sel2, ALU.max, [rt], [rt])
        self.tt("dve", rt, oh2, rt, sel2, rt, bc(m2, 8), ALU.is_equal)
        self.tt("dve", rt, d_, rt, m2, rt, m1, ALU.subtract)
        self.act(rt, e_, rt, d_, AF.Exp)
        self.ts("dve", rt, w1, rt, e_, 1.0, None, ALU.add)
        self.recip(rt, w1, rt, w1)
        self.tt("dve", rt, w2, rt, e_, rt, w1, ALU.mult)
        wv = lambda j: self.W[j][:, t0:t0 + 4].rearrange("p (t o) -> p t o", o=1)
        self.tt("dve", self.W[0], wv(0), rt, w1, rt, pg, ALU.mult)
        self.tt("dve", self.W[1], wv(1), rt, w2, rt, pg, ALU.mult)
        for j, oh in enumerate((oh1, oh2)):
            for g in range(4):
                self.tt("dve", ohg, ohg[:, :, j * 32 + g * 8:j * 32 + (g + 1) * 8], rt, oh, rt, bc(goh[:, :, g:g + 1], 8), ALU.mult)
        self.tt("dve", self.ohs, self.ohs[:], ohg, ohg[:, :, 0:32], ohg, ohg[:, :, 32:64], ALU.add)
        pr = self.psum.get()
        for t in range(4):
            self.mm(pr, pr[:, t * 32:(t + 1) * 32], self.cbf, self.cbf[:, 256:384], self.ohs, self.ohs[:, t, :])
        for t in range(4):
            self.mm(pr, pr[:, 128 + t * 32:128 + (t + 1) * 32], self.cbf, self.cbf[:, 384:512], self.ohs, self.ohs[:, t, :])
        for t in range(4):
            self.tt("dve", rk, rk[:, t, 0:32], pr, pr[:, t * 32:(t + 1) * 32], self.mcarry, self.mcarry[:], ALU.add)
            self.tt("dve", self.mcarry, self.mcarry[:], self.mcarry, self.mcarry[:], pr, pr[:, 128 + t * 32:128 + (t + 1) * 32], ALU.add)
        self.psum.put(pr)
        iota_bc = self.cst(C_IOTA32, 32).rearrange("p (o e) -> p o e", o=1).broadcast_to([128, 4, 32])
        ev = lambda T_, j: T_[j][:, t0:t0 + 4]
        for j in range(2):
            oj = ohg[:, :, j * 32:(j + 1) * 32]
            self.tt("dve", rk, rk[:, :, 32:64], ohg, oj, self.consts, iota_bc, ALU.mult)
            red(ev(self.E, j), rk[:, :, 32:64], ALU.add, [rk], [self.E[j]])
            self.tt("dve", rk, rk[:, :, 64:96], ohg, oj, rk, rk[:, :, 0:32], ALU.mult)
            red(ev(self.R, j), rk[:, :, 64:96], ALU.add, [rk], [self.R[j]])

    def moe_sparse(self, l, dst, dstb, last):
        S = self.S
        T = self.T
        NT = T // 128
        NBLK, NRW = self.NBLK, self.NRW
        sm, der = self.small[l], self.der[l]
        xm_v = self.xmid.rearrange("(k p) t -> p k t", p=128)
        dst_v = dst.rearrange("(k p) t -> p k t", p=128)
        idb = self.cbf[:, 0:128]
        IOA = bass.IndirectOffsetOnAxis
        S.dma("sp", self.wr32[:], self.w_r[l].rearrange("(k p) c -> p k c", p=128), writes=[self.wr32])
        S.dma("sp", self.br[:], self.b_r[l], writes=[self.br])
        self.memset(self.mcarry, self.mcarry[:], 0.0)
        self.memset(self.zrow, self.zrow[:], 0.0)
        S.dma("sp", self.h2rows[T:T + 1, :], self.zrow[:], reads=[self.zrow], writes=[self.h2rows_b[NT]])
        real_tmp = list(self.tmp.free)
        views = [View(w, k, f"vw{i}_{k}") for i, w in enumerate(self.wge + self.wue + self.wde) for k in range(4)]
        assert tuple(views[0][:].shape) == (128, 512), views[0][:].shape
        self.tmp = Pool_(real_tmp + views)
        for pc in range(T // 512):
            tsl = slice(pc * TB, (pc + 1) * TB)
            xs = [self.tmp.get() for _ in range(8)]
            for k in range(8):
                S.dma("sp", xs[k][:], xm_v[:, k, tsl], reads=[self.db["xmid"][pc]], writes=[xs[k]])
            self.norm_block(xs, lambda k: self.G2(l, k), lambda k: self.shift2(l, k), lambda k: (xs[k], xs[k][:]), der)
            for k in range(8):
                self.cp("pool", self.h2T, self.h2T[:, k, :], xs[k], xs[k][:])
            pl = self.psum.get()
            for tl in range(4):
                for k in range(8):
                    self.mm(pl, pl[:, tl * 36:(tl + 1) * 36], xs[k], xs[k][:, tl * 128:(tl + 1) * 128], self.wr32, self.wr32[:, k, :], k == 0, k == 7)
            self.tt("dve", self.lg, self.lg[:], pl, pl[:, 0:144].rearrange("p (t c) -> p t c", t=4), self.br,
                    self.br[:].rearrange("p (o c) -> p o c", o=1).broadcast_to([128, 4, 36]), ALU.add)
            self.psum.put(pl)
            self.route_piece(l, pc)
            self.tmp.put(*xs)
            for tl in range(4):
                ti = pc * 4 + tl
                pA, pB = self.psum.get(), self.psum.get()
                for k in range(8):
                    p_ = pA if k < 4 else pB
                    self.mm(p_, p_[:, (k % 4) * 128:(k % 4 + 1) * 128], self.h2T, self.h2T[:, k, tl * 128:(tl + 1) * 128], self.cbf, idb)
                hm = self.h2tm[ti % 2]
                self.cp("act", hm, hm[:, 0:512], pA, pA[:])
                self.cp("dve", hm, hm[:, 512:1024], pB, pB[:])
                self.psum.put(pA, pB)
                S.dma("sp", self.h2rows[ti * 128:(ti + 1) * 128, :], hm[:], reads=[hm], writes=[self.h2rows_b[ti]])
        cnt = self.mcarry
        cmp, sm32 = self.cmp, self.sm32
        nblk, nblkB, pend, pstart, ones32 = (sm32[:, i, :] for i in range(5))
        mB = lambda n: self.cst(C_MB, n)
        for e_i in range(NE):
            self.ts("dve", cmp, cmp[:, e_i * 128:(e_i + 1) * 128], self.consts, mB(128), cnt[:, e_i:e_i + 1], None, ALU.is_lt, extra=[cnt])
        self.reduce(sm32, nblk, cmp, cmp[:, 0:32 * 128].rearrange("p (e m) -> p e m", m=128), ALU.add)
        self.ts("dve", sm32, nblkB, sm32, nblk, float(BLK))
        self.memset(sm32, ones32, 1.0)
        self.scan(sm32, pend, sm32, ones32, sm32, nblkB)
        self.tt("dve", sm32, pstart, sm32, pend, sm32, nblkB, ALU.subtract)
        for e_i in range(NE):
            self.ts("dve", cmp, cmp[:, e_i * NBLK:(e_i + 1) * NBLK], self.consts, mB(NBLK), pend[:, e_i:e_i + 1], None, ALU.is_ge, extra=[sm32])
        self.reduce(self.bef, self.bef[:, 0:NBLK], cmp, cmp[:, 0:32 * NBLK].rearrange("p (e b) -> p b e", b=NBLK), ALU.add)
        self.ts("dve", self.bef, self.bef[:, 0:NBLK], self.bef, self.bef[:, 0:NBLK], float(NE - 1), None, ALU.min)
        self.ts("dve", self.idxwf, self.idxwf[:, 0:NBLK], self.bef, self.bef[:, 0:NBLK], 128.0, self.cst(C_PID, 1), ALU.mult, ALU.add, extra=[self.consts])
        self.cp("dve", self.idxw, self.idxw[:, 0:NBLK], self.idxwf, self.idxwf[:, 0:NBLK])
        iota = self.cst(C_IOTA32, 32)
        CH = 16
        for j in range(2):
            for c0 in range(0, NT, CH):
                n = min(CH, NT - c0)
                oh = self.tmp.get()
                ohv = oh[:, 0:n * 32].rearrange("p (t e) -> p t e", e=32)
                self.tt("dve", oh, ohv, self.consts, iota.rearrange("p (o e) -> p o e", o=1).broadcast_to([128, n, 32]),
                        self.E[j], self.E[j][:, c0:c0 + n].rearrange("p (t o) -> p t o", o=1).broadcast_to([128, n, 32]), ALU.is_equal)
                self.tt("dve", oh, ohv, oh, ohv, sm32, pstart.rearrange("p (o e) -> p o e", o=1).broadcast_to([128, n, 32]), ALU.mult)
                self.reduce(self.Df[j], self.Df[j][:, c0:c0 + n], oh, ohv, ALU.add)
                self.tmp.put(oh)
            self.tt("dve", self.Df[j], self.Df[j][:], self.Df[j], self.Df[j][:], self.R[j], self.R[j][:], ALU.add)
            self.cp("dve", self.Di[j], self.Di[j][:], self.Df[j], self.Df[j][:])
        self.ts("dve", self.tidf, self.tidf[:], self.consts, self.cst(C_MB, NT), 128.0 / BLK, self.cst(C_PID, 1), ALU.mult, ALU.add)
        self.cp("dve", self.tidi, self.tidi[:], self.tidf, self.tidf[:])
        self.memset(self.sent, self.sent[:], int(T))
        S.dma("sp", self.rowtok.rearrange("(p n) o -> p (n o)", p=128), self.sent[:], reads=[self.sent], writes=[self.rowtok_fill_b])
        for j in range(2):
            for ti in range(NT):
                self.idma(self.rowtok[:, :], IOA(ap=self.Di[j][:, ti:ti + 1], axis=0), self.tidi[:, ti:ti + 1], None,
                          [self.Di[j], self.tidi, self.rowtok_fill_b], [self.scat_b[j * NT + ti]])
        S.barrier()
        self.tmp = Pool_(real_tmp)
        scat_all = [self.rowtok_fill_b] + self.scat_b[:2 * NT]
        for b in range(NBLK):
            wg, wu, wd = self.wge[b % 2], self.wue[b % 2], self.wde[b % 2]
            for which, w in enumerate((wg, wu, wd)):
                self.idma(w[:].rearrange("p k c -> p (k c)"), None, self.wexp[l][which][:, :], IOA(ap=self.idxw[:, b:b + 1], axis=0),
                          [self.idxw] + self.wexp_b[l][which * NE:(which + 1) * NE], [w])
            xgs = []
            for r in range(RT):
                rr_ = b * RT + r
                it = self.idxr[rr_ % 4]
                S.dma("sp", it[:], self.rowtok[rr_ * 128:(rr_ + 1) * 128, :], reads=scat_all, writes=[it])
                xg = self.xg[rr_ % 3]
                self.idma(xg[:], None, self.h2rows[:, :], IOA(ap=it[:, 0:1], axis=0), [it] + self.h2rows_b, [xg])
                xgs.append(xg)
            xgT = self.xgT[b % 2]
            for kq in range(4):
                p_ = self.psum.get()
                for kk in range(2):
                    k = kq * 2 + kk
                    for r in range(RT):
                        self.mm(p_, p_[:, kk * BLK + r * 128:kk * BLK + (r + 1) * 128], xgs[r], xgs[r][:, k * 128:(k + 1) * 128], self.cbf, idb)
                self.cp("act" if kq % 2 == 0 else "dve", xgT, xgT[:, kq * 2:kq * 2 + 2, :], p_, p_[:].rearrange("p (k n) -> p k n", k=2))
                self.psum.put(p_)
            aT = self.actT[b % 2]
            for fc in range(4):
                cols = slice(fc * 128, (fc + 1) * 128)
                pG, pU = self.psum.get(), self.psum.get()
                for k in range(8):
                    self.mm(pG, pG[:, 0:BLK], wg, wg[:, k, cols], xgT, xgT[:, k, :], k == 0, k == 7)
                for k in range(8):
                    self.mm(pU, pU[:, 0:BLK], wu, wu[:, k, cols], xgT, xgT[:, k, :], k == 0, k == 7)
                sg = self.tmp.get()
                self.act(sg, sg[:, 0:BLK], pG, pG[:, 0:BLK], AF.Silu)
                self.psum.put(pG)
                self.tt("dve", aT, aT[:, fc, :], pU, pU[:, 0:BLK], sg, sg[:, 0:BLK], ALU.mult)
                self.psum.put(pU)
                self.tmp.put(sg)
            for r in range(RT):
                rr_ = b * RT + r
                yb = self.yb[rr_ % 2]
                for dh in range(2):
                    pD = self.psum.get()
                    for fc in range(4):
                        self.mm(pD, pD[:], aT, aT[:, fc, r * 128:(r + 1) * 128], wd, wd[:, fc, dh * 512:(dh + 1) * 512], fc == 0, fc == 3)
                    self.cp("act" if dh == 0 else "dve", yb, yb[:, dh * 512:(dh + 1) * 512], pD, pD[:])
                    self.psum.put(pD)
                S.dma("sp", self.yrows[rr_ * 128:(rr_ + 1) * 128, :], yb[:], reads=[yb], writes=[self.yrows_b[rr_]])
        S.barrier()
        self.tmp = Pool_(real_tmp + [View(w, k, f"vx{i}_{k}") for i, w in enumerate(self.wge + self.wue + self.wde) for k in range(4)])
        mbs = [self.xg[0], self.xg[1], self.xg[2], self.h2tm[0]]
        for pc in range(T // 512):
            tsl = slice(pc * TB, (pc + 1) * TB)
            for tl in range(4):
                ti = pc * 4 + tl
                m = self.mt[tl]
                ys = []
                for j in range(2):
                    y = m if j == 0 else self.yg[ti % 2]
                    self.idma(y[:], None, self.yrows[:, :], IOA(ap=self.Di[j][:, ti:ti + 1], axis=0), [self.Di[j]] + self.yrows_b, [y])
                    ys.append(y)
                self.ts("dve", m, m[:], ys[0], ys[0][:], self.W[0][:, ti:ti + 1], extra=[self.W[0]])
                mb = mbs[tl]
                self.stt("dve", mb, mb[:], ys[1], ys[1][:], self.W[1][:, ti:ti + 1], m, m[:], ALU.mult, ALU.add, extra=[self.W[1]])
            xs = []
            for dc in range(8):
                pT = self.psum.get()
                for tl in range(4):
                    self.mm(pT, pT[:, tl * 128:(tl + 1) * 128], mbs[tl], mbs[tl][:, dc * 128:(dc + 1) * 128], self.cbf, idb)
                xt = self.tmp.get()
                S.dma("sp", xt[:], xm_v[:, dc, tsl], reads=[self.db["xmid"][pc]], writes=[xt])
                self.stt("dve", xt, xt[:], pT, pT[:], self.gate2(l, dc), xt, xt[:], ALU.mult, ALU.add, extra=[der])
                self.psum.put(pT)
                if self.dbg:
                    S.dma("sp", self.dbg_out["d_xl"][l].rearrange("(k p) t -> p k t", p=128)[:, dc, tsl], xt[:], reads=[xt])
                if last:
                    xs.append(xt)
                else:
                    S.dma("sp", dst_v[:, dc, tsl], xt[:], reads=[xt], writes=[dstb[pc]])
                    self.tmp.put(xt)
            if last:
                fn = lambda k: sm[:, O_FN + k:O_FN + k + 1]
                self.norm_block(xs, fn, None, lambda k: (xs[k], xs[k][:]), sm)
                for dc in range(8):
                    S.dma("sp", dst_v[:, dc, tsl], xs[dc][:], reads=[xs[dc]], writes=[dstb[pc]])
                self.tmp.put(*xs)


def make_consts():
    c = np.zeros((128, NCONST), np.float64)
    c[:, C_ID:C_ID + 128] = np.eye(128)
    c[:, C_MEAND:C_MEAND + 128] = 1.0 / 1024.0
    c[:, C_MEAN128:C_MEAN128 + 128] = 1.0 / 128.0
    blk = np.kron(np.eye(2), np.ones((64, 64)))
    c[:, C_B64MEAN:C_B64MEAN + 128] = blk / 64.0
    c[:, C_B64ONES:C_B64ONES + 128] = blk
    R = np.zeros((128, 128))
    for m in range(64):
        R[m, m + 64] = -1.0
        R[m + 64, m] = 1.0
    c[:, C_RMT:C_RMT + 128] = R.T
    j = np.arange(128)[:, None]
    i = np.arange(128)[None, :]
    for h in range(4):
        lg = math.log1p(-2.0 ** (-5 - h))
        c[:, C_DT + h * 128:C_DT + (h + 1) * 128] = np.where(i >= j, np.exp(lg * np.maximum(i - j, 0)), 0.0) / math.sqrt(128.0)
        c[:, C_XI + h * 128:C_XI + (h + 1) * 128] = np.exp(lg * (i + 1.0)) + 0 * j
        c[:, C_ZETA + h] = np.exp(lg * (127.0 - np.arange(128))) / math.sqrt(128.0)
    invf = (np.float32(10000.0) ** (-(np.arange(64, dtype=np.float32) / np.float32(64)))).astype(np.float64)
    c[:, C_INVF] = np.concatenate([invf, invf])
    s = np.arange(64)[:, None]
    t = np.arange(64)[None, :]
    for r0 in (0, 64):
        c[r0:r0 + 64, C_MST:C_MST + 512] = np.tile((s < t).astype(np.float64), (1, 8))
        c[r0:r0 + 64, C_MIT:C_MIT + 512] = np.tile((s <= t).astype(np.float64), (1, 8))
        c[r0:r0 + 64, C_MSX:C_MSX + 512] = np.tile((s > t).astype(np.float64), (1, 8))
        c[r0:r0 + 64, C_I64:C_I64 + 512] = np.tile(np.eye(64), (1, 8))
    seg = np.ones(512)
    seg[::64] = 0.0
    c[:, C_SEG:C_SEG + 512] = seg[None, :]
    c[:, C_IOTA32:C_IOTA32 + 32] = np.arange(32)[None, :]
    c[:, C_MB:C_MB + 256] = (np.arange(256) * BLK)[None, :]
    c[:, C_PID] = np.arange(128)
    tp = np.arange(128)[:, None]
    tt_ = np.arange(128)[None, :]
    c[:, C_LTRI:C_LTRI + 128] = (tp < tt_).astype(np.float64)
    c[:, C_ONES:C_ONES + 128] = 1.0
    return c.astype(np.float32)


def colpack(v):
    v = np.asarray(v, np.float32).reshape(-1, 128)
    return np.ascontiguousarray(v.T)


def prep_shared(inp):
    Lr = L_DEPTH
    small = np.zeros((Lr, 128, NS), np.float32)
    for l in range(Lr):
        parts = [inp["norm1_g"][l], inp["norm2_g"][l], inp["b_ada"][l], inp["b_gate"][l], inp["ret_gn_g"][l], inp["rwkv_mu"][l],
                 inp["rwkv_w0"][l], inp["rwkv_a0"][l], inp["rwkv_k_k"][l], inp["rwkv_k_a"][l], inp["rwkv_r_k"][l].reshape(-1),
                 inp["rwkv_gn_g"][l], inp["conv_w"][l][0], inp["conv_w"][l][1], inp["conv_w"][l][2], inp["final_norm_g"]]
        small[l] = np.concatenate([colpack(p) for p in parts], axis=1)
    w_r = np.ascontiguousarray(np.concatenate([inp["w_router_group"], inp["w_router_expert"]], axis=2), dtype=np.float32)
    b_r1 = np.concatenate([inp["b_router_group"], inp["b_router_expert"]], axis=1).astype(np.float32)
    b_r = np.ascontiguousarray(np.broadcast_to(b_r1[:, None, :], (Lr, 128, 36)))
    sh = dict(small=small, consts=make_consts(), w_r=w_r, b_r=b_r)
    for k in ("w_ada", "w_in", "w_gate", "rwkv_w_lora", "rwkv_a_lora", "rwkv_g_lora", "w_branch", "w_o",
              "w_exp_gate", "w_exp_up", "w_exp_down"):
        sh[k] = np.ascontiguousarray(inp[k], dtype=np.float32)
    return sh


def core_inputs(inp, sh, bidx, T):
    m = dict(sh)
    m["xT"] = np.ascontiguousarray(np.asarray(inp["x"][bidx, :T], np.float32).T)
    m["pos"] = np.ascontiguousarray(np.asarray(inp["positions"][bidx, :T], np.int32)[None, :])
    m["cT"] = colpack(inp["c"][bidx])
    return m


_NC_CACHE = {}


def kernel(**inputs):
    inp = {k: np.asarray(v) for k, v in inputs.items()}
    B, T, _ = inp["x"].shape
    if T not in _NC_CACHE:
        _NC_CACHE[T] = Builder(T).build()
    nc = _NC_CACHE[T]
    sh = prep_shared(inp)
    in_maps = [core_inputs(inp, sh, b, T) for b in range(B)]
    res = run_bass_kernel_spmd(nc, in_maps, core_ids=list(range(B)))
    out = np.stack([np.ascontiguousarray(res.results[b]["outT"].T) for b in range(B)], axis=0)
    return out.astype(np.float32)
```

```python
import contextlib
import math
import os
import numpy as np
import concourse.bass as bass
import concourse.mybir as mybir
from concourse.bass_utils import run_bass_kernel_spmd

ALU = mybir.AluOpType
AF = mybir.ActivationFunctionType
AX = mybir.AxisListType
F32 = mybir.dt.float32
BF16 = mybir.dt.bfloat16
I32 = mybir.dt.int32
EPOCH = 30000

D = 1024
MW = 512
TB = 512
L_DEPTH = 2
NE = 32
FF = 512
CDEC = math.exp(-0.5)
O_N1, O_N2, O_BADA, O_BGATE, O_RGN, O_MU, O_W0, O_A0, O_KK, O_KA, O_RK, O_WGN, O_CONVW, O_FN, NS = \
    0, 8, 16, 64, 88, 92, 106, 110, 114, 118, 122, 126, 130, 142, 150
C_ID, C_MEAND, C_MEAN128, C_B64MEAN, C_B64ONES, C_RMT, C_DT, C_XI, C_ZETA, C_INVF, C_MST, C_MIT, C_MSX, C_I64, C_SEG, C_IOTA32, C_MB, C_PID, C_LTRI, C_ONES, NCONST = \
    0, 128, 256, 384, 512, 640, 768, 1280, 1792, 1796, 1797, 2309, 2821, 3333, 3845, 4357, 4389, 4645, 4646, 4774, 4902
RT = 2
BLK = 128 * RT


class Buf:
    __slots__ = ("name", "w", "r", "excl", "rowgrp")

    def __init__(self, name="", excl=False):
        self.name = name
        self.w = None
        self.r = {}
        self.excl = excl
        self.rowgrp = None


class Tl:
    __slots__ = ("h", "b")

    def __init__(self, h, name="", excl=False):
        self.h = h
        self.b = Buf(name, excl)

    def __getitem__(self, idx):
        return self.h[idx]


class Sched:
    def __init__(self, nc, stack, n_dma=24, same_sync=True):
        self.nc = nc
        self.stack = stack
        self.E = {"pe": nc.tensor, "act": nc.scalar, "dve": nc.vector, "pool": nc.gpsimd, "sp": nc.sync}
        self.sem = {}
        self.cnt = {k: 0 for k in ("pe", "act", "dve", "pool")}
        self.ndma = n_dma
        for cls in ("dh", "ds"):
            for j in range(n_dma):
                self.sem[(cls, j)] = stack.enter_context(nc.semaphore(f"s_{cls}{j}"))
        self.dval = {(cls, j): 0 for cls in ("dh", "ds") for j in range(n_dma)}
        self.di = {"dh": 0, "ds": 0}
        self.seen = {e: {} for e in self.E}
        self.same_sync = same_sync
        self.nins = 0
        self.rec = []
        self.lazy_pe = os.environ.get("LAZY_PE", "0") == "1"
        self.pe_emitted = 0
        self.pe_last_ins = None
        self.pe_mark_idx = []
        self.pe_mark_kv = []
        self.pe_seen_idx = {}
        self.reorder = os.environ.get("NO_REORDER", "0") != "1"
        self.window = int(os.environ.get("SCHED_WINDOW", "6"))

    def _semfor(self, key):
        if key not in self.sem:
            self.sem[key] = self.stack.enter_context(self.nc.semaphore(f"s_{key[0]}{key[1]}"))
        return self.sem[key]

    def _resolve_pe(self, idx):
        import bisect
        j = bisect.bisect_left(self.pe_mark_idx, idx)
        if j < len(self.pe_mark_idx):
            return self.pe_mark_kv[j]
        c = len(self.pe_mark_idx)
        key, v = ("pe", c // EPOCH), c % EPOCH + 1
        self.pe_last_ins.then_inc(self._semfor(key), 1)
        self.pe_mark_idx.append(self.pe_emitted)
        self.pe_mark_kv.append((key, v))
        return key, v

    def _wait(self, e, key, val):
        if key == ("pe", "lazy"):
            if val <= 0 or self.pe_seen_idx.get(e, 0) >= val:
                return
            self.pe_seen_idx[e] = val
            key, val = self._resolve_pe(val)
        if val <= 0 or self.seen[e].get(key, 0) >= val:
            return
        self.E[e].wait_ge(self._semfor(key), val)
        self.seen[e][key] = val
        self.nins += 1

    def _deps(self, e, reads, writes):
        need = {}

        def add(k, v):
            if k[0] == e and (e == "pe" or not self.same_sync):
                return
            if need.get(k, 0) < v:
                need[k] = v

        for b in reads:
            if b.w is not None:
                add(*b.w)
            if b.excl:
                for k, v in b.r.items():
                    if k[0] != e:
                        add(k, v)
        for b in writes:
            if b.w is not None:
                add(*b.w)
            for k, v in b.r.items():
                add(k, v)
        for k, v in need.items():
            self._wait(e, k, v)

    @staticmethod
    def _stamp(st, reads, writes):
        k, v = st
        for b in reads:
            if b.r.get(k, 0) < v:
                b.r[k] = v
        for b in writes:
            b.w = st
            b.r = {}

    def op(self, e, fn, reads=(), writes=(), cost=200.0, rowgrp=-1):
        reads = [getattr(x, "b", x) for x in reads]
        writes = [getattr(x, "b", x) for x in writes]
        self.rec.append(("op", e, fn, reads, writes, float(cost), float(cost), rowgrp))

    def dma(self, q, out, in_, reads=(), writes=(), starter=None, nbytes=65536, **kw):
        reads = [getattr(x, "b", x) for x in reads]
        writes = [getattr(x, "b", x) for x in writes]
        if starter is None:
            starter = lambda E: E.dma_start(out=out, in_=in_, **kw)
        issue = 1200.0 if q == "pool" else 150.0
        lat = 2500.0 + nbytes / 150.0
        self.rec.append(("dma", q, starter, reads, writes, issue, lat, -1))

    def _emit_op(self, e, fn, reads, writes, rowgrp):
        if rowgrp != -1 and writes:
            b = writes[0]
            if rowgrp is not None and b.rowgrp is not None and b.rowgrp != rowgrp and b.w is not None and b.w[0][0] == "pe":
                self._wait("pe", b.w[0], b.w[1])
            b.rowgrp = rowgrp
        self._deps(e, reads, writes)
        ins = fn(self.E[e])
        self.nins += 1
        if e == "pe" and self.lazy_pe:
            self.pe_emitted += 1
            self.pe_last_ins = ins
            self.cnt[e] += 1
            self._stamp((("pe", "lazy"), self.pe_emitted), reads, writes)
            return
        c = self.cnt[e]
        ep, v = c // EPOCH, c % EPOCH + 1
        self.cnt[e] = c + 1
        key = (e, ep)
        ins.then_inc(self._semfor(key), 1)
        self._stamp((key, v), reads, writes)

    def _emit_dma(self, q, starter, reads, writes):
        cls = "ds" if q == "pool" else "dh"
        j = self.di[cls] % self.ndma
        self.di[cls] += 1
        key = (cls, j)
        self._wait(q, key, self.dval[key])
        self._deps(q, reads, writes)
        ins = starter(self.E[q])
        ins.then_inc(self.sem[key], 16)
        self.dval[key] += 16
        self.nins += 1
        self._stamp((key, self.dval[key]), reads, writes)

    def flush(self):
        rec, self.rec = self.rec, []
        n = len(rec)
        if n == 0:
            return
        order = range(n)
        if self.reorder and n > 2:
            order = self._schedule(rec)
        for i in order:
            kind, e, fn, reads, writes, _, _, rowgrp = rec[i]
            if kind == "op":
                self._emit_op(e, fn, reads, writes, rowgrp)
            else:
                self._emit_dma(e, fn, reads, writes)

    def _schedule(self, rec):
        import heapq
        n = len(rec)
        lastw, readers = {}, {}
        npred = [0] * n
        succ = [[] for _ in range(n)]
        for i, (kind, e, fn, reads, writes, ic, lat, rg) in enumerate(rec):
            deps = set()
            for b in reads:
                w = lastw.get(id(b))
                if w is not None:
                    deps.add(w)
            for b in writes:
                w = lastw.get(id(b))
                if w is not None:
                    deps.add(w)
                for r in readers.get(id(b), ()):
                    deps.add(r)
            deps.discard(i)
            npred[i] = len(deps)
            for d in deps:
                succ[d].append(i)
            for b in reads:
                readers.setdefault(id(b), []).append(i)
            for b in writes:
                lastw[id(b)] = i
                readers[id(b)] = []
        engs = ("pe", "act", "dve", "pool", "sp")
        prio_mode = os.environ.get("SCHED_PRIO", "cp")
        if prio_mode == "cp":
            bl = [0.0] * n
            for i in range(n - 1, -1, -1):
                m = 0.0
                for j in succ[i]:
                    if bl[j] > m:
                        m = bl[j]
                bl[i] = m + rec[i][6]
            if os.environ.get("SCHED_DEBUG") == "1":
                print(f"[sched] critical path us={max(bl) / 1e3:.1f}", flush=True)
            rank = sorted(range(n), key=lambda i: (-bl[i], i))
            pos = [0] * n
            for r_, i in enumerate(rank):
                pos[i] = r_
            enc = lambda i: pos[i]
            dec = lambda p: rank[p]
        else:
            enc = dec = lambda i: i
        ready = {e: [] for e in engs}
        avail = [0.0] * n
        tfree = {e: 0.0 for e in engs}
        for i in range(n):
            if npred[i] == 0:
                heapq.heappush(ready[rec[i][1]], enc(i))
        out = []
        W = self.window
        DELTA = 300.0
        SYNC = 350.0
        done = 0
        while done < n:
            best = None
            for e in engs:
                h = ready[e]
                if not h:
                    continue
                te = tfree[e]
                cand = heapq.nsmallest(W, h) if len(h) > 1 else h
                for p in cand:
                    i = dec(p)
                    st = avail[i] if avail[i] > te else te
                    if best is None or st < best[0] - DELTA or (abs(st - best[0]) <= DELTA and p < best[1]):
                        best = (st, p, e)
                    if st <= te:
                        break
            st, p, e = best
            i = dec(p)
            h = ready[e]
            if h[0] == p:
                heapq.heappop(h)
            else:
                h.remove(p)
                heapq.heapify(h)
            kind, _, _, _, _, ic, lat, _ = rec[i]
            tfree[e] = st + ic
            fin = st + lat
            out.append(i)
            done += 1
            for j in succ[i]:
                a = fin + (SYNC if rec[j][1] != e else 60.0)
                if a > avail[j]:
                    avail[j] = a
                npred[j] -= 1
                if npred[j] == 0:
                    heapq.heappush(ready[rec[j][1]], enc(j))
        if os.environ.get("SCHED_DEBUG") == "1":
            busy = {e: 0.0 for e in engs}
            for r in rec:
                busy[r[1]] += r[5]
            print(f"[sched] n={n} makespan_us={max(tfree.values()) / 1e3:.1f} busy_us=" + " ".join(f"{e}:{busy[e] / 1e3:.1f}" for e in engs), flush=True)
        return out

    def cur(self, k):
        if k == "pe" and self.lazy_pe:
            return (("pe", "lazy"), self.pe_emitted) if self.pe_emitted > 0 else None
        c = self.cnt[k]
        return ((k, (c - 1) // EPOCH), (c - 1) % EPOCH + 1) if c > 0 else None

    def barrier(self, engines=("pe", "act", "dve", "pool", "sp")):
        self.flush()
        for e in engines:
            for key, v in self.dval.items():
                self._wait(e, key, v)
            for k in ("pe", "act", "dve", "pool"):
                if k == e:
                    continue
                st = self.cur(k)
                if st is not None:
                    self._wait(e, st[0], st[1])


class _VH:
    dtype = F32

    def __init__(self, h, k):
        nd = len(h.shape)
        flat = h[:].rearrange("p a c -> p (a c)") if nd == 3 else h[:]
        self.ap = flat[:, k * 1024:(k + 1) * 1024].bitcast(F32)

    def __getitem__(self, idx):
        return self.ap[idx]


class View:
    __slots__ = ("h", "b")

    def __init__(self, base, k, name):
        self.h = _VH(base.h, k)
        self.b = Buf(name)

    def __getitem__(self, idx):
        return self.h[idx]


class Pool_:
    def __init__(self, tiles):
        self.free = list(tiles)

    def get(self):
        return self.free.pop(0)

    def put(self, *ts):
        for t in ts:
            self.free.append(t)


class Builder:
    def __init__(self, T, dbg=False, stages=("conv", "ret", "rwkv", "moe"), nlayers=L_DEPTH, same_sync=True):
        assert T % 1024 == 0
        self.T = T
        self.NB = T // TB
        self.stages = stages
        self.nlayers = nlayers
        self.same_sync = same_sync
        self.sparse = os.environ.get("MOE_DENSE", "0") != "1"
        self.nc = bass.Bass("TRN2", target_bir_lowering=False)
        self.dbg = dbg

    def sb(self, st, name, shape, dt):
        self._uid = getattr(self, "_uid", 0) + 1
        return Tl(st.enter_context(self.nc.sbuf_tensor(f"sb{self._uid}_{name}", shape, dt)), name)

    @staticmethod
    def _fsz(ap):
        n = 1
        for d in ap.shape[1:]:
            n *= int(d)
        return n

    def _ecost(self, eng, ap):
        n = self._fsz(ap)
        return (130.0 + 1.05 * n) if eng == "dve" else (200.0 + 2.0 * n)

    def mm(self, ps, out_ap, lt, lhsT_ap, rt, rhs_ap, start=True, stop=True, rowgrp=None):
        n = self._fsz(rhs_ap)
        cost = 64.0 + 0.45 * n
        if lt.h.dtype == F32:
            cost *= 4.0
        self.S.op("pe", lambda e: e.matmul(out_ap, lhsT_ap, rhs_ap, start=start, stop=stop), [lt, rt], [ps], cost=cost, rowgrp=rowgrp)

    def act(self, out_t, out_ap, in_t, in_ap, func, bias=None, scale=None, extra=()):
        kw = {}
        if bias is not None:
            kw["bias"] = bias
        if scale is not None:
            kw["scale"] = scale
        self.S.op("act", lambda e: e.activation(out=out_ap, in_=in_ap, func=func, **kw), [in_t, *extra], [out_t], cost=230.0 + 0.85 * self._fsz(out_ap))

    def tt(self, eng, out_t, out_ap, a_t, a_ap, b_t, b_ap, op):
        self.S.op(eng, lambda e: e.tensor_tensor(out=out_ap, in0=a_ap, in1=b_ap, op=op), [a_t, b_t], [out_t], cost=self._ecost(eng, out_ap))

    def ts(self, eng, out_t, out_ap, a_t, a_ap, s1, s2=None, op0=ALU.mult, op1=None, extra=()):
        if op1 is None:
            self.S.op(eng, lambda e: e.tensor_scalar(out=out_ap, in0=a_ap, scalar1=s1, scalar2=None, op0=op0), [a_t, *extra], [out_t], cost=self._ecost(eng, out_ap))
        else:
            self.S.op(eng, lambda e: e.tensor_scalar(out=out_ap, in0=a_ap, scalar1=s1, scalar2=s2, op0=op0, op1=op1), [a_t, *extra], [out_t], cost=self._ecost(eng, out_ap))

    def stt(self, eng, out_t, out_ap, a_t, a_ap, scalar, b_t, b_ap, op0, op1, extra=()):
        self.S.op(eng, lambda e: e.scalar_tensor_tensor(out=out_ap, in0=a_ap, scalar=scalar, in1=b_ap, op0=op0, op1=op1),
                  [a_t, b_t, *extra], [out_t], cost=self._ecost(eng, out_ap))

    def cp(self, eng, out_t, out_ap, in_t, in_ap):
        if eng == "act":
            self.S.op("act", lambda e: e.copy(out=out_ap, in_=in_ap), [in_t], [out_t], cost=230.0 + 0.85 * self._fsz(out_ap))
        else:
            self.S.op(eng, lambda e: e.tensor_copy(out=out_ap, in_=in_ap), [in_t], [out_t], cost=self._ecost(eng, out_ap))

    def memset(self, t, ap, val, eng="pool"):
        self.S.op(eng, lambda e: e.memset(ap, val), [], [t], cost=150.0 + 0.5 * self._fsz(ap))

    def reduce(self, out_t, out_ap, in_t, in_ap, op, extra=()):
        self.S.op("dve", lambda e: e.tensor_reduce(out=out_ap, in_=in_ap, axis=AX.X, op=op), [in_t, *extra], [out_t], cost=130.0 + 1.05 * self._fsz(in_ap))

    def scan(self, out_t, out_ap, d0_t, d0_ap, d1_t, d1_ap):
        self.S.op("dve", lambda e: e.tensor_tensor_scan(out=out_ap, data0=d0_ap, data1=d1_ap, initial=0.0, op0=ALU.mult, op1=ALU.add),
                  [d0_t, d1_t], [out_t], cost=130.0 + 2.1 * self._fsz(out_ap))

    def recip(self, out_t, out_ap, in_t, in_ap):
        self.S.op("dve", lambda e: e.reciprocal(out=out_ap, in_=in_ap), [in_t], [out_t], cost=120.0 + 4.0 * self._fsz(out_ap))

    def rsqrt_inplace(self, t, ap, src_t, src_ap, eps):
        self.act(t, ap, src_t, src_ap, AF.Sqrt, bias=self.eps_ap(eps), scale=1.0, extra=[self.epsT])
        self.recip(t, ap, t, ap)

    def eps_ap(self, eps):
        return self.epsT[:, self.eps_idx[eps]:self.eps_idx[eps] + 1]

    def build(self):
        nc = self.nc
        T = self.T
        Lr = L_DEPTH
        dt_in = lambda n, s, d=F32: nc.dram_tensor(n, s, d, kind="ExternalInput").ap()
        self.xT_in = dt_in("xT", [D, T])
        self.pos_in = dt_in("pos", [1, T], I32)
        self.cT_in = dt_in("cT", [128, 8])
        self.small_in = dt_in("small", [Lr, 128, NS])
        self.consts_in = dt_in("consts", [128, NCONST])
        self.w_ada = dt_in("w_ada", [Lr, D, 6 * D])
        self.w_in = dt_in("w_in", [Lr, D, 5376])
        self.w_gate = dt_in("w_gate", [Lr, D, 3 * D])
        self.w_lora = dt_in("rwkv_w_lora", [Lr, 64, MW])
        self.a_lora = dt_in("rwkv_a_lora", [Lr, 64, MW])
        self.g_lora = dt_in("rwkv_g_lora", [Lr, 128, MW])
        self.w_branch = dt_in("w_branch", [Lr, 3, MW, D])
        self.w_o = dt_in("w_o", [Lr, D, D])
        self.w_r = dt_in("w_r", [Lr, D, 36])
        self.b_r = dt_in("b_r", [Lr, 128, 36])
        self.w_eg = dt_in("w_exp_gate", [Lr, NE, D, FF])
        self.w_eu = dt_in("w_exp_up", [Lr, NE, D, FF])
        self.w_ed = dt_in("w_exp_down", [Lr, NE, FF, D])
        self.out = nc.dram_tensor("outT", [D, T], F32, kind="ExternalOutput").ap()
        self.xmid = nc.dram_tensor("xmid", [D, T], F32).ap()
        self.xl0 = nc.dram_tensor("xl0", [D, T], F32).ap()
        self.NG = 25
        self.wbf = nc.dram_tensor("wbf", [Lr, self.NG, 128, 4096], BF16).ap()
        self.wbf_b = [[Buf(f"wbf{l}_{g}") for g in range(self.NG)] for l in range(Lr)]
        self.NBLK = (2 * T) // BLK + NE
        self.NRW = self.NBLK * BLK
        self.h2rows = nc.dram_tensor("h2rows", [T + 1, D], BF16).ap()
        self.yrows = nc.dram_tensor("yrows", [self.NRW, D], F32).ap()
        self.rowtok = nc.dram_tensor("rowtok", [self.NRW, 1], I32).ap()
        self.wexp = [[nc.dram_tensor(f"wexp{l}_{w}", [NE * 128, 4096], BF16).ap() for w in range(3)] for l in range(Lr)]
        self.wexp_b = [[Buf(f"wexp{l}_{i}") for i in range(3 * NE)] for l in range(Lr)]
        self.h2rows_b = [Buf(f"h2r{i}") for i in range(T // 128 + 1)]
        self.yrows_b = [Buf(f"yr{i}") for i in range(self.NBLK * RT)]
        self.rowtok_fill_b = Buf("rtfill")
        self.scat_b = [Buf(f"scat{i}") for i in range(2 * (T // 128))]
        self.dbg_out = {}
        if self.dbg:
            for nm, rows in (("d_h", D), ("d_ycv", MW), ("d_yrt", MW), ("d_yrw", MW), ("d_xmid", D), ("d_xl", D)):
                self.dbg_out[nm] = nc.dram_tensor(nm, [Lr, rows, T], F32, kind="ExternalOutput").ap()
        self.db = {n: [Buf(f"{n}{b}") for b in range(self.NB)] for n in ("xmid", "xl0", "out")}

        with contextlib.ExitStack() as st:
            self.S = S = Sched(nc, st, n_dma=12, same_sync=self.same_sync)
            sb = lambda n, s, d: self.sb(st, n, s, d)
            self.consts = sb("consts", [128, NCONST], F32)
            self.cbf = sb("cbf", [128, 1024], BF16)
            self.small = [sb(f"small{l}", [128, NS], F32) for l in range(Lr)]
            self.der = [sb(f"der{l}", [128, 96], F32) for l in range(Lr)]
            self.epsT = sb("epsT", [128, 4], F32)
            self.eps_idx = {1e-6: 0, 1e-5: 1, 64e-5: 2, 0.0: 3}
            self.cT = sb("cT", [128, 8], F32)
            self.ret_st32 = [sb(f"rst32_{l}", [128, 4, 128], F32) for l in range(Lr)]
            self.ret_stb = [sb(f"rstb_{l}", [128, 4, 128], BF16) for l in range(Lr)]
            self.rw_S32 = [sb(f"wS32_{l}", [128, 4, 128], F32) for l in range(Lr)]
            self.rw_Sb = [sb(f"wSb_{l}", [128, 4, 128], BF16) for l in range(Lr)]
            self.uext = [[sb(f"uext{l}_{c}", [128, 514], F32) for c in range(4)] for l in range(Lr)]
            self.carry = [sb(f"carry{l}", [128, 14], F32) for l in range(Lr)]
            self.lora_bf = [sb(f"lora{l}", [128, 2, MW], BF16) for l in range(Lr)]
            self.psum = Pool_([Tl(st.enter_context(nc.psum_tensor(f"ps{i}", [128, 512], F32)), f"ps{i}", excl=True) for i in range(8)])

            self.setup()
            for l in range(self.nlayers):
                self.wconvert(l)
            with contextlib.ExitStack() as st0:
                for l in range(self.nlayers):
                    self.layer_setup(l, [self.sb(st0, f"wada{l}_{i}", [128, 8, 1024], F32) for i in range(2)])
                S.barrier()
            for l in range(self.nlayers):
                src = self.xT_in if l == 0 else self.xl0
                srcb = None if l == 0 else self.db["xl0"]
                with contextlib.ExitStack() as st2:
                    self.mixer_alloc(st2)
                    self.ws_begin(l)
                    for b in range(self.NB):
                        self.mixer_block(l, b, src, srcb)
                    S.barrier()
                last = (l == self.nlayers - 1)
                dst, dstb = (self.out, self.db["out"]) if last else (self.xl0, self.db["xl0"])
                with contextlib.ExitStack() as st2:
                    if self.sparse:
                        self.moe_alloc_sparse(st2)
                        self.moe_sparse(l, dst, dstb, last)
                    else:
                        self.moe_alloc(st2)
                        for sbk in range(T // 1024):
                            self.moe_super(l, sbk, dst, dstb, last)
                    S.barrier()
            S.barrier()
        return nc

    def wgroups(self, l):
        win = self.w_in[l].rearrange("(k p) c -> p k c", p=128)
        wg_v = self.w_gate[l].rearrange("(k p) c -> p k c", p=128)
        wo_v = self.w_o[l].rearrange("(k p) c -> p k c", p=128)
        gs = []
        for c0 in (3840, 4352, 4864, 0, 512, 1024, 1536, 2048, 2560, 3072):
            gs.append((win[:, :, c0:c0 + 512], 8, 512))
        gs.append((win[:, :, 3584:3840], 8, 256))
        for i in range(3):
            wb_v = self.w_branch[l, i].rearrange("(k p) c -> p k c", p=128)
            for half in range(2):
                gs.append((wg_v[:, :, i * 1024 + half * 512:i * 1024 + (half + 1) * 512], 8, 512))
                gs.append((wb_v[:, :, half * 512:(half + 1) * 512], 4, 512))
        for half in range(2):
            gs.append((wo_v[:, :, half * 512:(half + 1) * 512], 8, 512))
        assert len(gs) == self.NG
        return gs

    def wconvert(self, l):
        for g, (src, k, c) in enumerate(self.wgroups(l)):
            dst = self.wbf[l, g][:, 0:k * c].rearrange("p (k c) -> p k c", k=k)
            self.S.dma("pool", dst, src, writes=[self.wbf_b[l][g]])

    def econvert(self, l, i):
        which, e = i // NE, i % NE
        srcs = (self.w_eg, self.w_eu, self.w_ed)
        k = 8 if which < 2 else 4
        src = srcs[which][l, e].rearrange("(k p) c -> p k c", p=128)
        dst = self.wexp[l][which][e * 128:(e + 1) * 128, :].rearrange("p (k c) -> p k c", k=k)
        self.S.dma("pool", dst, src, writes=[self.wexp_b[l][i]])

    def idma(self, out, out_off, in_, in_off, reads, writes, **kw):
        nb = 128 * self._fsz(out) * 2
        self.S.dma("pool", None, None, reads=reads, writes=writes, nbytes=nb,
                   starter=lambda E: E.indirect_dma_start(out=out, out_offset=out_off, in_=in_, in_offset=in_off, **kw))

    def ws_begin(self, l):
        self.ws_l = l
        self.ws_shapes = [(k, c) for (_, k, c) in self.wgroups(l)]
        self.ws_idx = 0
        self.ws_total = self.NB * self.NG
        self.ws_q = []
        self._ws_issue()

    def _ws_issue(self):
        l = self.ws_l
        while self.wring.free and self.ws_idx < self.ws_total:
            t = self.wring.get()
            g = self.ws_idx % self.NG
            self.ws_idx += 1
            k, c = self.ws_shapes[g]
            src = self.wbf[l, g][:, 0:k * c].rearrange("p (k c) -> p k c", k=k)
            self.S.dma("sp", t[:, 0:k, 0:c], src, reads=[self.wbf_b[l][g]], writes=[t])
            self.ws_q.append(t)

    def wnext(self):
        if not self.ws_q:
            self._ws_issue()
        return self.ws_q.pop(0)

    def wput(self, *ts):
        self.wring.put(*ts)
        self._ws_issue()

    def setup(self):
        S = self.S
        S.dma("sp", self.consts[:], self.consts_in, writes=[self.consts])
        S.dma("pool", self.cbf[:, 0:128], self.consts_in[:, C_ID:C_ID + 128], writes=[self.cbf])
        S.dma("pool", self.cbf[:, 128:256], self.consts_in[:, C_RMT:C_RMT + 128], writes=[self.cbf])
        S.dma("pool", self.cbf[:, 256:512], self.consts_in[:, C_LTRI:C_LTRI + 256], writes=[self.cbf])
        S.dma("pool", self.cbf[:, 512:1024], self.consts_in[:, C_MEAND:C_MEAND + 512], writes=[self.cbf])
        S.dma("sp", self.cT[:], self.cT_in, writes=[self.cT])
        for eps, i in self.eps_idx.items():
            self.memset(self.epsT, self.epsT[:, i:i + 1], float(eps))
        for l in range(L_DEPTH):
            S.dma("sp", self.small[l][:], self.small_in[l], writes=[self.small[l]])
            S.dma("pool", self.lora_bf[l][0:64, 0, :], self.w_lora[l], writes=[self.lora_bf[l]])
            S.dma("pool", self.lora_bf[l][64:128, 0, :], self.a_lora[l], writes=[self.lora_bf[l]])
            S.dma("pool", self.lora_bf[l][:, 1, :], self.g_lora[l], writes=[self.lora_bf[l]])
            for t in (self.ret_st32[l], self.ret_stb[l], self.rw_S32[l], self.rw_Sb[l], self.carry[l]):
                self.memset(t, t[:], 0.0)
            for c in range(4):
                self.memset(self.uext[l][c], self.uext[l][c][:], 0.0)
        self.act(self.cT, self.cT[:], self.cT, self.cT[:], AF.Silu)

    def layer_setup(self, l, wt):
        S = self.S
        nc = self.nc
        der = self.der[l]
        sm = self.small[l]
        wa_v = self.w_ada[l].rearrange("(k p) c -> p k c", p=128)
        pm = self.psum.get()
        if True:
            for g in range(6):
                w = wt[g % 2]
                S.dma("sp", w[:], wa_v[:, :, g * 1024:(g + 1) * 1024], writes=[w])
                for j in range(8):
                    col = g * 8 + j
                    for k in range(8):
                        self.mm(pm, pm[:, col:col + 1], w, w[:, k, j * 128:(j + 1) * 128], self.cT, self.cT[:, k:k + 1], k == 0, k == 7)
            self.tt("dve", der, der[:, 0:48], pm, pm[:, 0:48], sm, sm[:, O_BADA:O_BADA + 48], ALU.add)
        self.psum.put(pm)
        self.stt("dve", der, der[:, 48:56], der, der[:, 8:16], 1.0, sm, sm[:, O_N1:O_N1 + 8], ALU.add, ALU.mult)
        self.stt("dve", der, der[:, 56:64], der, der[:, 32:40], 1.0, sm, sm[:, O_N2:O_N2 + 8], ALU.add, ALU.mult)
        self.ts("dve", der, der[:, 64:78], sm, sm[:, O_MU:O_MU + 14], -1.0, 1.0, ALU.mult, ALU.add)
        self.ts("dve", der, der[:, 78:82], sm, sm[:, O_KA:O_KA + 4], -1.0, 1.0, ALU.mult, ALU.add)

    def shift1(self, l, k): return self.der[l][:, 0 + k:1 + k]
    def gate1(self, l, k): return self.der[l][:, 16 + k:17 + k]
    def shift2(self, l, k): return self.der[l][:, 24 + k:25 + k]
    def gate2(self, l, k): return self.der[l][:, 40 + k:41 + k]
    def G1(self, l, k): return self.der[l][:, 48 + k:49 + k]
    def G2(self, l, k): return self.der[l][:, 56 + k:57 + k]
    def ommu(self, l, ch): return self.der[l][:, 64 + ch:65 + ch]
    def omka(self, l, hp): return self.der[l][:, 78 + hp:79 + hp]

    def cstb(self, off):
        o = 512 + (off - C_MEAND)
        return self.cbf[:, o:o + 128]

    def cst(self, off, n=128, rows=slice(0, 128)):
        return self.consts[rows, off:off + n]

    def norm_block(self, xs, Gf, shiftf, out_fn, der_t):
        pm = self.psum.get()
        for k in range(8):
            sq = self.btmp.get()
            self.act(sq, sq[:], xs[k], xs[k][:], AF.Square)
            self.mm(pm, pm[:], self.cbf, self.cstb(C_MEAND), sq, sq[:], k == 0, k == 7)
            self.btmp.put(sq)
        rs = self.tmp.get()
        self.rsqrt_inplace(rs, rs[:], pm, pm[:], 1e-6)
        self.psum.put(pm)
        for k in range(8):
            t = self.tmp.get()
            self.stt("dve", t, t[:], xs[k], xs[k][:], Gf(k), rs, rs[:], ALU.mult, ALU.mult, extra=[der_t])
            ot, oap = out_fn(k)
            if shiftf is None:
                self.cp("act", ot, oap, t, t[:])
            else:
                self.act(ot, oap, t, t[:], AF.Identity, bias=shiftf(k), scale=1.0, extra=[der_t])
            self.tmp.put(t)
        self.tmp.put(rs)

    def wload(self, w, src_ap):
        self.S.dma("pool", w[:], src_ap, writes=[w])

    def mixer_alloc(self, st):
        sb = lambda n, s, d: self.sb(st, n, s, d)
        self.tmp = Pool_([sb(f"tmp{i}", [128, 512], F32) for i in range(15)])
        self.btmp = Pool_([sb(f"btmp{i}", [128, 512], BF16) for i in range(3)])
        self.wring = Pool_([sb(f"wr{i}", [128, 8, 512], BF16) for i in range(3)])
        self.hT = sb("hT", [128, 8, 512], BF16)
        self.cosT = sb("cosT", [128, 512], F32)
        self.sinT = sb("sinT", [128, 512], F32)
        self.ycv = sb("ycv", [128, 4, 512], BF16)
        self.yrt = sb("yrt", [128, 4, 512], BF16)
        self.yrw = sb("yrw", [128, 4, 512], BF16)
        self.B4 = [sb(f"b4_{i}", [128, 4, 512], BF16) for i in range(5)]
        self.rr = sb("rr", [128, 4, 512], F32)
        self.kk = sb("kk", [128, 4, 512], F32)
        self.lw_in = sb("lw_in", [128, 512], BF16)
        self.gsig = sb("gsig", [128, 512], BF16)
        self.pext = [sb("pext0", [128, 513], F32)] * 2
        self.YY = sb("YY", [128, 4096], BF16)
        self.PC = sb("PC", [128, 4, 8], F32)
        c64 = lambda n, w, d: sb(n, [128, w], d)
        self.KTtm = [c64("KTtm0", 512, BF16)] * 2
        self.BDtm = [c64("BDtm0", 512, BF16)] * 2
        self.Vtm = [c64("Vtm0", 512, BF16)] * 2
        self.Vpad = [c64("Vpad0", 1024, BF16)] * 2
        self.nU = c64("nU", 512, BF16)
        self.nUpad = c64("nUpad", 1024, BF16)
        self.AktT = c64("AktT", 512, BF16)
        self.BktT = c64("BktT", 512, BF16)
        self.BbT = c64("BbT", 512, BF16)
        self.Zp = [c64(f"Zp{i}", 512, BF16) for i in range(2)]
        self.Xp = [c64(f"Xp{i}", 512, BF16) for i in range(2)]
        self.RT32 = c64("RT32", 512, F32)
        self.R32 = c64("R32", 512, F32)
        self.RTb = [c64("RTb0", 512, BF16)] * 2
        self.Rb = [c64("Rb0", 512, BF16)] * 2
        self.RHSb = c64("RHSb", 512, BF16)
        self.stmp = [sb("stmp0", [128, 128], F32)] * 2
        S = self.S
        for t in (self.Vpad[0], self.nUpad):
            self.memset(t, t[:], 0.0)

    def rope_tables(self, b):
        S = self.S
        t0 = b * TB
        posi = self.tmp.get()
        S.dma("sp", posi[:].bitcast(I32), self.pos_in[:, t0:t0 + TB].partition_broadcast(128), writes=[posi])
        ang = self.tmp.get()
        kf = self.tmp.get()
        ki = self.tmp.get()
        self.cp("dve", ang, ang[:], posi, posi[:].bitcast(I32))
        self.tmp.put(posi)
        self.ts("dve", ang, ang[:], ang, ang[:], self.cst(C_INVF, 1), extra=[self.consts])
        for which, dst in ((0, self.sinT), (1, self.cosT)):
            r = self.tmp.get()
            if which == 1:
                self.ts("dve", r, r[:], ang, ang[:], float(np.pi / 2), None, ALU.add)
                src = r
            else:
                src = ang
            self.ts("dve", kf, kf[:], src, src[:], float(1.0 / (2 * np.pi)))
            kiv = ki[:].bitcast(I32)
            self.cp("dve", ki, kiv, kf, kf[:])
            self.cp("dve", kf, kf[:], ki, kiv)
            self.stt("dve", r, r[:], kf, kf[:], float(-2 * np.pi), src, src[:], ALU.mult, ALU.add)
            self.ts("dve", kf, kf[:], r, r[:], float(np.pi), float(-2 * np.pi), ALU.is_gt, ALU.mult)
            self.tt("dve", r, r[:], r, r[:], kf, kf[:], ALU.add)
            self.ts("dve", r, r[:], r, r[:], float(np.pi), float(-np.pi), ALU.min, ALU.max)
            self.act(dst, dst[:], r, r[:], AF.Sin)
            self.tmp.put(r)
        self.tmp.put(ang, kf, ki)

    def proj(self, ps, w, cols, rhs_t=None, rhs_ap_fn=None):
        for k in range(8):
            self.mm(ps, ps[:], w, w[:, k, cols], self.hT, self.hT[:, k, :], k == 0, k == 7)

    def mixer_block(self, l, b, src, srcb):
        S = self.S
        t0 = b * TB
        tsl = slice(t0, t0 + TB)
        sm = self.small[l]
        der = self.der[l]
        src_v = src.rearrange("(k p) t -> p k t", p=128)
        win_v = self.w_in[l].rearrange("(k p) c -> p k c", p=128)
        if self.sparse:
            per = -(-3 * NE // self.NB)
            for i in range(b * per, min((b + 1) * per, 3 * NE)):
                self.econvert(l, i)
        xs = [self.tmp.get() for _ in range(8)]
        for k in range(8):
            S.dma("sp", xs[k][:], src_v[:, k, tsl], reads=([srcb[b]] if srcb else []), writes=[xs[k]])
        self.norm_block(xs, lambda k: self.G1(l, k), lambda k: self.shift1(l, k), lambda k: (self.hT, self.hT[:, k, :]), der)
        self.tmp.put(*xs)
        def zero_y(y, ngroups=0):
            self.memset(y, y[:], 0.0)
            for _ in range(ngroups):
                self.wput(self.wnext())
        if "conv" in self.stages:
            self.conv_stage(l, b, win_v)
        else:
            zero_y(self.ycv, 3)
        if "ret" in self.stages:
            self.rope_tables(b)
            self.ret_stage(l, b, win_v)
        else:
            zero_y(self.yrt, 4)
        if "rwkv" in self.stages:
            self.rwkv_stage(l, b, win_v)
        else:
            zero_y(self.yrw, 4)
        if self.dbg:
            for nm, y in (("d_h", self.hT), ("d_ycv", self.ycv), ("d_yrt", self.yrt), ("d_yrw", self.yrw)):
                S.dma("pool", self.dbg_out[nm][l].rearrange("(k p) t -> p k t", p=128)[:, :, tsl], y[:], reads=[y])
        self.merge_stage(l, b, src_v, srcb)

    def conv_stage(self, l, b, win_v):
        sm = self.small[l]
        ws = [self.wnext() for _ in range(3)]
        wh, wB, wC = ws
        for cc in range(4):
            cols = slice(cc * 128, (cc + 1) * 128)
            ph, pB, pC = self.psum.get(), self.psum.get(), self.psum.get()
            self.proj(ph, wh, cols)
            self.proj(pC, wC, cols)
            self.proj(pB, wB, cols)
            hc = self.tmp.get()
            self.cp("act", hc, hc[:], ph, ph[:])
            self.psum.put(ph)
            ue = self.uext[l][cc]
            self.tt("dve", ue, ue[:, 2:514], pC, pC[:], hc, hc[:], ALU.mult)
            self.psum.put(pC)
            c1 = hc
            cw = lambda tap: sm[:, O_CONVW + tap * 4 + cc:O_CONVW + tap * 4 + cc + 1]
            self.ts("dve", c1, c1[:], ue, ue[:, 0:512], cw(0), extra=[sm])
            self.stt("dve", c1, c1[:], ue, ue[:, 1:513], cw(1), c1, c1[:], ALU.mult, ALU.add, extra=[sm])
            self.stt("dve", c1, c1[:], ue, ue[:, 2:514], cw(2), c1, c1[:], ALU.mult, ALU.add, extra=[sm])
            self.tt("dve", self.ycv, self.ycv[:, cc, :], pB, pB[:], c1, c1[:], ALU.mult)
            self.psum.put(pB)
            self.cp("pool", ue, ue[:, 0:2], ue, ue[:, 512:514])
            self.tmp.put(c1)
        self.wput(*ws)

    def ret_stage(self, l, b, win_v):
        S = self.S
        sm = self.small[l]
        qr, kr, v_tm, gs, kz_tm = self.B4
        idb = self.cbf[:, 0:128]
        rmt = self.cbf[:, 128:256]
        g128 = [math.exp(128.0 * math.log1p(-2.0 ** (-5 - h))) for h in range(4)]
        wq, wk, wv = self.wnext(), self.wnext(), self.wnext()
        for w_, dst in ((wq, qr), (wk, kr)):
            for h in range(4):
                p_ = self.psum.get()
                self.proj(p_, w_, slice(h * 128, (h + 1) * 128))
                SUB = int(os.environ.get("RET_SUB", 9))
                qb = self.btmp.get()
                if SUB >= 1:
                    self.cp("act", qb, qb[:], p_, p_[:])
                pr = self.psum.get()
                if SUB >= 2:
                    self.mm(pr, pr[:], self.cbf, rmt, qb, qb[:])
                t1, t2 = self.tmp.get(), self.tmp.get()
                if SUB >= 3:
                    self.tt("dve", t1, t1[:], p_, p_[:], self.cosT, self.cosT[:], ALU.mult)
                self.psum.put(p_)
                if SUB >= 4:
                    self.tt("dve", t2, t2[:], pr, pr[:], self.sinT, self.sinT[:], ALU.mult)
                self.psum.put(pr)
                if SUB >= 0:
                    self.tt(os.environ.get("ROT_ENG", "dve"), dst, dst[:, h, :], t1, t1[:], t2, t2[:], ALU.add)
                self.tmp.put(t1, t2)
                self.btmp.put(qb)
        self.wput(wq, wk)
        wg = self.wnext()
        for c in range(4):
            p_ = self.psum.get()
            for k in range(8):
                self.mm(p_, p_[:], self.hT, self.hT[:, k, c * 128:(c + 1) * 128], wv, wv[:, k, :], k == 0, k == 7)
            self.cp("act", v_tm, v_tm[:, c, :], p_, p_[:])
            self.psum.put(p_)
        self.wput(wv)
        for h in range(4):
            p_ = self.psum.get()
            self.proj(p_, wg, slice(h * 128, (h + 1) * 128))
            self.act(gs, gs[:, h, :], p_, p_[:], AF.Silu)
            self.psum.put(p_)
        self.wput(wg)
        for c in range(4):
            p_ = self.psum.get()
            for h in range(4):
                self.mm(p_, p_[:, h * 128:(h + 1) * 128], kr, kr[:, h, c * 128:(c + 1) * 128], self.cbf, idb)
            for h in range(4):
                self.act(kz_tm, kz_tm[:, c, h * 128:(h + 1) * 128], p_, p_[:, h * 128:(h + 1) * 128], AF.Copy,
                         scale=self.cst(C_ZETA + h, 1), extra=[self.consts])
            self.psum.put(p_)
        st32, stb = self.ret_st32[l], self.ret_stb[l]
        if int(os.environ.get("RET_STOP", 99)) <= 2:
            self.memset(self.yrt, self.yrt[:], 0.0)
            return
        for h in range(4):
            hs = slice(h * 128, (h + 1) * 128)
            psT = self.psum.get()
            for c in range(4):
                cs = slice(c * 128, (c + 1) * 128)
                self.mm(psT, psT[:, cs], kr, kr[:, h, cs], qr, qr[:, h, cs])
            sT = self.btmp.get()
            for c in range(4):
                cs = slice(c * 128, (c + 1) * 128)
                self.tt("dve", sT, sT[:, cs], psT, psT[:, cs], self.consts, self.cst(C_DT + h * 128), ALU.mult)
            self.psum.put(psT)
            pI, pC, pKV = self.psum.get(), self.psum.get(), self.psum.get()
            for c in range(4):
                cs = slice(c * 128, (c + 1) * 128)
                self.mm(pI, pI[:, cs], v_tm, v_tm[:, c, hs], sT, sT[:, cs])
            for c in range(4):
                cs = slice(c * 128, (c + 1) * 128)
                self.mm(pKV, pKV[:, cs], kz_tm, kz_tm[:, c, hs], v_tm, v_tm[:, c, hs])
            for c in range(4):
                cs = slice(c * 128, (c + 1) * 128)
                self.mm(pC, pC[:, cs], stb, stb[:, h, :], qr, qr[:, h, cs])
                self.stt("dve", st32, st32[:, h, :], st32, st32[:, h, :], float(g128[h]), pKV, pKV[:, cs], ALU.mult, ALU.add)
                self.cp("act", stb, stb[:, h, :], st32, st32[:, h, :])
            self.btmp.put(sT)
            self.psum.put(pKV)
            t1, o = self.tmp.get(), self.tmp.get()
            for c in range(4):
                cs = slice(c * 128, (c + 1) * 128)
                self.tt("dve", t1, t1[:, cs], pC, pC[:, cs], self.consts, self.cst(C_XI + h * 128), ALU.mult)
            self.psum.put(pC)
            self.tt("dve", o, o[:], pI, pI[:], t1, t1[:], ALU.add)
            self.psum.put(pI)
            self.head_norm(o, t1, C_MEAN128, 1e-5)
            self.stt("dve", self.yrt, self.yrt[:, h, :], o, o[:], sm[:, O_RGN + h:O_RGN + h + 1], gs, gs[:, h, :], ALU.mult, ALU.mult, extra=[sm])
            self.tmp.put(t1, o)

    def head_norm(self, o, scratch, mean_off, eps):
        pm = self.psum.get()
        self.mm(pm, pm[:], self.consts, self.cst(mean_off), o, o[:])
        self.tt("dve", o, o[:], o, o[:], pm, pm[:], ALU.subtract)
        self.psum.put(pm)
        sqb = self.btmp.get()
        self.act(sqb, sqb[:], o, o[:], AF.Square)
        pv = self.psum.get()
        self.mm(pv, pv[:], self.cbf, self.cstb(mean_off), sqb, sqb[:])
        self.btmp.put(sqb)
        sq = scratch
        self.rsqrt_inplace(sq, sq[:], pv, pv[:], eps)
        self.psum.put(pv)
        self.tt("dve", o, o[:], o, o[:], sq, sq[:], ALU.mult)

    def rwkv_stage(self, l, b, win_v):
        S = self.S
        sm = self.small[l]
        der = self.der[l]
        KT, BD, vv, g_rw, bon = self.B4
        rr, kk = self.rr, self.kk
        YY5 = self.YY[:].rearrange("p (hp c two t) -> p hp c two t", hp=4, c=8, two=2)
        idb = self.cbf[:, 0:128]
        lora = self.lora_bf[l]
        carry = self.carry[l]
        S32, Sb = self.rw_S32[l], self.rw_Sb[l]

        def shifted(p_, ch, out_t, out_ap, rows=slice(0, 128)):
            pe_ = self.pext[ch % 2]
            self.cp("act", pe_, pe_[:, 1:513], p_, p_[:])
            self.cp("pool", pe_, pe_[:, 0:1], carry, carry[:, ch:ch + 1])
            t = self.tmp.get()
            self.act(t, t[:], p_, p_[:], AF.Copy, scale=self.ommu(l, ch), extra=[der])
            self.stt("dve", out_t, out_ap, pe_, pe_[:, 0:512], sm[:, O_MU + ch:O_MU + ch + 1], t, t[:], ALU.mult, ALU.add, extra=[sm])
            self.cp("pool", carry, carry[:, ch:ch + 1], pe_, pe_[:, 512:513])
            self.tmp.put(t)

        for gi, dst in ((0, rr), (1, kk), (2, vv)):
            w = self.wnext()
            for cc in range(4):
                p_ = self.psum.get()
                self.proj(p_, w, slice(cc * 128, (cc + 1) * 128))
                shifted(p_, gi * 4 + cc, dst, dst[:, cc, :])
                self.psum.put(p_)
            self.wput(w)
        w = self.wnext()
        lo = self.tmp.get()
        p_ = self.psum.get()
        self.proj(p_, w, slice(0, 128))
        shifted(p_, 12, lo, lo[:])
        self.psum.put(p_)
        self.act(self.lw_in, self.lw_in[0:64, :], lo, lo[0:64, :], AF.Tanh)
        self.cp("act", self.lw_in, self.lw_in[64:128, :], lo, lo[64:128, :])
        p_ = self.psum.get()
        self.proj(p_, w, slice(128, 256))
        shifted(p_, 13, lo, lo[:])
        self.psum.put(p_)
        self.act(self.gsig, self.gsig[:], lo, lo[:], AF.Sigmoid)
        self.tmp.put(lo)
        self.wput(w)

        for hp in range(4):
            hc = slice(hp * 128, (hp + 1) * 128)
            sc = lambda off: sm[:, off + hp:off + hp + 1]
            pw = self.psum.get()
            self.mm(pw, pw[:], lora, lora[0:64, 0, hc], self.lw_in, self.lw_in[0:64, :], rowgrp=0)
            sgw = self.tmp.get()
            self.act(sgw, sgw[:], pw, pw[:], AF.Sigmoid, bias=sc(O_W0), scale=1.0, extra=[sm])
            self.psum.put(pw)
            pa = self.psum.get()
            self.mm(pa, pa[:], lora, lora[64:128, 0, hc], self.lw_in, self.lw_in[64:128, :], rowgrp=1)
            a_ = self.tmp.get()
            self.act(a_, a_[:], pa, pa[:], AF.Sigmoid, bias=sc(O_A0), scale=1.0, extra=[sm])
            self.psum.put(pa)
            pg = self.psum.get()
            self.mm(pg, pg[:], lora, lora[:, 1, hc], self.gsig, self.gsig[:])
            self.cp("act", g_rw, g_rw[:, hp, :], pg, pg[:])
            self.psum.put(pg)
            sqb = self.btmp.get()
            self.act(sqb, sqb[:], kk, kk[:, hp, :], AF.Square, scale=sc(O_KK), extra=[sm])
            pss = self.psum.get()
            self.mm(pss, pss[:], self.cbf, self.cstb(C_B64ONES), sqb, sqb[:])
            self.btmp.put(sqb)
            rn = self.tmp.get()
            self.ts("dve", rn, rn[:], pss, pss[:], 1e-12, None, ALU.max)
            self.psum.put(pss)
            self.act(rn, rn[:], rn, rn[:], AF.Sqrt, bias=self.eps_ap(0.0), scale=1.0, extra=[self.epsT])
            self.recip(rn, rn[:], rn, rn[:])
            kh = self.tmp.get()
            self.stt("dve", kh, kh[:], kk, kk[:, hp, :], sc(O_KK), rn, rn[:], ALU.mult, ALU.mult, extra=[sm])
            self.tmp.put(rn)
            kt = self.tmp.get()
            self.ts("dve", kt, kt[:], a_, a_[:], sc(O_KA), self.omka(l, hp), ALU.mult, ALU.add, extra=[sm, der])
            self.tt("dve", kt, kt[:], kt, kt[:], kk, kk[:, hp, :], ALU.mult)
            bb = a_
            self.tt("pool", bb, bb[:], a_, a_[:], kh, kh[:], ALU.mult)
            cum = self.tmp.get()
            self.scan(cum, cum[:], self.consts, self.cst(C_SEG, 512), sgw, sgw[:])
            einc, eneg = self.tmp.get(), self.tmp.get()
            self.act(einc, einc[:], cum, cum[:], AF.Exp, scale=-CDEC)
            self.act(eneg, eneg[:], cum, cum[:], AF.Exp, scale=CDEC)
            eexc = cum
            self.tt("dve", eexc, eexc[:], cum, cum[:], sgw, sgw[:], ALU.subtract)
            self.act(eexc, eexc[:], eexc, eexc[:], AF.Exp, scale=-CDEC)
            self.tmp.put(sgw)
            v3 = lambda ap: ap.rearrange("p (c t) -> p c t", t=64)
            self.tt("dve", self.YY, YY5[:, hp, :, 0, :], kh, v3(kh[:]), eexc, v3(eexc[:]), ALU.mult)
            self.tt("pool", self.YY, YY5[:, hp, :, 1, :], rr, v3(rr[:, hp, :]), einc, v3(einc[:]), ALU.mult)
            self.tt("dve", KT, KT[:, hp, :], kt, kt[:], eneg, eneg[:], ALU.mult)
            self.tt("pool", BD, BD[:, hp, :], bb, bb[:], eneg, eneg[:], ALU.mult)
            self.cp("act", self.PC, self.PC[:, hp, :], einc, v3(einc[:])[:, :, 63])
            rk = self.btmp.get()
            self.stt("dve", rk, rk[:], rr, rr[:, hp, :], sc(O_RK), kt, kt[:], ALU.mult, ALU.mult, extra=[sm])
            pb = self.psum.get()
            self.mm(pb, pb[:], self.cbf, self.cstb(C_B64ONES), rk, rk[:])
            self.btmp.put(rk)
            self.tt("dve", bon, bon[:, hp, :], pb, pb[:], vv, vv[:, hp, :], ALU.mult)
            self.psum.put(pb)
            self.tmp.put(kh, kt, bb, eexc, einc, eneg)

        yraw = [self.tmp.get() for _ in range(4)]
        hsl = lambda h: slice(h * 64, (h + 1) * 64)
        KTtm, BDtm, Vtm, Vpad = self.KTtm[0], self.BDtm[0], self.Vtm[0], self.Vpad[0]
        Vpad4 = Vpad[:].rearrange("p (hp hf v) -> p hp hf v", hp=4, hf=2)
        nUpad4 = self.nUpad[:].rearrange("p (hp hf v) -> p hp hf v", hp=4, hf=2)
        mst, mit, msx, i64 = (self.consts[:, o:o + 512] for o in (C_MST, C_MIT, C_MSX, C_I64))
        for cpair in range(4):
            chunks = (2 * cpair, 2 * cpair + 1)
            pqs = (slice(0, 64), slice(64, 128))
            csl = lambda c: slice(c * 64, (c + 1) * 64)
            for srct, dstt in ((KT, KTtm), (BD, BDtm), (vv, Vtm)):
                pT = self.psum.get()
                for q, c in enumerate(chunks):
                    for hp in range(4):
                        self.mm(pT, pT[pqs[q], hp * 128:(hp + 1) * 128], srct, srct[:, hp, csl(c)], self.cbf, idb)
                self.cp("act", dstt, dstt[:], pT, pT[:])
                if srct is vv:
                    pT4 = pT[:].rearrange("p (hp hf v) -> p hp hf v", hp=4, hf=2)
                    for hf in range(2):
                        self.cp("dve", Vpad, Vpad4[:, :, hf, hf * 64:(hf + 1) * 64], pT, pT4[:, :, hf, :])
                self.psum.put(pT)

            def abmat(lt, l_fn, rt, r_fn):
                p_ = self.psum.get()
                for hf in range(2):
                    prr = slice(hf * 64, hf * 64 + 64)
                    for q, c in enumerate(chunks):
                        for hp in range(4):
                            h = 2 * hp + hf
                            self.mm(p_, p_[pqs[q], hsl(h)], lt, l_fn(prr, hp, c), rt, r_fn(prr, hp, c), rowgrp=hf)
                return p_
            kt_fn = lambda prr, hp, c: KT[prr, hp, csl(c)]
            bd_fn = lambda prr, hp, c: BD[prr, hp, csl(c)]
            khd_fn = lambda prr, hp, c: YY5[prr, hp, c, 0, :]
            rd_fn = lambda prr, hp, c: YY5[prr, hp, c, 1, :]
            p_ = abmat(KT, kt_fn, self.YY, khd_fn)
            self.tt("dve", self.AktT, self.AktT[:], p_, p_[:], self.consts, mst, ALU.mult)
            self.psum.put(p_)
            p_ = abmat(KT, kt_fn, self.YY, rd_fn)
            self.tt("dve", self.BktT, self.BktT[:], p_, p_[:], self.consts, mit, ALU.mult)
            self.psum.put(p_)
            p_ = abmat(BD, bd_fn, self.YY, rd_fn)
            self.tt("dve", self.BbT, self.BbT[:], p_, p_[:], self.consts, mit, ALU.mult)
            self.psum.put(p_)
            Zc, Xc = self.Zp[0], self.Xp[0]
            p_ = abmat(BD, bd_fn, self.YY, khd_fn)
            self.stt("dve", Zc, Zc[:], p_, p_[:], -1.0, self.consts, mst, ALU.mult, ALU.mult)
            self.psum.put(p_)
            p_ = abmat(self.YY, khd_fn, BD, bd_fn)
            self.stt("dve", Xc, Xc[:], p_, p_[:], -1.0, self.consts, msx, ALU.mult, ALU.mult)
            self.psum.put(p_)
            RT32 = self.RT32
            RTb = self.RTb[0]
            self.tt("dve", RT32, RT32[:], Zc, Zc[:], self.consts, i64, ALU.add)
            self.cp("act", RTb, RTb[:], RT32, RT32[:])

            def sq(p_, lt, rt):
                for q in range(2):
                    for h in range(8):
                        self.mm(p_, p_[pqs[q], hsl(h)], lt, lt[pqs[q], hsl(h)], rt, rt[pqs[q], hsl(h)], rowgrp=q)
            for i in range(1, 6):
                lastit = (i == 5)
                Zn, Xn = self.Zp[i % 2], self.Xp[i % 2]
                px = self.psum.get()
                sq(px, Zc, Xc)
                if not lastit:
                    pz = self.psum.get()
                    sq(pz, Xc, Zc)
                self.cp("act", Xn, Xn[:], px, px[:])
                self.psum.put(px)
                if not lastit:
                    self.cp("dve", Zn, Zn[:], pz, pz[:])
                    self.psum.put(pz)
                prt = self.psum.get()
                sq(prt, Xn, RTb)
                self.tt("dve", RT32, RT32[:], RT32, RT32[:], prt, prt[:], ALU.add)
                self.psum.put(prt)
                self.cp("act", RTb, RTb[:], RT32, RT32[:])
                Zc, Xc = Zn, Xn
            pY = self.psum.get()
            for q, c in enumerate(chunks):
                pq = pqs[q]
                pR = self.psum.get()
                for hp in range(4):
                    self.mm(pR, pR[pq, hp * 128:(hp + 1) * 128], self.YY, YY5[:, hp, c, 0, :], Sb, Sb[:, hp, :], True, False)
                    for hf in range(2):
                        h = 2 * hp + hf
                        self.mm(pR, pR[pq, hsl(h)], self.AktT, self.AktT[pq, hsl(h)], Vtm, Vtm[pq, hsl(h)], False, hf == 1, rowgrp=q)
                self.cp("act", self.RHSb, self.RHSb[pq, :], pR, pR[pq, :])
                self.psum.put(pR)
                pU = self.psum.get()
                for h in range(8):
                    self.mm(pU, pU[pq, hsl(h)], RTb, RTb[pq, hsl(h)], self.RHSb, self.RHSb[pq, hsl(h)], rowgrp=q)
                self.act(self.nU, self.nU[pq, :], pU, pU[pq, :], AF.Copy, scale=-1.0)
                pU4 = pU[pq, :].rearrange("p (hp hf v) -> p hp hf v", hp=4, hf=2)
                for hf in range(2):
                    self.ts("dve", self.nUpad, nUpad4[pq, :, hf, hf * 64:(hf + 1) * 64], pU, pU4[:, :, hf, :], -1.0)
                self.psum.put(pU)
                for hp in range(4):
                    o = pY[:, q * 256 + hp * 64:q * 256 + (hp + 1) * 64]
                    self.mm(pY, o, Sb, Sb[:, hp, :], self.YY, YY5[:, hp, c, 1, :], True, False)
                    for hf in range(2):
                        h = 2 * hp + hf
                        self.mm(pY, o, Vpad, Vpad4[pq, hp, hf, :], self.BktT, self.BktT[pq, hsl(h)], False, False, rowgrp=q)
                        self.mm(pY, o, self.nUpad, nUpad4[pq, hp, hf, :], self.BbT, self.BbT[pq, hsl(h)], False, hf == 1, rowgrp=q)
                pS = self.psum.get()
                for hp in range(4):
                    hc = slice(hp * 128, (hp + 1) * 128)
                    self.mm(pS, pS[:, hc], KTtm, KTtm[pq, hc], Vtm, Vtm[pq, hc], True, False, rowgrp=q)
                    self.mm(pS, pS[:, hc], BDtm, BDtm[pq, hc], self.nU, self.nU[pq, hc], False, True, rowgrp=q)
                for hp in range(4):
                    hc = slice(hp * 128, (hp + 1) * 128)
                    t2 = self.stmp[hp % 2]
                    pc = self.PC[:, hp, c:c + 1]
                    self.stt("dve", t2, t2[:], pS, pS[:, hc], pc, self.consts, self.cst(C_B64ONES), ALU.mult, ALU.mult, extra=[self.PC])
                    self.stt("dve", S32, S32[:, hp, :], S32, S32[:, hp, :], pc, t2, t2[:], ALU.mult, ALU.add, extra=[self.PC])
                    self.cp("act", Sb, Sb[:, hp, :], S32, S32[:, hp, :])
                self.psum.put(pS)
            pY4 = pY[:].rearrange("p (q hp t) -> p q hp t", q=2, hp=4)
            for hp in range(4):
                self.cp("act", yraw[hp], yraw[hp][:, cpair * 128:(cpair + 1) * 128].rearrange("p (q t) -> p q t", q=2), pY, pY4[:, :, hp, :])
            self.psum.put(pY)
        for hp in range(4):
            o = yraw[hp]
            t1 = self.tmp.get()
            self.head_norm(o, t1, C_B64MEAN, 64e-5)
            self.stt("dve", o, o[:], o, o[:], sm[:, O_WGN + hp:O_WGN + hp + 1], bon, bon[:, hp, :], ALU.mult, ALU.add, extra=[sm])
            self.tt("dve", self.yrw, self.yrw[:, hp, :], o, o[:], g_rw, g_rw[:, hp, :], ALU.mult)
            self.tmp.put(t1)
        self.tmp.put(*yraw)

    def merge_stage(self, l, b, src_v, srcb):
        S = self.S
        sm = self.small[l]
        der = self.der[l]
        t0 = b * TB
        tsl = slice(t0, t0 + TB)
        macc = [self.rr, self.kk]
        merged = self.YY[:].rearrange("p (k t) -> p k t", k=8)
        wg_v = self.w_gate[l].rearrange("(k p) c -> p k c", p=128)
        ys = (self.yrt, self.yrw, self.ycv)
        for i in range(3):
            wb_v = self.w_branch[l, i].rearrange("(k p) c -> p k c", p=128)
            for half in range(2):
                wg = self.wnext()
                wb = self.wnext()
                for dcl in range(4):
                    cols = slice(dcl * 128, (dcl + 1) * 128)
                    pA, pB = self.psum.get(), self.psum.get()
                    for k in range(4):
                        self.mm(pA, pA[:], wb, wb[:, k, cols], ys[i], ys[i][:, k, :], k == 0, k == 3)
                    self.proj(pB, wg, cols)
                    sg = self.tmp.get()
                    dc = half * 4 + dcl
                    self.act(sg, sg[:], pB, pB[:], AF.Sigmoid, bias=sm[:, O_BGATE + i * 8 + dc:O_BGATE + i * 8 + dc + 1], scale=1.0, extra=[sm])
                    self.psum.put(pB)
                    m = macc[half]
                    if i == 0:
                        self.tt("dve", m, m[:, dcl, :], pA, pA[:], sg, sg[:], ALU.mult)
                    else:
                        self.tt("dve", sg, sg[:], pA, pA[:], sg, sg[:], ALU.mult)
                        if i < 2:
                            self.tt("pool", m, m[:, dcl, :], m, m[:, dcl, :], sg, sg[:], ALU.add)
                        else:
                            self.tt("pool", self.YY, merged[:, dc, :], m, m[:, dcl, :], sg, sg[:], ALU.add)
                    self.psum.put(pA)
                    self.tmp.put(sg)
                self.wput(wg, wb)
        wo_v = self.w_o[l].rearrange("(k p) c -> p k c", p=128)
        dst_v = self.xmid.rearrange("(k p) t -> p k t", p=128)
        for half in range(2):
            wo = self.wnext()
            for dcl in range(4):
                dc = half * 4 + dcl
                p_ = self.psum.get()
                for k in range(8):
                    self.mm(p_, p_[:], wo, wo[:, k, dcl * 128:(dcl + 1) * 128], self.YY, merged[:, k, :], k == 0, k == 7)
                xt = self.tmp.get()
                S.dma("sp", xt[:], src_v[:, dc, tsl], reads=([srcb[b]] if srcb else []), writes=[xt])
                self.stt("dve", xt, xt[:], p_, p_[:], self.gate1(l, dc), xt, xt[:], ALU.mult, ALU.add, extra=[der])
                self.psum.put(p_)
                S.dma("sp", dst_v[:, dc, tsl], xt[:], reads=[xt], writes=[self.db["xmid"][b]])
                if self.dbg:
                    S.dma("sp", self.dbg_out["d_xmid"][l].rearrange("(k p) t -> p k t", p=128)[:, dc, tsl], xt[:], reads=[xt])
                self.tmp.put(xt)
            self.wput(wo)

    def moe_alloc(self, st):
        sb = lambda n, s, d: self.sb(st, n, s, d)
        self.tmp = Pool_([sb(f"mtmp{i}", [128, 512], F32) for i in range(14)])
        self.h2T = sb("h2T", [128, 8, 1024], BF16)
        self.btmp = Pool_([sb(f"dbtmp{i}", [128, 512], BF16) for i in range(2)])
        self.acc = sb("acc", [128, 8, 1024], F32)
        self.Wt = sb("Wt", [128, 8, 32], F32)
        self.wr32 = sb("wr32", [128, 8, 36], F32)
        self.br = sb("br", [128, 36], F32)
        self.lg = sb("lg", [128, 36], F32)
        self.rt = sb("rt", [128, 64], F32)
        self.wge = [sb(f"wge{i}", [128, 8, 512], BF16) for i in range(2)]
        self.wue = [sb(f"wue{i}", [128, 8, 512], BF16) for i in range(2)]
        self.wde = [sb(f"wde{i}", [128, 4, 1024], BF16) for i in range(2)]
        self.actT = [sb(f"actT{i}", [128, 4, 512], BF16) for i in range(2)]
        self.moe_l = None

    def route_tile(self, l, ti):
        lg, rt, Wt = self.lg, self.rt, self.Wt
        S = self.S
        c = lambda i, n=1: rt[:, i:i + n]
        gmax, ngmax, gsum, pg, m1, m2, d_, e_, w1, w2 = (c(i) for i in range(10))
        goh, ge, sel, oh1, oh2, sel2, we = c(16, 4), c(20, 4), c(24, 8), c(32, 8), c(40, 8), c(48, 8), c(56, 8)
        R = lambda out_ap, in_ap: S.op("dve", lambda e: e.tensor_reduce(out=out_ap, in_=in_ap, axis=AX.X, op=ALU.max), [lg, rt], [rt])
        R(gmax, lg[:, 0:4])
        self.ts("dve", rt, goh, lg, lg[:, 0:4], gmax, None, ALU.is_equal, extra=[rt])
        self.ts("dve", rt, ngmax, rt, gmax, -1.0)
        self.act(rt, ge, lg, lg[:, 0:4], AF.Exp, bias=ngmax, scale=1.0, extra=[rt])
        S.op("dve", lambda e: e.tensor_reduce(out=gsum, in_=ge, axis=AX.X, op=ALU.add), [rt], [rt])
        self.recip(rt, pg, rt, gsum)
        el = lambda g: lg[:, 4 + g * 8:12 + g * 8]
        self.ts("dve", rt, sel, lg, el(0), goh[:, 0:1], extra=[rt])
        for g in range(1, 4):
            self.stt("dve", rt, sel, lg, el(g), goh[:, g:g + 1], rt, sel, ALU.mult, ALU.add)
        R(m1, sel)
        self.ts("dve", rt, oh1, rt, sel, m1, None, ALU.is_equal)
        self.stt("dve", rt, sel2, rt, oh1, -1e30, rt, sel, ALU.mult, ALU.add)
        R(m2, sel2)
        self.ts("dve", rt, oh2, rt, sel2, m2, None, ALU.is_equal)
        self.tt("dve", rt, d_, rt, m2, rt, m1, ALU.subtract)
        self.act(rt, e_, rt, d_, AF.Exp)
        self.ts("dve", rt, w1, rt, e_, 1.0, None, ALU.add)
        self.recip(rt, w1, rt, w1)
        self.tt("dve", rt, w2, rt, e_, rt, w1, ALU.mult)
        self.tt("dve", rt, w1, rt, w1, rt, pg, ALU.mult)
        self.tt("dve", rt, w2, rt, w2, rt, pg, ALU.mult)
        self.ts("dve", rt, we, rt, oh1, w1)
        self.stt("dve", rt, we, rt, oh2, w2, rt, we, ALU.mult, ALU.add)
        for g in range(4):
            self.ts("dve", Wt, Wt[:, ti, g * 8:(g + 1) * 8], rt, we, goh[:, g:g + 1])

    def moe_super(self, l, sbk, dst, dstb, last):
        S = self.S
        sm = self.small[l]
        der = self.der[l]
        xm_v = self.xmid.rearrange("(k p) t -> p k t", p=128)
        dst_v = dst.rearrange("(k p) t -> p k t", p=128)
        if self.moe_l != l:
            self.moe_l = l
            S.dma("sp", self.wr32[:], self.w_r[l].rearrange("(k p) c -> p k c", p=128), writes=[self.wr32])
            S.dma("sp", self.br[:], self.b_r[l], writes=[self.br])
        self.memset(self.acc, self.acc[:], 0.0)
        if "moe" in self.stages:
            for pc in range(2):
                b = sbk * 2 + pc
                tsl = slice(b * TB, (b + 1) * TB)
                xs = [self.tmp.get() for _ in range(8)]
                for k in range(8):
                    S.dma("sp", xs[k][:], xm_v[:, k, tsl], reads=[self.db["xmid"][b]], writes=[xs[k]])
                self.norm_block(xs, lambda k: self.G2(l, k), lambda k: self.shift2(l, k), lambda k: (xs[k], xs[k][:]), der)
                for k in range(8):
                    self.cp("pool", self.h2T, self.h2T[:, k, pc * 512:(pc + 1) * 512], xs[k], xs[k][:])
                for tl in range(4):
                    ti = pc * 4 + tl
                    pl = self.psum.get()
                    for k in range(8):
                        self.mm(pl, pl[:, 0:36], xs[k], xs[k][:, tl * 128:(tl + 1) * 128], self.wr32, self.wr32[:, k, :], k == 0, k == 7)
                    self.tt("dve", self.lg, self.lg[:], pl, pl[:, 0:36], self.br, self.br[:], ALU.add)
                    self.psum.put(pl)
                    self.route_tile(l, ti)
                self.tmp.put(*xs)
            for e_i in range(NE):
                wg, wu, wd, = self.wge[e_i % 2], self.wue[e_i % 2], self.wde[e_i % 2]
                self.wload(wg, self.w_eg[l, e_i].rearrange("(k p) c -> p k c", p=128))
                self.wload(wu, self.w_eu[l, e_i].rearrange("(k p) c -> p k c", p=128))
                self.wload(wd, self.w_ed[l, e_i].rearrange("(k p) c -> p k c", p=128))
                for pc in range(2):
                    aT = self.actT[pc]
                    for fc in range(4):
                        cols = slice(fc * 128, (fc + 1) * 128)
                        pG, pU = self.psum.get(), self.psum.get()
                        for k in range(8):
                            self.mm(pG, pG[:], wg, wg[:, k, cols], self.h2T, self.h2T[:, k, pc * 512:(pc + 1) * 512], k == 0, k == 7)
                        for k in range(8):
                            self.mm(pU, pU[:], wu, wu[:, k, cols], self.h2T, self.h2T[:, k, pc * 512:(pc + 1) * 512], k == 0, k == 7)
                        sg = self.tmp.get()
                        self.act(sg, sg[:], pG, pG[:], AF.Silu)
                        self.psum.put(pG)
                        self.tt("dve", aT, aT[:, fc, :], pU, pU[:], sg, sg[:], ALU.mult)
                        self.psum.put(pU)
                        self.tmp.put(sg)
                    for tl in range(4):
                        ti = pc * 4 + tl
                        for dh in range(2):
                            pD = self.psum.get()
                            for fc in range(4):
                                self.mm(pD, pD[:], aT, aT[:, fc, tl * 128:(tl + 1) * 128], wd, wd[:, fc, dh * 512:(dh + 1) * 512], fc == 0, fc == 3)
                            a_ap = self.acc[:, ti, dh * 512:(dh + 1) * 512]
                            self.stt("dve", self.acc, a_ap, pD, pD[:], self.Wt[:, ti, e_i:e_i + 1], self.acc, a_ap, ALU.mult, ALU.add, extra=[self.Wt])
                            self.psum.put(pD)
        for pc in range(2):
            b = sbk * 2 + pc
            tsl = slice(b * TB, (b + 1) * TB)
            xs = []
            for dc in range(8):
                pT = self.psum.get()
                for tl in range(4):
                    ti = pc * 4 + tl
                    self.mm(pT, pT[:, tl * 128:(tl + 1) * 128], self.acc, self.acc[:, ti, dc * 128:(dc + 1) * 128], self.consts, self.cst(C_ID))
                xt = self.tmp.get()
                S.dma("sp", xt[:], xm_v[:, dc, tsl], reads=[self.db["xmid"][b]], writes=[xt])
                self.stt("dve", xt, xt[:], pT, pT[:], self.gate2(l, dc), xt, xt[:], ALU.mult, ALU.add, extra=[der])
                self.psum.put(pT)
                if self.dbg:
                    S.dma("sp", self.dbg_out["d_xl"][l].rearrange("(k p) t -> p k t", p=128)[:, dc, tsl], xt[:], reads=[xt])
                if last:
                    xs.append(xt)
                else:
                    S.dma("sp", dst_v[:, dc, tsl], xt[:], reads=[xt], writes=[dstb[b]])
                    self.tmp.put(xt)
            if last:
                fn = lambda k: sm[:, O_FN + k:O_FN + k + 1]
                self.norm_block(xs, fn, None, lambda k: (xs[k], xs[k][:]), sm)
                for dc in range(8):
                    S.dma("sp", dst_v[:, dc, tsl], xs[dc][:], reads=[xs[dc]], writes=[dstb[b]])
                self.tmp.put(*xs)


    def moe_alloc_sparse(self, st):
        sb = lambda n, s, d: self.sb(st, n, s, d)
        NT = self.T // 128
        self.tmp = Pool_([sb(f"stmp{i}", [128, 512], F32) for i in range(10)])
        self.h2T = sb("h2Ts", [128, 8, 512], BF16)
        self.btmp = Pool_([sb(f"sbtmp{i}", [128, 512], BF16) for i in range(2)])
        self.h2tm = [sb("h2tm0", [128, 1024], BF16)] * 2
        self.wr32 = sb("wr32s", [128, 8, 36], F32)
        self.br = sb("brs", [128, 36], F32)
        self.lg = sb("lgs", [128, 4, 36], F32)
        self.rt = sb("rts", [128, 4, 64], F32)
        self.ohg = sb("ohg", [128, 4, 64], F32)
        self.ohs = sb("ohs", [128, 4, 32], BF16)
        self.rk = sb("rk", [128, 4, 96], F32)
        self.mcarry = sb("mcarry", [128, 32], F32)
        self.E = [sb(f"E{j}", [128, NT], F32) for j in range(2)]
        self.R = [sb(f"R{j}", [128, NT], F32) for j in range(2)]
        self.W = [sb(f"W{j}", [128, NT], F32) for j in range(2)]
        self.Df = [sb(f"Df{j}", [128, NT], F32) for j in range(2)]
        self.Di = [sb(f"Di{j}", [128, NT], I32) for j in range(2)]
        self.tidf = sb("tidf", [128, NT], F32)
        self.tidi = sb("tidi", [128, NT], I32)
        self.cmp = sb("cmp", [128, 32 * 128], BF16)
        self.sm32 = sb("sm32", [128, 5, 32], F32)
        self.bef = sb("bef", [128, 128], F32)
        self.idxwf = sb("idxwf", [128, 128], F32)
        self.idxw = sb("idxw", [128, 128], I32)
        self.sent = sb("sent", [128, self.NRW // 128], I32)
        self.zrow = sb("zrow", [1, 1024], BF16)
        self.idxr = [sb(f"idxr{i}", [128, 1], I32) for i in range(4)]
        self.xg = [sb(f"xg{i}", [128, 1024], BF16) for i in range(3)]
        self.xgT = [sb(f"xgT{i}", [128, 8, BLK], BF16) for i in range(2)]
        self.wge = [sb(f"swge{i}", [128, 8, 512], BF16) for i in range(2)]
        self.wue = [sb(f"swue{i}", [128, 8, 512], BF16) for i in range(2)]
        self.wde = [sb(f"swde{i}", [128, 4, 1024], BF16) for i in range(2)]
        self.actT = [sb(f"sactT{i}", [128, 4, BLK], BF16) for i in range(2)]
        self.yb = [sb(f"yb{i}", [128, 1024], F32) for i in range(2)]
        self.mt = [sb(f"mt{i}", [128, 1024], F32) for i in range(4)]
        self.yg = [sb(f"yg{i}", [128, 1024], F32) for i in range(2)]
        assert self.NBLK <= 128

    def route_tile2(self, l, ti):
        lg, rt = self.lg, self.rt
        S = self.S
        c = lambda i, n=1: rt[:, i:i + n]
        gmax, ngmax, gsum, pg, m1, m2, d_, e_, w1, w2 = (c(i) for i in range(10))
        goh, ge, sel, oh1, oh2, sel2, scr = c(16, 4), c(20, 4), c(24, 8), c(32, 8), c(40, 8), c(48, 8), c(56, 8)
        R = lambda out_ap, in_ap: S.op("dve", lambda e: e.tensor_reduce(out=out_ap, in_=in_ap, axis=AX.X, op=ALU.max), [lg, rt], [rt])
        R(gmax, lg[:, 0:4])
        self.ts("dve", rt, goh, lg, lg[:, 0:4], gmax, None, ALU.is_equal, extra=[rt])
        self.ts("dve", rt, ngmax, rt, gmax, -1.0)
        self.act(rt, ge, lg, lg[:, 0:4], AF.Exp, bias=ngmax, scale=1.0, extra=[rt])
        S.op("dve", lambda e: e.tensor_reduce(out=gsum, in_=ge, axis=AX.X, op=ALU.add), [rt], [rt])
        self.recip(rt, pg, rt, gsum)
        el = lambda g: lg[:, 4 + g * 8:12 + g * 8]
        self.ts("dve", rt, sel, lg, el(0), goh[:, 0:1], extra=[rt])
        for g in range(1, 4):
            self.stt("dve", rt, sel, lg, el(g), goh[:, g:g + 1], rt, sel, ALU.mult, ALU.add)
        R(m1, sel)
        self.ts("dve", rt, oh1, rt, sel, m1, None, ALU.is_equal)
        self.stt("dve", rt, sel2, rt, oh1, -1e30, rt, sel, ALU.mult, ALU.add)
        R(m2, sel2)
        self.ts("dve", rt, oh2, rt, sel2, m2, None, ALU.is_equal)
        self.tt("dve", rt, d_, rt, m2, rt, m1, ALU.subtract)
        self.act(rt, e_, rt, d_, AF.Exp)
        self.ts("dve", rt, w1, rt, e_, 1.0, None, ALU.add)
        self.recip(rt, w1, rt, w1)
        self.tt("dve", rt, w2, rt, e_, rt, w1, ALU.mult)
        self.tt("dve", self.W[0], self.W[0][:, ti:ti + 1], rt, w1, rt, pg, ALU.mult)
        self.tt("dve", self.W[1], self.W[1][:, ti:ti + 1], rt, w2, rt, pg, ALU.mult)
        ohg, rk = self.ohg, self.rk
        for j, oh in enumerate((oh1, oh2)):
            for g in range(4):
                self.ts("dve", ohg, ohg[:, j * 32 + g * 8:j * 32 + (g + 1) * 8], rt, oh, goh[:, g:g + 1])
        self.tt("dve", self.ohs, self.ohs[:], ohg, ohg[:, 0:32], ohg, ohg[:, 32:64], ALU.add)
        pr = self.psum.get()
        self.mm(pr, pr[:, 0:32], self.cbf, self.cbf[:, 256:384], self.ohs, self.ohs[:])
        self.mm(pr, pr[:, 32:64], self.cbf, self.cbf[:, 384:512], self.ohs, self.ohs[:])
        self.tt("dve", rk, rk[:, 0:32], pr, pr[:, 0:32], self.mcarry, self.mcarry[:], ALU.add)
        self.tt("dve", self.mcarry, self.mcarry[:], self.mcarry, self.mcarry[:], pr, pr[:, 32:64], ALU.add)
        self.psum.put(pr)
        iota = self.cst(C_IOTA32, 32)
        for j in range(2):
            oj = ohg[:, j * 32:(j + 1) * 32]
            self.tt("dve", rk, rk[:, 32:64], ohg, oj, self.consts, iota, ALU.mult)
            S.op("dve", lambda e: e.tensor_reduce(out=self.E[j][:, ti:ti + 1], in_=rk[:, 32:64], axis=AX.X, op=ALU.add), [rk], [self.E[j]])
            self.tt("dve", rk, rk[:, 64:96], ohg, oj, rk, rk[:, 0:32], ALU.mult)
            S.op("dve", lambda e: e.tensor_reduce(out=self.R[j][:, ti:ti + 1], in_=rk[:, 64:96], axis=AX.X, op=ALU.add), [rk], [self.R[j]])

    def route_piece(self, l, pc):
        lg, rt, ohg, rk = self.lg, self.rt, self.ohg, self.rk
        S = self.S
        t0 = pc * 4
        c = lambda i, n=1: rt[:, :, i:i + n]
        bc = lambda ap, n: ap.broadcast_to([128, 4, n])
        gmax, gsum, pg, m1, m2, d_, e_, w1, w2 = (c(i) for i in range(9))
        goh, ge, sel, oh1, oh2, sel2, scr = c(16, 4), c(20, 4), c(24, 8), c(32, 8), c(40, 8), c(48, 8), c(56, 8)
        red = lambda out_ap, in_ap, op, rd, wr: self.reduce(wr[0], out_ap, rd[0], in_ap, op)
        gl = lg[:, :, 0:4]
        red(gmax, gl, ALU.max, [lg], [rt])
        self.tt("dve", rt, goh, lg, gl, rt, bc(gmax, 4), ALU.is_equal)
        self.tt("dve", rt, ge, lg, gl, rt, bc(gmax, 4), ALU.subtract)
        self.act(rt, ge, rt, ge, AF.Exp)
        red(gsum, ge, ALU.add, [rt], [rt])
        self.recip(rt, pg, rt, gsum)
        el = lambda g: lg[:, :, 4 + g * 8:12 + g * 8]
        self.tt("dve", rt, sel, lg, el(0), rt, bc(goh[:, :, 0:1], 8), ALU.mult)
        for g in range(1, 4):
            self.tt("dve", rt, scr, lg, el(g), rt, bc(goh[:, :, g:g + 1], 8), ALU.mult)
            self.tt("dve", rt, sel, rt, sel, rt, scr, ALU.add)
        red(m1, sel, ALU.max, [rt], [rt])
        self.tt("dve", rt, oh1, rt, sel, rt, bc(m1, 8), ALU.is_equal)
        self.stt("dve", rt, sel2, rt, oh1, -1e30, rt, sel, ALU.mult, ALU.add)
        red(m2, sel2, ALU.max, [rt], [rt])
        self.tt("dve", rt, oh2, rt, sel2, rt, bc(m2, 8), ALU.is_equal)
        self.tt("dve", rt, d_, rt, m2, rt, m1, ALU.subtract)
        self.act(rt, e_, rt, d_, AF.Exp)
        self.ts("dve", rt, w1, rt, e_, 1.0, None, ALU.add)
        self.recip(rt, w1, rt, w1)
        self.tt("dve", rt, w2, rt, e_, rt, w1, ALU.mult)
        wv = lambda j: self.W[j][:, t0:t0 + 4].rearrange("p (t o) -> p t o", o=1)
        self.tt("dve", self.W[0], wv(0), rt, w1, rt, pg, ALU.mult)
        self.tt("dve", self.W[1], wv(1), rt, w2, rt, pg, ALU.mult)
        for j, oh in enumerate((oh1, oh2)):
            for g in range(4):
                self.tt("dve", ohg, ohg[:, :, j * 32 + g * 8:j * 32 + (g + 1) * 8], rt, oh, rt, bc(goh[:, :, g:g + 1], 8), ALU.mult)
        self.tt("dve", self.ohs, self.ohs[:], ohg, ohg[:, :, 0:32], ohg, ohg[:, :, 32:64], ALU.add)
        pr = self.psum.get()
        for t in range(4):
            self.mm(pr, pr[:, t * 32:(t + 1) * 32], self.cbf, self.cbf[:, 256:384], self.ohs, self.ohs[:, t, :])
        for t in range(4):
            self.mm(pr, pr[:, 128 + t * 32:128 + (t + 1) * 32], self.cbf, self.cbf[:, 384:512], self.ohs, self.ohs[:, t, :])
        for t in range(4):
            self.tt("dve", rk, rk[:, t, 0:32], pr, pr[:, t * 32:(t + 1) * 32], self.mcarry, self.mcarry[:], ALU.add)
            self.tt("dve", self.mcarry, self.mcarry[:], self.mcarry, self.mcarry[:], pr, pr[:, 128 + t * 32:128 + (t + 1) * 32], ALU.add)
        self.psum.put(pr)
        iota_bc = self.cst(C_IOTA32, 32).rearrange("p (o e) -> p o e", o=1).broadcast_to([128, 4, 32])
        ev = lambda T_, j: T_[j][:, t0:t0 + 4]
        for j in range(2):
            oj = ohg[:, :, j * 32:(j + 1) * 32]
            self.tt("dve", rk, rk[:, :, 32:64], ohg, oj, self.consts, iota_bc, ALU.mult)
            red(ev(self.E, j), rk[:, :, 32:64], ALU.add, [rk], [self.E[j]])
            self.tt("dve", rk, rk[:, :, 64:96], ohg, oj, rk, rk[:, :, 0:32], ALU.mult)
            red(ev(self.R, j), rk[:, :, 64:96], ALU.add, [rk], [self.R[j]])

    def moe_sparse(self, l, dst, dstb, last):
        S = self.S
        T = self.T
        NT = T // 128
        NBLK, NRW = self.NBLK, self.NRW
        sm, der = self.small[l], self.der[l]
        xm_v = self.xmid.rearrange("(k p) t -> p k t", p=128)
        dst_v = dst.rearrange("(k p) t -> p k t", p=128)
        idb = self.cbf[:, 0:128]
        IOA = bass.IndirectOffsetOnAxis
        S.dma("sp", self.wr32[:], self.w_r[l].rearrange("(k p) c -> p k c", p=128), writes=[self.wr32])
        S.dma("sp", self.br[:], self.b_r[l], writes=[self.br])
        self.memset(self.mcarry, self.mcarry[:], 0.0)
        self.memset(self.zrow, self.zrow[:], 0.0)
        S.dma("sp", self.h2rows[T:T + 1, :], self.zrow[:], reads=[self.zrow], writes=[self.h2rows_b[NT]])
        real_tmp = list(self.tmp.free)
        views = [View(w, k, f"vw{i}_{k}") for i, w in enumerate(self.wge + self.wue + self.wde) for k in range(4)]
        assert tuple(views[0][:].shape) == (128, 512), views[0][:].shape
        self.tmp = Pool_(real_tmp + views)
        for pc in range(T // 512):
            tsl = slice(pc * TB, (pc + 1) * TB)
            xs = [self.tmp.get() for _ in range(8)]
            for k in range(8):
                S.dma("sp", xs[k][:], xm_v[:, k, tsl], reads=[self.db["xmid"][pc]], writes=[xs[k]])
            self.norm_block(xs, lambda k: self.G2(l, k), lambda k: self.shift2(l, k), lambda k: (xs[k], xs[k][:]), der)
            for k in range(8):
                self.cp("pool", self.h2T, self.h2T[:, k, :], xs[k], xs[k][:])
            pl = self.psum.get()
            for tl in range(4):
                for k in range(8):
                    self.mm(pl, pl[:, tl * 36:(tl + 1) * 36], xs[k], xs[k][:, tl * 128:(tl + 1) * 128], self.wr32, self.wr32[:, k, :], k == 0, k == 7)
            self.tt("dve", self.lg, self.lg[:], pl, pl[:, 0:144].rearrange("p (t c) -> p t c", t=4), self.br,
                    self.br[:].rearrange("p (o c) -> p o c", o=1).broadcast_to([128, 4, 36]), ALU.add)
            self.psum.put(pl)
            self.route_piece(l, pc)
            self.tmp.put(*xs)
            for tl in range(4):
                ti = pc * 4 + tl
                pA, pB = self.psum.get(), self.psum.get()
                for k in range(8):
                    p_ = pA if k < 4 else pB
                    self.mm(p_, p_[:, (k % 4) * 128:(k % 4 + 1) * 128], self.h2T, self.h2T[:, k, tl * 128:(tl + 1) * 128], self.cbf, idb)
                hm = self.h2tm[ti % 2]
                self.cp("act", hm, hm[:, 0:512], pA, pA[:])
                self.cp("dve", hm, hm[:, 512:1024], pB, pB[:])
                self.psum.put(pA, pB)
                S.dma("sp", self.h2rows[ti * 128:(ti + 1) * 128, :], hm[:], reads=[hm], writes=[self.h2rows_b[ti]])
        cnt = self.mcarry
        cmp, sm32 = self.cmp, self.sm32
        nblk, nblkB, pend, pstart, ones32 = (sm32[:, i, :] for i in range(5))
        mB = lambda n: self.cst(C_MB, n)
        for e_i in range(NE):
            self.ts("dve", cmp, cmp[:, e_i * 128:(e_i + 1) * 128], self.consts, mB(128), cnt[:, e_i:e_i + 1], None, ALU.is_lt, extra=[cnt])
        self.reduce(sm32, nblk, cmp, cmp[:, 0:32 * 128].rearrange("p (e m) -> p e m", m=128), ALU.add)
        self.ts("dve", sm32, nblkB, sm32, nblk, float(BLK))
        self.memset(sm32, ones32, 1.0)
        self.scan(sm32, pend, sm32, ones32, sm32, nblkB)
        self.tt("dve", sm32, pstart, sm32, pend, sm32, nblkB, ALU.subtract)
        for e_i in range(NE):
            self.ts("dve", cmp, cmp[:, e_i * NBLK:(e_i + 1) * NBLK], self.consts, mB(NBLK), pend[:, e_i:e_i + 1], None, ALU.is_ge, extra=[sm32])
        self.reduce(self.bef, self.bef[:, 0:NBLK], cmp, cmp[:, 0:32 * NBLK].rearrange("p (e b) -> p b e", b=NBLK), ALU.add)
        self.ts("dve", self.bef, self.bef[:, 0:NBLK], self.bef, self.bef[:, 0:NBLK], float(NE - 1), None, ALU.min)
        self.ts("dve", self.idxwf, self.idxwf[:, 0:NBLK], self.bef, self.bef[:, 0:NBLK], 128.0, self.cst(C_PID, 1), ALU.mult, ALU.add, extra=[self.consts])
        self.cp("dve", self.idxw, self.idxw[:, 0:NBLK], self.idxwf, self.idxwf[:, 0:NBLK])
        iota = self.cst(C_IOTA32, 32)
        CH = 16
        for j in range(2):
            for c0 in range(0, NT, CH):
                n = min(CH, NT - c0)
                oh = self.tmp.get()
                ohv = oh[:, 0:n * 32].rearrange("p (t e) -> p t e", e=32)
                self.tt("dve", oh, ohv, self.consts, iota.rearrange("p (o e) -> p o e", o=1).broadcast_to([128, n, 32]),
                        self.E[j], self.E[j][:, c0:c0 + n].rearrange("p (t o) -> p t o", o=1).broadcast_to([128, n, 32]), ALU.is_equal)
                self.tt("dve", oh, ohv, oh, ohv, sm32, pstart.rearrange("p (o e) -> p o e", o=1).broadcast_to([128, n, 32]), ALU.mult)
                self.reduce(self.Df[j], self.Df[j][:, c0:c0 + n], oh, ohv, ALU.add)
                self.tmp.put(oh)
            self.tt("dve", self.Df[j], self.Df[j][:], self.Df[j], self.Df[j][:], self.R[j], self.R[j][:], ALU.add)
            self.cp("dve", self.Di[j], self.Di[j][:], self.Df[j], self.Df[j][:])
        self.ts("dve", self.tidf, self.tidf[:], self.consts, self.cst(C_MB, NT), 128.0 / BLK, self.cst(C_PID, 1), ALU.mult, ALU.add)
        self.cp("dve", self.tidi, self.tidi[:], self.tidf, self.tidf[:])
        self.memset(self.sent, self.sent[:], int(T))
        S.dma("sp", self.rowtok.rearrange("(p n) o -> p (n o)", p=128), self.sent[:], reads=[self.sent], writes=[self.rowtok_fill_b])
        for j in range(2):
            for ti in range(NT):
                self.idma(self.rowtok[:, :], IOA(ap=self.Di[j][:, ti:ti + 1], axis=0), self.tidi[:, ti:ti + 1], None,
                          [self.Di[j], self.tidi, self.rowtok_fill_b], [self.scat_b[j * NT + ti]])
        S.barrier()
        self.tmp = Pool_(real_tmp)
        scat_all = [self.rowtok_fill_b] + self.scat_b[:2 * NT]
        for b in range(NBLK):
            wg, wu, wd = self.wge[b % 2], self.wue[b % 2], self.wde[b % 2]
            for which, w in enumerate((wg, wu, wd)):
                self.idma(w[:].rearrange("p k c -> p (k c)"), None, self.wexp[l][which][:, :], IOA(ap=self.idxw[:, b:b + 1], axis=0),
                          [self.idxw] + self.wexp_b[l][which * NE:(which + 1) * NE], [w])
            xgs = []
            for r in range(RT):
                rr_ = b * RT + r
                it = self.idxr[rr_ % 4]
                S.dma("sp", it[:], self.rowtok[rr_ * 128:(rr_ + 1) * 128, :], reads=scat_all, writes=[it])
                xg = self.xg[rr_ % 3]
                self.idma(xg[:], None, self.h2rows[:, :], IOA(ap=it[:, 0:1], axis=0), [it] + self.h2rows_b, [xg])
                xgs.append(xg)
            xgT = self.xgT[b % 2]
            for kq in range(4):
                p_ = self.psum.get()
                for kk in range(2):
                    k = kq * 2 + kk
                    for r in range(RT):
                        self.mm(p_, p_[:, kk * BLK + r * 128:kk * BLK + (r + 1) * 128], xgs[r], xgs[r][:, k * 128:(k + 1) * 128], self.cbf, idb)
                self.cp("act" if kq % 2 == 0 else "dve", xgT, xgT[:, kq * 2:kq * 2 + 2, :], p_, p_[:].rearrange("p (k n) -> p k n", k=2))
                self.psum.put(p_)
            aT = self.actT[b % 2]
            for fc in range(4):
                cols = slice(fc * 128, (fc + 1) * 128)
                pG, pU = self.psum.get(), self.psum.get()
                for k in range(8):
                    self.mm(pG, pG[:, 0:BLK], wg, wg[:, k, cols], xgT, xgT[:, k, :], k == 0, k == 7)
                for k in range(8):
                    self.mm(pU, pU[:, 0:BLK], wu, wu[:, k, cols], xgT, xgT[:, k, :], k == 0, k == 7)
                sg = self.tmp.get()
                self.act(sg, sg[:, 0:BLK], pG, pG[:, 0:BLK], AF.Silu)
                self.psum.put(pG)
                self.tt("dve", aT, aT[:, fc, :], pU, pU[:, 0:BLK], sg, sg[:, 0:BLK], ALU.mult)
                self.psum.put(pU)
                self.tmp.put(sg)
            for r in range(RT):
                rr_ = b * RT + r
                yb = self.yb[rr_ % 2]
                for dh in range(2):
                    pD = self.psum.get()
                    for fc in range(4):
                        self.mm(pD, pD[:], aT, aT[:, fc, r * 128:(r + 1) * 128], wd, wd[:, fc, dh * 512:(dh + 1) * 512], fc == 0, fc == 3)
                    self.cp("act" if dh == 0 else "dve", yb, yb[:, dh * 512:(dh + 1) * 512], pD, pD[:])
                    self.psum.put(pD)
                S.dma("sp", self.yrows[rr_ * 128:(rr_ + 1) * 128, :], yb[:], reads=[yb], writes=[self.yrows_b[rr_]])
        S.barrier()
        self.tmp = Pool_(real_tmp + [View(w, k, f"vx{i}_{k}") for i, w in enumerate(self.wge + self.wue + self.wde) for k in range(4)])
        mbs = [self.xg[0], self.xg[1], self.xg[2], self.h2tm[0]]
        for pc in range(T // 512):
            tsl = slice(pc * TB, (pc + 1) * TB)
            for tl in range(4):
                ti = pc * 4 + tl
                m = self.mt[tl]
                ys = []
                for j in range(2):
                    y = m if j == 0 else self.yg[ti % 2]
                    self.idma(y[:], None, self.yrows[:, :], IOA(ap=self.Di[j][:, ti:ti + 1], axis=0), [self.Di[j]] + self.yrows_b, [y])
                    ys.append(y)
                self.ts("dve", m, m[:], ys[0], ys[0][:], self.W[0][:, ti:ti + 1], extra=[self.W[0]])
                mb = mbs[tl]
                self.stt("dve", mb, mb[:], ys[1], ys[1][:], self.W[1][:, ti:ti + 1], m, m[:], ALU.mult, ALU.add, extra=[self.W[1]])
            xs = []
            for dc in range(8):
                pT = self.psum.get()
                for tl in range(4):
                    self.mm(pT, pT[:, tl * 128:(tl + 1) * 128], mbs[tl], mbs[tl][:, dc * 128:(dc + 1) * 128], self.cbf, idb)
                xt = self.tmp.get()
                S.dma("sp", xt[:], xm_v[:, dc, tsl], reads=[self.db["xmid"][pc]], writes=[xt])
                self.stt("dve", xt, xt[:], pT, pT[:], self.gate2(l, dc), xt, xt[:], ALU.mult, ALU.add, extra=[der])
                self.psum.put(pT)
                if self.dbg:
                    S.dma("sp", self.dbg_out["d_xl"][l].rearrange("(k p) t -> p k t", p=128)[:, dc, tsl], xt[:], reads=[xt])
                if last:
                    xs.append(xt)
                else:
                    S.dma("sp", dst_v[:, dc, tsl], xt[:], reads=[xt], writes=[dstb[pc]])
                    self.tmp.put(xt)
            if last:
                fn = lambda k: sm[:, O_FN + k:O_FN + k + 1]
                self.norm_block(xs, fn, None, lambda k: (xs[k], xs[k][:]), sm)
                for dc in range(8):
                    S.dma("sp", dst_v[:, dc, tsl], xs[dc][:], reads=[xs[dc]], writes=[dstb[pc]])
                self.tmp.put(*xs)


def make_consts():
    c = np.zeros((128, NCONST), np.float64)
    c[:, C_ID:C_ID + 128] = np.eye(128)
    c[:, C_MEAND:C_MEAND + 128] = 1.0 / 1024.0
    c[:, C_MEAN128:C_MEAN128 + 128] = 1.0 / 128.0
    blk = np.kron(np.eye(2), np.ones((64, 64)))
    c[:, C_B64MEAN:C_B64MEAN + 128] = blk / 64.0
    c[:, C_B64ONES:C_B64ONES + 128] = blk
    R = np.zeros((128, 128))
    for m in range(64):
        R[m, m + 64] = -1.0
        R[m + 64, m] = 1.0
    c[:, C_RMT:C_RMT + 128] = R.T
    j = np.arange(128)[:, None]
    i = np.arange(128)[None, :]
    for h in range(4):
        lg = math.log1p(-2.0 ** (-5 - h))
        c[:, C_DT + h * 128:C_DT + (h + 1) * 128] = np.where(i >= j, np.exp(lg * np.maximum(i - j, 0)), 0.0) / math.sqrt(128.0)
        c[:, C_XI + h * 128:C_XI + (h + 1) * 128] = np.exp(lg * (i + 1.0)) + 0 * j
        c[:, C_ZETA + h] = np.exp(lg * (127.0 - np.arange(128))) / math.sqrt(128.0)
    invf = (np.float32(10000.0) ** (-(np.arange(64, dtype=np.float32) / np.float32(64)))).astype(np.float64)
    c[:, C_INVF] = np.concatenate([invf, invf])
    s = np.arange(64)[:, None]
    t = np.arange(64)[None, :]
    for r0 in (0, 64):
        c[r0:r0 + 64, C_MST:C_MST + 512] = np.tile((s < t).astype(np.float64), (1, 8))
        c[r0:r0 + 64, C_MIT:C_MIT + 512] = np.tile((s <= t).astype(np.float64), (1, 8))
        c[r0:r0 + 64, C_MSX:C_MSX + 512] = np.tile((s > t).astype(np.float64), (1, 8))
        c[r0:r0 + 64, C_I64:C_I64 + 512] = np.tile(np.eye(64), (1, 8))
    seg = np.ones(512)
    seg[::64] = 0.0
    c[:, C_SEG:C_SEG + 512] = seg[None, :]
    c[:, C_IOTA32:C_IOTA32 + 32] = np.arange(32)[None, :]
    c[:, C_MB:C_MB + 256] = (np.arange(256) * BLK)[None, :]
    c[:, C_PID] = np.arange(128)
    tp = np.arange(128)[:, None]
    tt_ = np.arange(128)[None, :]
    c[:, C_LTRI:C_LTRI + 128] = (tp < tt_).astype(np.float64)
    c[:, C_ONES:C_ONES + 128] = 1.0
    return c.astype(np.float32)


def colpack(v):
    v = np.asarray(v, np.float32).reshape(-1, 128)
    return np.ascontiguousarray(v.T)


def prep_shared(inp):
    Lr = L_DEPTH
    small = np.zeros((Lr, 128, NS), np.float32)
    for l in range(Lr):
        parts = [inp["norm1_g"][l], inp["norm2_g"][l], inp["b_ada"][l], inp["b_gate"][l], inp["ret_gn_g"][l], inp["rwkv_mu"][l],
                 inp["rwkv_w0"][l], inp["rwkv_a0"][l], inp["rwkv_k_k"][l], inp["rwkv_k_a"][l], inp["rwkv_r_k"][l].reshape(-1),
                 inp["rwkv_gn_g"][l], inp["conv_w"][l][0], inp["conv_w"][l][1], inp["conv_w"][l][2], inp["final_norm_g"]]
        small[l] = np.concatenate([colpack(p) for p in parts], axis=1)
    w_r = np.ascontiguousarray(np.concatenate([inp["w_router_group"], inp["w_router_expert"]], axis=2), dtype=np.float32)
    b_r1 = np.concatenate([inp["b_router_group"], inp["b_router_expert"]], axis=1).astype(np.float32)
    b_r = np.ascontiguousarray(np.broadcast_to(b_r1[:, None, :], (Lr, 128, 36)))
    sh = dict(small=small, consts=make_consts(), w_r=w_r, b_r=b_r)
    for k in ("w_ada", "w_in", "w_gate", "rwkv_w_lora", "rwkv_a_lora", "rwkv_g_lora", "w_branch", "w_o",
              "w_exp_gate", "w_exp_up", "w_exp_down"):
        sh[k] = np.ascontiguousarray(inp[k], dtype=np.float32)
    return sh


def core_inputs(inp, sh, bidx, T):
    m = dict(sh)
    m["xT"] = np.ascontiguousarray(np.asarray(inp["x"][bidx, :T], np.float32).T)
    m["pos"] = np.ascontiguousarray(np.asarray(inp["positions"][bidx, :T], np.int32)[None, :])
    m["cT"] = colpack(inp["c"][bidx])
    return m


_NC_CACHE = {}


def kernel(**inputs):
    inp = {k: np.asarray(v) for k, v in inputs.items()}
    B, T, _ = inp["x"].shape
    if T not in _NC_CACHE:
        _NC_CACHE[T] = Builder(T).build()
    nc = _NC_CACHE[T]
    sh = prep_shared(inp)
    in_maps = [core_inputs(inp, sh, b, T) for b in range(B)]
    res = run_bass_kernel_spmd(nc, in_maps, core_ids=list(range(B)))
    out = np.stack([np.ascontiguousarray(res.results[b]["outT"].T) for b in range(B)], axis=0)
    return out.astype(np.float32)
```
